# Optimizing a Trainium2 kernel written in Bass

```python
import jax
import jax.numpy as jnp
from jax import lax
import numpy as np


D_MODEL = 1024
BATCH = 4
SEQ = 4096
DEPTH = 2

CHUNK = 64
Q_BLOCK = 128

N_BRANCH = 4
BRANCH_WIDTH = 512

SC_WIDTH = 512
SC_CONV_LEN = 3

CF_WIDTH = 512
CF_CONV_LEN = 31

MLA_HEADS = 8
MLA_NOPE = 64
MLA_ROPE = 32
MLA_V = 64
MLA_Q_RANK = 256
MLA_KV_RANK = 128
ROPE_THETA = 10000.0

SB_HEADS = 8
SB_HEAD_DIM = 64

D_FF = 3584
N_EXPERTS = 8
TOP_K = 2
N_DENSE = (DEPTH + 1) // 2
N_MOE = DEPTH // 2

DN_ALPHA = (2 * DEPTH) ** 0.25
DN_BETA = (8 * DEPTH) ** -0.25

LN_EPS = 1e-5
RMS_EPS = 1e-6

IN_SIZES = (
    3 * SC_WIDTH,
    2 * CF_WIDTH,
    MLA_Q_RANK,
    MLA_KV_RANK,
    MLA_ROPE,
    3 * SB_HEADS * SB_HEAD_DIM,
    N_BRANCH * D_MODEL,
)
IN_COLS = sum(IN_SIZES)

kernel_name = "hybrid_gated_conv_mla_stickbreak_moe"


def _layer_norm(x, g, b):
    xf = x.astype(jnp.float32)
    mu = jnp.mean(xf, axis=-1, keepdims=True)
    var = jnp.mean(jnp.square(xf - mu), axis=-1, keepdims=True)
    y = (xf - mu) * lax.rsqrt(var + LN_EPS)
    return (y * g.astype(jnp.float32) + b.astype(jnp.float32)).astype(x.dtype)


def _rms_norm(x, g):
    xf = x.astype(jnp.float32)
    y = xf * lax.rsqrt(jnp.mean(jnp.square(xf), axis=-1, keepdims=True) + RMS_EPS)
    return (y * g.astype(jnp.float32)).astype(x.dtype)


def _rope_tables(positions):
    half = MLA_ROPE // 2
    inv_freq = 1.0 / (ROPE_THETA ** (jnp.arange(half, dtype=jnp.float32) * (2.0 / MLA_ROPE)))
    ang = positions.astype(jnp.float32)[..., None] * inv_freq
    return jnp.cos(ang), jnp.sin(ang)


def _apply_rope(x, cos, sin):
    half = x.shape[-1] // 2
    x1, x2 = x[..., :half], x[..., half:]
    cos = cos.astype(x.dtype)
    sin = sin.astype(x.dtype)
    return jnp.concatenate([x1 * cos - x2 * sin, x1 * sin + x2 * cos], axis=-1)


def _causal_dwconv(u, w):
    n_taps = w.shape[0]
    u_pad = jnp.pad(u, ((0, 0), (n_taps - 1, 0), (0, 0)))
    return lax.conv_general_dilated(
        u_pad, w[:, None, :].astype(u.dtype), window_strides=(1,), padding='VALID',
        dimension_numbers=('NWC', 'WIO', 'NWC'), feature_group_count=u.shape[-1])


def _split_cols(proj):
    parts = []
    start = 0
    for size in IN_SIZES:
        parts.append(proj[..., start:start + size])
        start += size
    return parts


def _short_gated_conv(b_gate, c_gate, h, w_conv):
    return b_gate * _causal_dwconv(c_gate * h, w_conv)


def _conformer_conv(val, gate, w_dw, b_dw, ln_g, ln_b):
    u = val * jax.nn.sigmoid(gate)
    u = _causal_dwconv(u, w_dw) + b_dw.astype(u.dtype)
    return jax.nn.silu(_layer_norm(u, ln_g, ln_b))


def _mla_attention(c_q, c_kv, k_rope_raw, cos, sin, q_norm, w_uq, kv_norm, w_ukv):
    bsz, seq, _ = c_q.shape
    q = (_rms_norm(c_q, q_norm) @ w_uq).reshape(bsz, seq, MLA_HEADS, MLA_NOPE + MLA_ROPE)
    q_nope = q[..., :MLA_NOPE]
    q_rope = _apply_rope(q[..., MLA_NOPE:], cos[:, :, None, :], sin[:, :, None, :])
    kv = (_rms_norm(c_kv, kv_norm) @ w_ukv).reshape(bsz, seq, MLA_HEADS, MLA_NOPE + MLA_V)
    k_nope, v = kv[..., :MLA_NOPE], kv[..., MLA_NOPE:]
    k_rope = _apply_rope(k_rope_raw, cos, sin)
    scale = (MLA_NOPE + MLA_ROPE) ** -0.5
    outs = []
    for blk in range(seq // Q_BLOCK):
        q0 = blk * Q_BLOCK
        q1 = q0 + Q_BLOCK
        s = (jnp.einsum('bqhd,bkhd->bhqk', q_nope[:, q0:q1], k_nope[:, :q1])
             + jnp.einsum('bqhd,bkd->bhqk', q_rope[:, q0:q1], k_rope[:, :q1])).astype(jnp.float32) * scale
        tq = jnp.arange(q0, q1)
        tk = jnp.arange(q1)
        allowed = (tk[None, :] // CHUNK) <= (tq[:, None] // CHUNK)
        s = jnp.where(allowed, s, -jnp.inf)
        p = jax.nn.softmax(s, axis=-1).astype(v.dtype)
        outs.append(jnp.einsum('bhqk,bkhd->bqhd', p, v[:, :q1]))
    return jnp.concatenate(outs, axis=1).reshape(bsz, seq, MLA_HEADS * MLA_V)


def _stick_breaking_attention(qkv):
    bsz, seq, _ = qkv.shape
    r = qkv.reshape(bsz, seq, 3, SB_HEADS, SB_HEAD_DIM)
    q, k, v = r[:, :, 0], r[:, :, 1], r[:, :, 2]
    scale = SB_HEAD_DIM ** -0.5
    outs = []
    for blk in range(seq // Q_BLOCK):
        q0 = blk * Q_BLOCK
        q1 = q0 + Q_BLOCK
        z = jnp.einsum('bqhd,bkhd->bhqk', q[:, q0:q1], k[:, :q1]).astype(jnp.float32) * scale
        tq = jnp.arange(q0, q1)
        tk = jnp.arange(q1)
        before = tk[None, :] < tq[:, None]
        log_stay = jnp.where(before, jax.nn.log_sigmoid(-z), 0.0)
        log_pass = lax.cumsum(log_stay, axis=3, reverse=True) - log_stay
        a = jnp.where(before, jnp.exp(jax.nn.log_sigmoid(z) + log_pass), 0.0).astype(v.dtype)
        outs.append(jnp.einsum('bhqk,bkhd->bqhd', a, v[:, :q1]))
    return jnp.concatenate(outs, axis=1).reshape(bsz, seq, SB_HEADS * SB_HEAD_DIM)


def _mixer_sublayer(x, cos, sin, w_in, b_gate, sc_conv, cf_conv, cf_conv_bias, cf_ln_g, cf_ln_b,
                    mla_q_norm, mla_w_uq, mla_kv_norm, mla_w_ukv, w_branch, w_out):
    bsz, seq, d = x.shape
    proj = x @ w_in
    sc_in, cf_in, c_q, c_kv, k_rope_raw, sb_in, gate_pre = _split_cols(proj)
    sc_b, sc_c, sc_h = jnp.split(sc_in, 3, axis=-1)
    cf_val, cf_gate = jnp.split(cf_in, 2, axis=-1)
    branches = (
        _short_gated_conv(sc_b, sc_c, sc_h, sc_conv),
        _conformer_conv(cf_val, cf_gate, cf_conv, cf_conv_bias, cf_ln_g, cf_ln_b),
        _mla_attention(c_q, c_kv, k_rope_raw, cos, sin, mla_q_norm, mla_w_uq, mla_kv_norm, mla_w_ukv),
        _stick_breaking_attention(sb_in),
    )
    gates = jax.nn.sigmoid(gate_pre + b_gate).reshape(bsz, seq, N_BRANCH, d)
    merged = gates[:, :, 0] * (branches[0] @ w_branch[0])
    for n in range(1, N_BRANCH):
        merged = merged + gates[:, :, n] * (branches[n] @ w_branch[n])
    return merged @ w_out


def _swiglu(x, w_gate, w_up, w_down):
    return (jax.nn.silu(x @ w_gate) * (x @ w_up)) @ w_down


def _moe_swiglu(x, w_router, w_gate, w_up, w_down):
    bsz, seq, d = x.shape
    xf = x.reshape(bsz * seq, d)
    logits = (xf @ w_router).astype(jnp.float32)
    top_vals, top_idx = lax.top_k(logits, TOP_K)
    top_w = jax.nn.softmax(top_vals, axis=-1)
    combine = jnp.sum(jax.nn.one_hot(top_idx, N_EXPERTS, dtype=jnp.float32) * top_w[..., None], axis=1)
    out = jnp.zeros_like(xf)
    for e in range(N_EXPERTS):
        out = out + combine[:, e:e + 1].astype(xf.dtype) * _swiglu(xf, w_gate[e], w_up[e], w_down[e])
    return out.reshape(bsz, seq, d)


def setup_inputs(seed: int = 0) -> dict:
    key = jax.random.key(seed)
    ks = jax.random.split(key, 32)
    L = DEPTH
    D = D_MODEL

    def nrm(i, shape, scale):
        return jax.random.normal(ks[i], shape, jnp.float32) * scale

    x = nrm(0, (BATCH, SEQ, D), 1.0)
    offsets = jax.random.randint(ks[1], (BATCH, 1), 0, 4 * SEQ)
    positions = (offsets + jnp.arange(SEQ)[None, :]).astype(jnp.int32)
    return {
        'x': x,
        'positions': positions,
        'w_in': nrm(2, (L, D, IN_COLS), D ** -0.5),
        'b_gate': nrm(3, (L, N_BRANCH * D), 0.02),
        'sc_conv': nrm(4, (L, SC_CONV_LEN, SC_WIDTH), SC_CONV_LEN ** -0.5),
        'cf_conv': nrm(5, (L, CF_CONV_LEN, CF_WIDTH), CF_CONV_LEN ** -0.5),
        'cf_conv_bias': nrm(6, (L, CF_WIDTH), 0.02),
        'cf_ln_g': 1.0 + nrm(7, (L, CF_WIDTH), 0.02),
        'cf_ln_b': nrm(8, (L, CF_WIDTH), 0.02),
        'mla_q_norm': 1.0 + nrm(9, (L, MLA_Q_RANK), 0.02),
        'mla_w_uq': nrm(10, (L, MLA_Q_RANK, MLA_HEADS * (MLA_NOPE + MLA_ROPE)), MLA_Q_RANK ** -0.5),
        'mla_kv_norm': 1.0 + nrm(11, (L, MLA_KV_RANK), 0.02),
        'mla_w_ukv': nrm(12, (L, MLA_KV_RANK, MLA_HEADS * (MLA_NOPE + MLA_V)), MLA_KV_RANK ** -0.5),
        'w_branch': nrm(13, (L, N_BRANCH, BRANCH_WIDTH, D), BRANCH_WIDTH ** -0.5 * DN_BETA),
        'w_out': nrm(14, (L, D, D), D ** -0.5 * DN_BETA),
        'ln_mix_g': 1.0 + nrm(15, (L, D), 0.02),
        'ln_mix_b': nrm(16, (L, D), 0.02),
        'ln_ffn_g': 1.0 + nrm(17, (L, D), 0.02),
        'ln_ffn_b': nrm(18, (L, D), 0.02),
        'ffn_w_gate': nrm(19, (N_DENSE, D, D_FF), D ** -0.5),
        'ffn_w_up': nrm(20, (N_DENSE, D, D_FF), D ** -0.5),
        'ffn_w_down': nrm(21, (N_DENSE, D_FF, D), D_FF ** -0.5 * DN_BETA),
        'router_w': nrm(22, (N_MOE, D, N_EXPERTS), D ** -0.5),
        'exp_w_gate': nrm(23, (N_MOE, N_EXPERTS, D, D_FF), D ** -0.5),
        'exp_w_up': nrm(24, (N_MOE, N_EXPERTS, D, D_FF), D ** -0.5),
        'exp_w_down': nrm(25, (N_MOE, N_EXPERTS, D_FF, D), D_FF ** -0.5 * DN_BETA),
    }


def reference(x, positions, w_in, b_gate, sc_conv, cf_conv, cf_conv_bias, cf_ln_g, cf_ln_b,
              mla_q_norm, mla_w_uq, mla_kv_norm, mla_w_ukv, w_branch, w_out,
              ln_mix_g, ln_mix_b, ln_ffn_g, ln_ffn_b, ffn_w_gate, ffn_w_up, ffn_w_down,
              router_w, exp_w_gate, exp_w_up, exp_w_down):
    cos, sin = _rope_tables(positions)
    for layer in range(DEPTH):
        y = _mixer_sublayer(x, cos, sin, w_in[layer], b_gate[layer], sc_conv[layer], cf_conv[layer],
                            cf_conv_bias[layer], cf_ln_g[layer], cf_ln_b[layer], mla_q_norm[layer],
                            mla_w_uq[layer], mla_kv_norm[layer], mla_w_ukv[layer], w_branch[layer],
                            w_out[layer])
        x = _layer_norm(DN_ALPHA * x + y, ln_mix_g[layer], ln_mix_b[layer])
        if layer % 2 == 0:
            i = layer // 2
            f = _swiglu(x, ffn_w_gate[i], ffn_w_up[i], ffn_w_down[i])
        else:
            i = layer // 2
            f = _moe_swiglu(x, router_w[i], exp_w_gate[i], exp_w_up[i], exp_w_down[i])
        x = _layer_norm(DN_ALPHA * x + f, ln_ffn_g[layer], ln_ffn_b[layer])
    return x
```

```python
from contextlib import ExitStack
import numpy as np
import concourse.bass as bass
import concourse.mybir as mybir
from concourse.bass_utils import run_bass_kernel_spmd

F32 = mybir.dt.float32
BF16 = mybir.dt.bfloat16
I32 = mybir.dt.int32
AF = mybir.ActivationFunctionType
ALU = mybir.AluOpType
AX = mybir.AxisListType

ENGS = ("pe", "act", "dve", "pool", "sp")
EPOCH = 16000
N_DMA_SEMS = 16
DMA_MAX_USES = 1900
SAME_ENG_SYNC = True


class Buf:
    def __init__(self, t, name):
        self.t = t
        self.name = name
        self.w = None
        self.r = {}
        self.psum = False

    def __getitem__(self, idx):
        return self.t[idx]

    def sub(self):
        return Buf(self.t, self.name + "_sub")


class Prog:
    def __init__(self):
        self.nc = bass.Bass("TRN2", target_bir_lowering=False)
        self.stack = ExitStack()
        self.root = self.stack
        self.scopes = []
        self.ops = {e: [] for e in ENGS}
        self.count = {e: 0 for e in ENGS}
        self.seen = {e: {} for e in ENGS}
        self.sems = {}
        self.dma_uses = [0] * (3 * N_DMA_SEMS)
        self.dma_rr = {"sp": 0, "act": 0, "pool": 0}
        self.dma_sem_h = []
        self.n_dram = 0
        self.out_events = []
        self.cc_inc = 16

    def dram(self, name, shape, dt, kind):
        return self.nc.dram_tensor(name, list(shape), dt, kind=kind).ap()

    def sb(self, name, shape, dt):
        self.uid = getattr(self, "uid", 0) + 1
        name = "%s_u%d" % (name, self.uid)
        t = self.stack.enter_context(self.nc.sbuf_tensor(name, list(shape), dt))
        return Buf(t, name)

    def ps(self, name, shape, dt):
        self.uid = getattr(self, "uid", 0) + 1
        name = "%s_u%d" % (name, self.uid)
        t = self.stack.enter_context(self.nc.psum_tensor(name, list(shape), dt))
        b = Buf(t, name)
        b.psum = True
        return b

    def _sem(self, key):
        if key not in self.sems:
            self.sems[key] = self.root.enter_context(self.nc.semaphore("s_%s_%s" % key))
        return self.sems[key]

    def push_scope(self):
        self.scopes.append(self.stack)
        self.stack = ExitStack()

    def pop_scope(self):
        self.barrier()
        self.flush()
        self.stack.close()
        self.stack = self.scopes.pop()

    def _need(self, eng, ev, waits):
        if ev is None:
            return
        kind, src, val = ev
        if kind == "e":
            if src == eng and (eng == "pe" or not SAME_ENG_SYNC):
                return
            if self.seen[eng].get(src, 0) >= val:
                return
            self.seen[eng][src] = val
            ep = (val - 1) // EPOCH
            waits.append((self._sem((src, ep)), val - ep * EPOCH))
        else:
            k = ("d", src)
            if self.seen[eng].get(k, 0) >= val:
                return
            self.seen[eng][k] = val
            waits.append((self.dma_sem_h[src], val))

    def _deps(self, eng, reads, writes):
        waits = []
        for b in reads:
            self._need(eng, b.w, waits)
            if b.psum:
                for k, ev in b.r.items():
                    if k != eng:
                        self._need(eng, ev, waits)
        for b in writes:
            self._need(eng, b.w, waits)
            for ev in b.r.values():
                self._need(eng, ev, waits)
        return waits

    def _mark(self, ev, key, reads, writes):
        for b in reads:
            b.r[key] = ev
        for b in writes:
            b.w = ev
            b.r = {}

    def op(self, eng, fn, reads=(), writes=()):
        reads = [b for b in reads if b is not None]
        writes = [b for b in writes if b is not None]
        waits = self._deps(eng, reads, writes)
        self.count[eng] += 1
        n = self.count[eng]
        ep = (n - 1) // EPOCH
        sem = self._sem((eng, ep))
        self.ops[eng].append((waits, fn, sem, 1))
        self._mark(("e", eng, n), eng, reads, writes)

    def dma(self, q, out, in_, reads=(), writes=(), is_output=False, **kw):
        reads = [b for b in reads if b is not None]
        writes = [b for b in writes if b is not None]
        if not self.dma_sem_h:
            for i in range(3 * N_DMA_SEMS):
                self.dma_sem_h.append(
                    self.root.enter_context(self.nc.semaphore("dsem%d" % i)))
        waits = self._deps(q, reads, writes)
        qbase = {"sp": 0, "act": 1, "pool": 2}[q] * N_DMA_SEMS
        i = qbase + self.dma_rr[q]
        self.dma_rr[q] = (self.dma_rr[q] + 1) % N_DMA_SEMS
        if self.dma_uses[i] > 0:
            self._need(q, ("d", i, 16 * self.dma_uses[i]), waits)
        self.dma_uses[i] += 1
        assert self.dma_uses[i] <= DMA_MAX_USES, "too many DMAs"
        val = 16 * self.dma_uses[i]
        sem = self.dma_sem_h[i]

        def fn(e, out=out, in_=in_, kw=kw):
            return e.dma_start(out=out, in_=in_, **kw)

        self.ops[q].append((waits, fn, sem, 16))
        ev = ("d", i, val)
        self._mark(ev, ("d", i), reads, writes)
        if is_output:
            self.out_events.append(ev)
        return ev

    def collective(self, fn, reads=(), writes=(), inc=16):
        if not self.dma_sem_h:
            for i in range(3 * N_DMA_SEMS):
                self.dma_sem_h.append(
                    self.root.enter_context(self.nc.semaphore("dsem%d" % i)))
        waits = self._deps("pool", list(reads), list(writes))
        sem = self.stack.enter_context(self.nc.semaphore("ccsem%d" % len(self.dma_sem_h)))
        self.dma_sem_h.append(sem)
        self.dma_uses.append(1)
        i = len(self.dma_sem_h) - 1
        self.ops["pool"].append((waits, fn, sem, inc))
        ev = ("d", i, inc)
        self._mark(ev, ("d", i), list(reads), list(writes))
        return ev

    def barrier(self):
        evs = [("e", e, self.count[e]) for e in ENGS if self.count[e] > 0]
        for i, u in enumerate(self.dma_uses):
            if u > 0:
                val = 16 * u if i < 3 * N_DMA_SEMS else self.cc_inc
                evs.append(("d", i, val))
        for eng in ENGS:
            waits = []
            for ev in evs:
                self._need(eng, ev, waits)
            if waits:
                self.ops[eng].append((waits, None, None, 0))

    def flush(self):
        nc = self.nc
        ops = self.ops
        if not any(ops[e] for e in ENGS):
            return

        def run(e, lst):
            for waits, fn, sem, inc in lst:
                for s, v in waits:
                    e.wait_ge(s, v)
                if fn is not None:
                    ins = fn(e)
                    ins.then_inc(sem, inc)

        with nc.Block() as block:
            @block.sync
            def _(e):
                run(e, ops["sp"])

            @block.scalar
            def _(e):
                run(e, ops["act"])

            @block.vector
            def _(e):
                run(e, ops["dve"])

            @block.gpsimd
            def _(e):
                run(e, ops["pool"])

            @block.tensor
            def _(e):
                run(e, ops["pe"])
        self.ops = {e: [] for e in ENGS}

    def build(self):
        for ev in self.out_events:
            w = []
            self._need("sp", ev, w)
            if w:
                self.ops["sp"].append((w, None, None, 0))
        self.flush()
        self.root.close()
        return self.nc

    def mm(self, out_b, out_ap, lhsT_b, lhsT_ap, rhs_b, rhs_ap, start, stop, sgc=False):
        self.op("pe", lambda e: e.matmul(out_ap, lhsT_ap, rhs_ap, start=start, stop=stop,
                                         skip_group_check=sgc),
                reads=[lhsT_b, rhs_b], writes=[out_b])

    def tr(self, out_b, out_ap, in_b, in_ap, id_b, id_ap):
        self.op("pe", lambda e: e.transpose(out_ap, in_ap, id_ap),
                reads=[in_b, id_b], writes=[out_b])

    def act(self, out_b, out_ap, in_b, in_ap, func, bias=None, scale=None, extra_reads=()):
        kw = {}
        if bias is not None:
            kw["bias"] = bias
        if scale is not None:
            kw["scale"] = scale
        self.op("act", lambda e: e.activation(out_ap, in_ap, func, **kw),
                reads=[in_b] + list(extra_reads), writes=[out_b])

    def tt(self, eng, out_b, out_ap, a_b, a_ap, b_b, b_ap, op):
        self.op(eng, lambda e: e.tensor_tensor(out_ap, a_ap, b_ap, op),
                reads=[a_b, b_b], writes=[out_b])

    def ts(self, eng, out_b, out_ap, a_b, a_ap, s1, s2, op0, op1=None, extra_reads=()):
        if op1 is None:
            self.op(eng, lambda e: e.tensor_scalar(out_ap, a_ap, s1, None, op0),
                    reads=[a_b] + list(extra_reads), writes=[out_b])
        else:
            self.op(eng, lambda e: e.tensor_scalar(out_ap, a_ap, s1, s2, op0, op1),
                    reads=[a_b] + list(extra_reads), writes=[out_b])

    def stt(self, out_b, out_ap, a_b, a_ap, scalar, b_b, b_ap, op0, op1, extra_reads=()):
        self.op("dve", lambda e: e.scalar_tensor_tensor(out_ap, a_ap, scalar, b_ap, op0, op1),
                reads=[a_b, b_b] + list(extra_reads), writes=[out_b])

    def copy(self, eng, out_b, out_ap, in_b, in_ap):
        if eng == "act":
            self.op("act", lambda e: e.copy(out_ap, in_ap), reads=[in_b], writes=[out_b])
        else:
            self.op(eng, lambda e: e.tensor_copy(out_ap, in_ap), reads=[in_b], writes=[out_b])

    def memset(self, eng, out_b, out_ap, val):
        self.op(eng, lambda e: e.memset(out_ap, val), reads=[], writes=[out_b])


def run_prog(nc, in_maps, n_cores=8, trace=False):
    res = run_bass_kernel_spmd(nc, in_maps, core_ids=list(range(n_cores)), trace=trace)
    return res


D = 1024
IN_COLS = 8608
C_SCB, C_SCC, C_SCH = 0, 512, 1024
C_CFV, C_CFG = 1536, 2048
C_CQ, C_CKV, C_KR = 2560, 2816, 2944
C_SBQ, C_SBK, C_SBV = 2976, 3488, 4000
C_GATE = 4512
NA = 4512
MLA_SCALE = 96 ** -0.5
SB_SCALE = 64 ** -0.5
TWO_PI = 2.0 * np.pi
CW1 = 6.28125
CW2 = TWO_PI - 6.28125
MAGIC = 12582912.0
PI_CL = 3.141592
DN_ALPHA = 4 ** 0.25
LN_EPS = 1e-5
RMS_EPS = 1e-6


class Banks:
    def __init__(self, P, n, prefix="pb"):
        self.b = [P.ps("%s%d" % (prefix, i), [128, 512], F32) for i in range(n)]
        self.i = 0

    def next(self):
        b = self.b[self.i]
        self.i = (self.i + 1) % len(self.b)
        return b


def build_phase_a(T=2048, P=None, io=None):
    standalone = P is None
    if standalone:
        P = Prog()
    P.push_scope()

    def D_(name, shape, dt, kind):
        return io[name] if io is not None else P.dram(name, shape, dt, kind)
    NT = T // 512
    x = D_("x", [T, D], F32, "ExternalInput")
    posr = D_("posr", [32, T], I32, "ExternalInput")
    w_in = D_("w_in", [D, IN_COLS], F32, "ExternalInput")
    w_uq = D_("w_uq", [256, 768], F32, "ExternalInput")
    w_ukv = D_("w_ukv", [128, 1024], F32, "ExternalInput")
    qn = D_("qn", [128, 2], F32, "ExternalInput")
    kvn = D_("kvn", [128, 1], F32, "ExternalInput")
    ident_d = D_("ident", [128, 128], F32, "ExternalInput")
    invf_d = D_("invf", [128, 1], F32, "ExternalInput")

    xT_o = D_("xT_o", [D, T], BF16, "ExternalOutput")
    scb_o = D_("scb_o", [512, T], BF16, "ExternalOutput")
    scch_o = D_("scch_o", [512, T], BF16, "ExternalOutput")
    cfu_o = D_("cfu_o", [512, T], BF16, "ExternalOutput")
    mq_o = D_("mq_o", [8, 96, T], BF16, "ExternalOutput")
    mk_o = D_("mk_o", [8, 96, T], BF16, "ExternalOutput")
    mv_o = D_("mv_o", [T, 512], BF16, "ExternalOutput")
    sq_o = D_("sq_o", [512, T], BF16, "ExternalOutput")
    sk_o = D_("sk_o", [512, T], BF16, "ExternalOutput")
    sv_o = D_("sv_o", [T, 512], BF16, "ExternalOutput")

    identb = P.sb("identb", [128, 128], BF16)
    P.dma("pool", identb[:, :], ident_d[:, :], writes=[identb])
    invf = P.sb("invf_s", [128, 1], F32)
    P.dma("sp", invf[:, :], invf_d[:, :], writes=[invf])
    qn_s = P.sb("qn_s", [128, 2], F32)
    P.dma("sp", qn_s[:, :], qn[:, :], writes=[qn_s])
    kvn_s = P.sb("kvn_s", [128, 1], F32)
    P.dma("sp", kvn_s[:, :], kvn[:, :], writes=[kvn_s])
    onesb = P.sb("onesb", [128, 128], BF16)
    P.memset("dve", onesb, onesb[:, :], 1.0)
    epsr = P.sb("epsr", [128, 1], F32)
    P.memset("dve", epsr, epsr[:, :], RMS_EPS)

    wb = P.sb("wb", [128, 8, NA], BF16)
    for kc in range(8):
        P.dma("pool", wb[:, kc, :], w_in[kc * 128:(kc + 1) * 128, 0:NA], writes=[wb])
    wq = P.sb("wq", [128, 2, 768], BF16)
    for j in range(2):
        P.dma("pool", wq[:, j, :], w_uq[j * 128:(j + 1) * 128, :], writes=[wq])
    wkv = P.sb("wkv", [128, 1024], BF16)
    P.dma("pool", wkv[:, :], w_ukv[:, :], writes=[wkv])
    wq_rh = P.sb("wq_rh", [128, 2, 8, 96], BF16)
    P.memset("pool", wq_rh, wq_rh[:, :, :, :], 0.0)
    wq4 = wq.t[:, :, :].rearrange("p j (h c) -> p j h c", c=96)
    for j in range(2):
        P.ts("dve", wq_rh, wq_rh[:, j, :, 64:80], wq, wq4[:, j, :, 80:96], -1.0, None, ALU.mult)
        P.copy("dve", wq_rh, wq_rh[:, j, :, 80:96], wq, wq4[:, j, :, 64:80])
    wkr = P.sb("wkr", [128, 8, 96], BF16)
    wkr_rh = P.sb("wkr_rh", [128, 8, 96], BF16)
    P.memset("pool", wkr, wkr[:, :, :], 0.0)
    P.memset("pool", wkr_rh, wkr_rh[:, :, :], 0.0)
    P.copy("dve", wkr, wkr[:, :, 64:96], wb, wb[:, :, C_KR:C_KR + 32])
    P.ts("dve", wkr_rh, wkr_rh[:, :, 64:80], wb, wb[:, :, C_KR + 16:C_KR + 32], -1.0, None, ALU.mult)
    P.copy("dve", wkr_rh, wkr_rh[:, :, 80:96], wb, wb[:, :, C_KR:C_KR + 16])

    posi = P.sb("posi", [128, 512], I32)
    ang = P.sb("ang", [128, 512], F32)
    kk = P.sb("kk", [128, 512], F32)
    rr = P.sb("rr", [128, 512], F32)
    sin2 = P.sb("sin2", [128, 512], F32)
    cos2 = P.sb("cos2", [128, 512], F32)
    R = slice(64, 96)

    def rope_tables(t0):
        P.dma("sp", posi[64:96, :], posr[:, t0:t0 + 512], writes=[posi])
        P.copy("dve", ang, ang[R, :], posi, posi[R, :])
        P.ts("dve", ang, ang[R, :], ang, ang[R, :], invf[R, 0:1], None, ALU.mult, extra_reads=[invf])
        P.ts("dve", kk, kk[R, :], ang, ang[R, :], 1.0 / TWO_PI, MAGIC, ALU.mult, ALU.add)
        P.ts("dve", kk, kk[R, :], kk, kk[R, :], -MAGIC, None, ALU.add)
        P.stt(rr, rr[R, :], kk, kk[R, :], -CW1, ang, ang[R, :], ALU.mult, ALU.add)
        P.stt(rr, rr[R, :], kk, kk[R, :], -CW2, rr, rr[R, :], ALU.mult, ALU.add)
        P.ts("dve", rr, rr[R, :], rr, rr[R, :], PI_CL, -PI_CL, ALU.min, ALU.max)
        P.act(sin2, sin2[R, :], rr, rr[R, :], AF.Sin)
        P.ts("dve", kk, kk[R, :], rr, rr[R, :], np.pi / 2, None, ALU.is_gt)
        P.stt(ang, ang[R, :], kk, kk[R, :], -TWO_PI, rr, rr[R, :], ALU.mult, ALU.add)
        P.ts("dve", ang, ang[R, :], ang, ang[R, :], np.pi / 2, PI_CL, ALU.add, ALU.min)
        P.ts("dve", ang, ang[R, :], ang, ang[R, :], -PI_CL, None, ALU.max)
        P.act(cos2, cos2[R, :], ang, ang[R, :], AF.Sin)

    pT = [P.ps("pT%d" % i, [128, 1024], BF16) for i in range(2)]
    banks = Banks(P, 6)

    xs = [P.sb("xs%d" % i, [128, 4, D], F32) for i in range(1)] * 2
    xbf = [P.sb("xbf%d" % i, [128, 4, D], BF16) for i in range(1)] * 2
    xT = [P.sb("xT%d" % i, [128, 8, 512], BF16) for i in range(1)] * 2
    st_b = [P.sb("st_b%d" % i, [128, 4, 512], BF16) for i in range(1)] * 2
    st_ch = [P.sb("st_ch%d" % i, [128, 4, 512], BF16) for i in range(1)] * 2
    st_u = [P.sb("st_u%d" % i, [128, 4, 512], BF16) for i in range(1)] * 2
    st_sq = [P.sb("st_sq%d" % i, [128, 4, 512], BF16) for i in range(1)] * 2
    st_sk = [P.sb("st_sk%d" % i, [128, 4, 512], BF16) for i in range(1)] * 2
    st_sv = [P.sb("st_sv%d" % i, [128, 4, 512], BF16) for i in range(1)] * 2
    st_mv = [P.sb("st_mv%d" % i, [128, 4, 512], BF16) for i in range(1)] * 2
    st_mq = [P.sb("st_mq%d" % i, [96, 8, 512], BF16) for i in range(1)] * 2
    st_mk = [P.sb("st_mk%d" % i, [96, 8, 512], BF16) for i in range(1)] * 2
    tmp_c = [P.sb("tmp_c%d" % i, [128, 512], F32) for i in range(3)]
    cq2 = P.sb("cq2", [128, 2, 512], BF16)
    cqg = P.sb("cqg", [128, 2, 512], BF16)
    ckv2 = P.sb("ckv2", [128, 512], BF16)
    ckvg = P.sb("ckvg", [128, 512], BF16)
    rq = P.sb("rq", [128, 512], F32)
    rkv = P.sb("rkv", [128, 512], F32)
    rkv_tm = P.sb("rkv_tm", [128, 4], F32)
    rkv_t0 = P.sb("rkv_t0", [128, 4], F32)
    rtmp = P.sb("rtmp", [128, 512], F32)
    rt1 = [P.sb("rt1_%d" % i, [128, 512], F32) for i in range(2)]
    rt2 = [P.sb("rt2_%d" % i, [128, 512], F32) for i in range(2)]
    tci = [0]

    def fm_chunk(tt, col0, M=128, w_b=None, w_ap_fn=None):
        b = banks.next()
        for kc in range(8):
            if w_ap_fn is None:
                lw = wb[:, kc, col0:col0 + M]
                lb = wb
            else:
                lw = w_ap_fn(kc)
                lb = w_b
            P.mm(b, b[0:M, :], lb, lw, xT[tt % 2], xT[tt % 2][:, kc, :], kc == 0, kc == 7)
        return b

    for tt in range(NT):
        par = tt % 2
        t0 = tt * 512
        xsb, xbb, xTb = xs[par], xbf[par], xT[par]
        rope_tables(t0)
        P.dma("sp", xsb[:, :, :], x[t0:t0 + 512, :].rearrange("(s p) d -> p s d", p=128), writes=[xsb])
        for s in range(4):
            P.copy("pool" if s % 2 else "act", xbb, xbb[:, s, :], xsb, xsb[:, s, :])
        for kc in range(8):
            pt = pT[kc % 2]
            for s in range(4):
                P.tr(pt, pt[:, s * 128:(s + 1) * 128], xbb, xbb[:, s, kc * 128:(kc + 1) * 128], identb, identb[:, :])
            P.copy("dve" if kc % 2 else "act", xTb, xTb[:, kc, :], pt, pt[:, 0:512])
        P.dma("sp", xT_o[:, t0:t0 + 512].rearrange("(k p) t -> p k t", p=128), xTb[:, :, :], reads=[xTb], is_output=True)

        for j in range(2):
            b = fm_chunk(tt, C_CQ + j * 128)
            P.act(cq2, cq2[:, j, :], b, b[:, :], AF.Square)
            P.ts("dve", cqg, cqg[:, j, :], b, b[:, :], qn_s[:, j:j + 1], None, ALU.mult, extra_reads=[qn_s])
        b = banks.next()
        for j in range(2):
            P.mm(b, b[:, :], onesb, onesb[:, :], cq2, cq2[:, j, :], j == 0, j == 1)
        P.act(rtmp, rtmp[:, :], b, b[:, :], AF.Sqrt, bias=epsr[:, 0:1], scale=1.0 / 256, extra_reads=[epsr])
        P.op("dve", lambda e: e.reciprocal(rq[:, :], rtmp[:, :]), reads=[rtmp], writes=[rq])
        b = fm_chunk(tt, C_CKV)
        P.act(ckv2, ckv2[:, :], b, b[:, :], AF.Square)
        P.ts("dve", ckvg, ckvg[:, :], b, b[:, :], kvn_s[:, 0:1], None, ALU.mult, extra_reads=[kvn_s])
        b = banks.next()
        P.mm(b, b[:, :], onesb, onesb[:, :], ckv2, ckv2[:, :], True, True)
        P.act(rtmp, rtmp[:, :], b, b[:, :], AF.Sqrt, bias=epsr[:, 0:1], scale=1.0 / 128, extra_reads=[epsr])
        P.op("dve", lambda e: e.reciprocal(rkv[:, :], rtmp[:, :]), reads=[rtmp], writes=[rkv])
        b = banks.next()
        for s in range(4):
            P.mm(b, b[:, s * 128:(s + 1) * 128], ckv2, ckv2[:, s * 128:(s + 1) * 128], onesb, onesb[:, :], True, True)
        P.act(rkv_t0, rkv_t0[:, :], b, b[:, :].rearrange("p (s c) -> p s c", c=128)[:, :, 0], AF.Sqrt, bias=epsr[:, 0:1], scale=1.0 / 128, extra_reads=[epsr])
        P.op("dve", lambda e: e.reciprocal(rkv_tm[:, :], rkv_t0[:, :]), reads=[rkv_t0], writes=[rkv_tm])
        for j in range(4):
            b = fm_chunk(tt, C_SCB + j * 128)
            P.copy("act", st_b[par], st_b[par][:, j, :], b, b[:, :])
            bc = fm_chunk(tt, C_SCC + j * 128)
            tc = tmp_c[tci[0] % 3]; tci[0] += 1
            P.copy("act", tc, tc[:, :], bc, bc[:, :])
            bh = fm_chunk(tt, C_SCH + j * 128)
            P.tt("dve", st_ch[par], st_ch[par][:, j, :], bh, bh[:, :], tc, tc[:, :], ALU.mult)
        P.dma("sp", scb_o[:, t0:t0 + 512].rearrange("(j p) t -> p j t", p=128), st_b[par][:, :, :], reads=[st_b[par]], is_output=True)
        P.dma("sp", scch_o[:, t0:t0 + 512].rearrange("(j p) t -> p j t", p=128), st_ch[par][:, :, :], reads=[st_ch[par]], is_output=True)
        for j in range(4):
            bg = fm_chunk(tt, C_CFG + j * 128)
            tc = tmp_c[tci[0] % 3]; tci[0] += 1
            P.act(tc, tc[:, :], bg, bg[:, :], AF.Sigmoid)
            bv = fm_chunk(tt, C_CFV + j * 128)
            P.tt("dve", st_u[par], st_u[par][:, j, :], bv, bv[:, :], tc, tc[:, :], ALU.mult)
        P.dma("sp", cfu_o[:, t0:t0 + 512].rearrange("(j p) t -> p j t", p=128), st_u[par][:, :, :], reads=[st_u[par]], is_output=True)
        for j in range(4):
            b = fm_chunk(tt, C_SBQ + j * 128)
            P.act(st_sq[par], st_sq[par][:, j, :], b, b[:, :], AF.Copy, scale=SB_SCALE)
            b = fm_chunk(tt, C_SBK + j * 128)
            P.copy("dve", st_sk[par], st_sk[par][:, j, :], b, b[:, :])
        P.dma("sp", sq_o[:, t0:t0 + 512].rearrange("(j p) t -> p j t", p=128), st_sq[par][:, :, :], reads=[st_sq[par]], is_output=True)
        P.dma("sp", sk_o[:, t0:t0 + 512].rearrange("(j p) t -> p j t", p=128), st_sk[par][:, :, :], reads=[st_sk[par]], is_output=True)
        for s in range(4):
            b = banks.next()
            for kc in range(8):
                P.mm(b, b[:, :], xTb, xTb[:, kc, s * 128:(s + 1) * 128], wb, wb[:, kc, C_SBV:C_SBV + 512], kc == 0, kc == 7)
            P.copy("act" if s % 2 else "dve", st_sv[par], st_sv[par][:, s, :], b, b[:, :])
        P.dma("sp", sv_o[t0:t0 + 512, :].rearrange("(s p) c -> p s c", p=128), st_sv[par][:, :, :], reads=[st_sv[par]], is_output=True)

        for h in range(8):
            bq = banks.next()
            for j in range(2):
                P.mm(bq, bq[0:96, :], wq, wq[:, j, h * 96:(h + 1) * 96], cqg, cqg[:, j, :], j == 0, j == 1)
            br = banks.next()
            for j in range(2):
                P.mm(br, br[0:96, :], wq_rh, wq_rh[:, j, h, :], cqg, cqg[:, j, :], j == 0, j == 1)
            P.stt(st_mq[par], st_mq[par][0:64, h, :], bq, bq[0:64, :], MLA_SCALE, rq, rq[0:64, :], ALU.mult, ALU.mult)
            a1, a2 = rt1[h % 2], rt2[h % 2]
            P.tt("dve", a1, a1[R, :], bq, bq[R, :], cos2, cos2[R, :], ALU.mult)
            P.tt("dve", a2, a2[R, :], br, br[R, :], sin2, sin2[R, :], ALU.mult)
            P.tt("pool", a1, a1[R, :], a1, a1[R, :], a2, a2[R, :], ALU.add)
            P.stt(st_mq[par], st_mq[par][R, h, :], a1, a1[R, :], MLA_SCALE, rq, rq[R, :], ALU.mult, ALU.mult)
        P.dma("sp", mq_o[:, :, t0:t0 + 512].rearrange("h p t -> p h t"), st_mq[par][:, :, :], reads=[st_mq[par]], is_output=True)
        bk = fm_chunk(tt, 0, M=96, w_b=wkr, w_ap_fn=lambda kc: wkr[:, kc, :])
        bkr = fm_chunk(tt, 0, M=96, w_b=wkr_rh, w_ap_fn=lambda kc: wkr_rh[:, kc, :])
        a1, a2 = rt1[0], rt2[0]
        P.tt("dve", a1, a1[R, :], bk, bk[R, :], cos2, cos2[R, :], ALU.mult)
        P.tt("dve", a2, a2[R, :], bkr, bkr[R, :], sin2, sin2[R, :], ALU.mult)
        for h in range(8):
            P.tt("pool" if h % 2 else "dve", st_mk[par], st_mk[par][R, h, :], a1, a1[R, :], a2, a2[R, :], ALU.add)
        for h in range(8):
            b = banks.next()
            P.mm(b, b[0:64, :], wkv, wkv[:, h * 128:h * 128 + 64], ckvg, ckvg[:, :], True, True)
            P.tt("dve", st_mk[par], st_mk[par][0:64, h, :], b, b[0:64, :], rkv, rkv[0:64, :], ALU.mult)
        P.dma("sp", mk_o[:, :, t0:t0 + 512].rearrange("h p t -> p h t"), st_mk[par][:, :, :], reads=[st_mk[par]], is_output=True)
        wkv_v = wkv.t[:, :].rearrange("p (h c) -> p h c", c=128)[:, :, 64:128]
        for s in range(4):
            b = banks.next()
            P.mm(b, b[:, :], ckvg, ckvg[:, s * 128:(s + 1) * 128], wkv, wkv_v, True, True)
            P.ts("dve", st_mv[par], st_mv[par][:, s, :], b, b[:, :], rkv_tm[:, s:s + 1], None, ALU.mult, extra_reads=[rkv_tm])
        P.dma("sp", mv_o[t0:t0 + 512, :].rearrange("(s p) c -> p s c", p=128), st_mv[par][:, :, :], reads=[st_mv[par]], is_output=True)
    P.pop_scope()
    if standalone:
        return P.build()
    return None


def run_pipeline(items, stages):
    n = len(items)
    d = len(stages)
    for step in range(n + d - 1):
        for si, st in enumerate(stages):
            k = step - si
            if 0 <= k < n:
                st(k, items[k])


def build_phase_b(S=4096, NH=4, CT=2048, do_conv=True, do_mla=True, do_sb=True, P=None, io=None):
    standalone = P is None
    if standalone:
        P = Prog()
    P.push_scope()

    def D_(name, shape, dt, kind):
        return io[name] if io is not None else P.dram(name, shape, dt, kind)
    HW = NH * 64
    NB = S // 128
    NQC = S // 512
    scb = D_("scb", [512, CT], BF16, "ExternalInput")
    scch = D_("scch", [512, CT + 2], BF16, "ExternalInput")
    cfu = D_("cfu", [512, CT + 30], BF16, "ExternalInput")
    sc_w = D_("sc_w", [128, 4, 3], F32, "ExternalInput")
    cf_w = D_("cf_w", [128, 4, 31], F32, "ExternalInput")
    cf_p = D_("cf_p", [128, 3, 4], F32, "ExternalInput")
    mq = D_("mq", [NH, 96, S], BF16, "ExternalInput")
    mk = D_("mk", [NH, 96, S], BF16, "ExternalInput")
    mv = D_("mv", [S, HW], BF16, "ExternalInput")
    sq = D_("sq", [HW, S], BF16, "ExternalInput")
    sk = D_("sk", [HW, S], BF16, "ExternalInput")
    sv = D_("sv", [S, HW], BF16, "ExternalInput")
    cst = D_("cst", [128, 4, 128], F32, "ExternalInput")
    br0 = D_("br0", [512, CT], BF16, "ExternalOutput")
    br1 = D_("br1", [512, CT], BF16, "ExternalOutput")
    mo = D_("mo", [HW, S], BF16, "ExternalOutput")
    so = D_("so", [HW, S], BF16, "ExternalOutput")

    pb = [P.ps("pb%d" % i, [128, 512], F32) for i in range(8)]
    cstf = P.sb("cstf", [128, 4, 128], F32)
    P.dma("sp", cstf[:, :, :], cst[:, :, :], writes=[cstf])
    cstb = P.sb("cstb", [128, 4, 128], BF16)
    P.copy("dve", cstb, cstb[:, :, :], cstf, cstf[:, :, :])
    onesb = P.sb("onesb", [128, 128], BF16)
    P.memset("dve", onesb, onesb[:, :], 1.0)
    onesf = P.sb("onesf", [128, 128], F32)
    P.memset("dve", onesf, onesf[:, :], 1.0)
    avgf = P.sb("avgf", [128, 128], F32)
    P.memset("dve", avgf, avgf[:, :], 1.0 / 512)
    epsl = P.sb("epsl", [128, 1], F32)
    P.memset("dve", epsl, epsl[:, :], LN_EPS)
    mask_sb16 = cstb[:, 1, :]
    negtri16 = cstb[:, 3, :]

    if do_conv:
        scw = P.sb("scw", [128, 4, 3], F32)
        P.dma("sp", scw[:, :, :], sc_w[:, :, :], writes=[scw])
        cfw = P.sb("cfw", [128, 4, 31], F32)
        P.dma("sp", cfw[:, :, :], cf_w[:, :, :], writes=[cfw])
        cfp = P.sb("cfp", [128, 3, 4], F32)
        P.dma("sp", cfp[:, :, :], cf_p[:, :, :], writes=[cfp])
        diag = P.sb("diag", [128, 4, 31, 128], BF16)
        dsubs = []
        for j in range(4):
            for l in range(31):
                dsub = diag.sub()
                dsubs.append(dsub)
                P.ts("dve" if (l % 4) else "pool", dsub, diag[:, j, l, :], cstf, cstf[:, 0, :], cfw[:, j, l:l + 1], None,
                     ALU.mult, extra_reads=[cfw])
        djoin = P.sb("djoin", [128, 1], F32)
        P.op("dve", lambda e: e.memset(djoin[:, :], 0.0), reads=dsubs, writes=[djoin, diag])
        chs = P.sb("chs", [128, 4, 514], BF16)
        bs = P.sb("bs", [128, 4, 512], BF16)
        us = P.sb("us", [128, 4, 542], BF16)
        acc = [P.sb("acc%d" % i, [128, 512], F32) for i in range(2)]
        o0 = P.sb("o0", [128, 4, 512], BF16)
        o1 = P.sb("o1", [128, 4, 512], BF16)
        vt = P.sb("vt", [128, 4, 512], F32)
        v2 = P.sb("v2", [128, 4, 512], F32)
        mean_s = P.sb("mean_s", [128, 512], F32)
        m2 = P.sb("m2", [128, 512], F32)
        var_s = P.sb("var_s", [128, 512], F32)
        rstd = P.sb("rstd", [128, 512], F32)
        xn = [P.sb("xn%d" % i, [128, 512], F32) for i in range(2)]
        for tt in range(CT // 512):
            t0 = tt * 512
            P.dma("sp", chs[:, :, :], scch[:, t0:t0 + 514].rearrange("(j p) t -> p j t", p=128), writes=[chs])
            P.dma("sp", bs[:, :, :], scb[:, t0:t0 + 512].rearrange("(j p) t -> p j t", p=128), writes=[bs])
            P.dma("act", us[:, :, :], cfu[:, t0:t0 + 542].rearrange("(j p) t -> p j t", p=128), writes=[us])
            for j in range(4):
                a = acc[j % 2]
                P.ts("dve", a, a[:, :], chs, chs[:, j, 0:512], scw[:, j, 0:1], None, ALU.mult, extra_reads=[scw])
                P.stt(a, a[:, :], chs, chs[:, j, 1:513], scw[:, j, 1:2], a, a[:, :], ALU.mult, ALU.add, extra_reads=[scw])
                P.stt(a, a[:, :], chs, chs[:, j, 2:514], scw[:, j, 2:3], a, a[:, :], ALU.mult, ALU.add, extra_reads=[scw])
                P.tt("pool", o0, o0[:, j, :], a, a[:, :], bs, bs[:, j, :], ALU.mult)
            P.dma("sp", br0[:, t0:t0 + 512].rearrange("(j p) t -> p j t", p=128), o0[:, :, :], reads=[o0], is_output=True)
            for j in range(4):
                b = pb[j % 3]
                for l in range(31):
                    P.mm(b, b[:, :], diag, diag[:, j, l, :], us, us[:, j, l:l + 512], l == 0, l == 30)
                P.act(vt, vt[:, j, :], b, b[:, :], AF.Identity, bias=cfp[:, 0, j:j + 1], extra_reads=[cfp])
                P.act(v2, v2[:, j, :], vt, vt[:, j, :], AF.Square)
            bm, bq = pb[3], pb[4]
            for j in range(4):
                P.mm(bm, bm[:, :], avgf, avgf[:, :], vt, vt[:, j, :], j == 0, j == 3)
            for j in range(4):
                P.mm(bq, bq[:, :], avgf, avgf[:, :], v2, v2[:, j, :], j == 0, j == 3)
            P.copy("act", mean_s, mean_s[:, :], bm, bm[:, :])
            P.tt("pool", m2, m2[:, :], mean_s, mean_s[:, :], mean_s, mean_s[:, :], ALU.mult)
            P.tt("dve", var_s, var_s[:, :], bq, bq[:, :], m2, m2[:, :], ALU.subtract)
            P.act(var_s, var_s[:, :], var_s, var_s[:, :], AF.Sqrt, bias=epsl[:, 0:1], extra_reads=[epsl])
            P.op("dve", lambda e: e.reciprocal(rstd[:, :], var_s[:, :]), reads=[var_s], writes=[rstd])
            for j in range(4):
                xx = xn[j % 2]
                P.tt("pool", xx, xx[:, :], vt, vt[:, j, :], mean_s, mean_s[:, :], ALU.subtract)
                P.tt("dve", xx, xx[:, :], xx, xx[:, :], rstd, rstd[:, :], ALU.mult)
                P.act(o1, o1[:, j, :], xx, xx[:, :], AF.Silu, bias=cfp[:, 2, j:j + 1], scale=cfp[:, 1, j:j + 1], extra_reads=[cfp])
            P.dma("sp", br1[:, t0:t0 + 512].rearrange("(j p) t -> p j t", p=128), o1[:, :, :], reads=[o1], is_output=True)

    if do_mla:
        kT = [P.sb("kT%d" % i, [96, S], BF16) for i in range(1)] * 2
        qT = [P.sb("qT%d" % i, [96, S], BF16) for i in range(1)] * 2
        vA = [P.sb("vA%d" % i, [128, NB, 65], BF16) for i in range(2)]
        pT = [P.sb("pTm%d" % i, [128, 512], BF16) for i in range(3)]
        dsb = P.sb("dsb", [128, 512], F32)
        rsb = P.sb("rsb", [128, 512], F32)
        bcs = P.sb("bcs", [64, 512], F32)
        ost = [P.sb("ost%d" % i, [64, 512], BF16) for i in range(2)]
        items = []
        for h in range(NH):
            for qc in range(NQC):
                nk = 4 * (qc + 1)
                for kb in range(nk):
                    items.append((h, qc, kb, nk))
        loaded = set()

        def load_head(h):
            if h in loaded or h >= NH:
                return
            loaded.add(h)
            P.dma("sp", kT[h % 2][:, :], mk[h, :, :], writes=[kT[h % 2]])
            P.dma("act", qT[h % 2][:, :], mq[h, :, :], writes=[qT[h % 2]])
            P.memset("pool", vA[h % 2], vA[h % 2][:, :, :], 1.0)
            P.dma("sp", vA[h % 2][:, :, 0:64], mv[:, h * 64:(h + 1) * 64].rearrange("(b p) c -> p b c", p=128), writes=[vA[h % 2]])

        def gidx(h, qc):
            return h * NQC + qc

        def m_s1(k, it):
            h, qc, kb, nk = it
            load_head(h)
            c0 = max(0, kb - 4 * qc) * 128
            Sb = pb[k % 3]
            P.mm(Sb, Sb[:, c0:512], kT[h % 2], kT[h % 2][:, kb * 128:(kb + 1) * 128], qT[h % 2], qT[h % 2][:, qc * 512 + c0:qc * 512 + 512], True, True)
            pt = pT[k % 3]
            P.act(pt, pt[:, c0:512], Sb, Sb[:, c0:512], AF.Exp)
            if kb >= 4 * qc:
                P.memset("pool", pt, pt[64:128, c0:c0 + 64], 0.0)

        def m_s2(k, it):
            h, qc, kb, nk = it
            c0 = max(0, kb - 4 * qc) * 128
            O = pb[3 + gidx(h, qc) % 2]
            pt = pT[k % 3]
            P.mm(O, O[0:65, c0:512], vA[h % 2], vA[h % 2][:, kb, :], pt, pt[:, c0:512], kb == 0, True, sgc=(kb > 0))
            if kb == nk - 1:
                g = gidx(h, qc)
                P.copy("act", dsb, dsb[64:65, :], O, O[64:65, :])
                P.op("dve", lambda e: e.reciprocal(rsb[64:65, :], dsb[64:65, :]), reads=[dsb], writes=[rsb])
                bc = pb[5]
                P.mm(bc, bc[0:64, :], onesf, onesf[64:65, 0:64], rsb, rsb[64:65, :], True, True)
                P.copy("act", bcs, bcs[:, :], bc, bc[0:64, :])
                os_ = ost[g % 2]
                P.tt("dve", os_, os_[:, :], O, O[0:64, :], bcs, bcs[:, :], ALU.mult)
                P.dma("sp", mo[h * 64:(h + 1) * 64, qc * 512:(qc + 1) * 512], os_[:, :], reads=[os_], is_output=True)

        run_pipeline(items, [m_s1, m_s2])

    if do_sb:
        GH = min(NH, 4)
        NP_ = GH // 2
        skT = [P.sb("skT%d" % i, [128, S], BF16) for i in range(NP_)]
        sqT = [P.sb("sqT%d" % i, [128, S], BF16) for i in range(NP_)]
        svt = [P.sb("svt%d" % i, [128, NB, 64], BF16) for i in range(GH)]
        e_t = [P.sb("e_t%d" % i, [128, 512], F32) for i in range(3)]
        lp16 = [P.sb("lp16_%d" % i, [128, 512], BF16) for i in range(3)]
        t_t = [P.sb("t_t%d" % i, [128, 512], F32) for i in range(3)]
        a16 = [P.sb("a16_%d" % i, [128, 512], BF16) for i in range(3)]
        cacc = [P.sb("cacc%d" % i, [128, 512], F32) for i in range(2)]
        sst = [P.sb("sst%d" % i, [64, 512], BF16) for i in range(2)]
        hb_ = [0]

        def gidx2(h, qc):
            return h * NQC + qc

        def s_s1(k, it):
            h, qc, kb, nk = it
            c0 = max(0, kb - 4 * qc) * 128
            g = gidx2(h, qc)
            if kb == nk - 1:
                P.memset("pool", cacc[g % 2], cacc[g % 2][:, :], 0.0)
            Zb = pb[k % 3]
            hp = slice((h % 2) * 64, (h % 2) * 64 + 64)
            P.mm(Zb, Zb[:, c0:512], skT[h // 2], skT[h // 2][hp, kb * 128:(kb + 1) * 128], sqT[h // 2], sqT[h // 2][hp, qc * 512 + c0:qc * 512 + 512], True, True)
            P.act(e_t[k % 3], e_t[k % 3][:, c0:512], Zb, Zb[:, c0:512], AF.Exp)
            P.act(lp16[k % 3], lp16[k % 3][:, c0:512], e_t[k % 3], e_t[k % 3][:, c0:512], AF.Ln, bias=1.0)
            if kb >= 4 * qc:
                P.tt("pool", lp16[k % 3], lp16[k % 3][:, c0:c0 + 128], lp16[k % 3], lp16[k % 3][:, c0:c0 + 128], cstb, mask_sb16, ALU.mult)

        def s_s2(k, it):
            h, qc, kb, nk = it
            c0 = max(0, kb - 4 * qc) * 128
            g = gidx2(h, qc)
            Zb = pb[k % 3]
            Cb = pb[5 + k % 2]
            L = lp16[k % 3]
            P.mm(Zb, Zb[:, c0:512], cstb, negtri16, L, L[:, c0:512], False, True, sgc=True)
            P.mm(Cb, Cb[:, c0:512], onesb, onesb[:, :], L, L[:, c0:512], True, True)
            ca = cacc[g % 2]
            P.tt("dve", t_t[k % 3], t_t[k % 3][:, c0:512], Zb, Zb[:, c0:512], ca, ca[:, c0:512], ALU.subtract)
            P.act(a16[k % 3], a16[k % 3][:, c0:512], t_t[k % 3], t_t[k % 3][:, c0:512], AF.Exp)
            if kb >= 4 * qc:
                P.tt("pool", a16[k % 3], a16[k % 3][:, c0:c0 + 128], a16[k % 3], a16[k % 3][:, c0:c0 + 128], cstb, mask_sb16, ALU.mult)
            if kb > 0:
                P.tt("dve", ca, ca[:, c0:512], Cb, Cb[:, c0:512], ca, ca[:, c0:512], ALU.add)

        def s_s3(k, it):
            h, qc, kb, nk = it
            c0 = max(0, kb - 4 * qc) * 128
            g = gidx2(h, qc)
            O = pb[3 + g % 2]
            P.mm(O, O[0:64, c0:512], svt[h], svt[h][:, kb, :], a16[k % 3], a16[k % 3][:, c0:512], kb == nk - 1, True, sgc=(kb < nk - 1))
            if kb == 0:
                os_ = sst[g % 2]
                P.copy("act", os_, os_[:, :], O, O[0:64, :])
                hg_ = hb_[0] + h
                P.dma("sp", so[hg_ * 64:(hg_ + 1) * 64, qc * 512:(qc + 1) * 512], os_[:, :], reads=[os_], is_output=True)

        for hg in range(NH // GH):
            hb_[0] = hg * GH
            for pr in range(NP_):
                r0 = (hg * NP_ + pr) * 128
                P.dma("sp", skT[pr][:, :], sk[r0:r0 + 128, :], writes=[skT[pr]])
                P.dma("act", sqT[pr][:, :], sq[r0:r0 + 128, :], writes=[sqT[pr]])
            for h in range(GH):
                hg_ = hg * GH + h
                P.dma("sp", svt[h][:, :, :], sv[:, hg_ * 64:(hg_ + 1) * 64].rearrange("(b p) c -> p b c", p=128), writes=[svt[h]])
            items = []
            for h in range(GH):
                for qc in range(NQC):
                    nk = 4 * (qc + 1)
                    for kb in range(nk - 1, -1, -1):
                        items.append((h, qc, kb, nk))
            run_pipeline(items, [s_s1, s_s2, s_s3])
    P.pop_scope()
    if standalone:
        return P.build()
    return None


def build_phase_c(T=2048, E=1, P=None, io=None):
    moe = E > 1
    standalone = P is None
    if standalone:
        P = Prog()
    P.push_scope()

    def D_(name, shape, dt, kind):
        return io[name] if io is not None else P.dram(name, shape, dt, kind)
    NT = T // 512
    NS = T // 128
    x = D_("x", [T, D], F32, "ExternalInput")
    xT_d = D_("xT", [D, T], BF16, "ExternalInput")
    brs = D_("brs", [4, 512, T], BF16, "ExternalInput")
    w_in = D_("w_in", [D, IN_COLS], F32, "ExternalInput")
    bgc_d = D_("bgc", [128, 32], F32, "ExternalInput")
    w_br = D_("w_br", [4, 512, D], F32, "ExternalInput")
    w_o = D_("w_o", [D, D], F32, "ExternalInput")
    lnp = D_("lnp", [4, D], F32, "ExternalInput")
    f_g = D_("f_g", [E, D, 3584], F32, "ExternalInput")
    f_u = D_("f_u", [E, D, 3584], F32, "ExternalInput")
    f_d = D_("f_d", [E, 3584, D], F32, "ExternalInput")
    wr_d = D_("wr", [128, 8, 8], F32, "ExternalInput")
    ident_d = D_("ident", [128, 128], F32, "ExternalInput")
    xo = D_("xo", [T, D], F32, "ExternalOutput")

    pb = Banks(P, 7)
    pbl = P.ps("pbl", [128, 512], F32)
    identf = P.sb("identf", [128, 128], F32)
    P.dma("sp", identf[:, :], ident_d[:, :], writes=[identf])
    bgc = P.sb("bgc_s", [128, 32], F32)
    P.dma("sp", bgc[:, :], bgc_d[:, :], writes=[bgc])
    epsl = P.sb("epsl", [128, 1], F32)
    P.memset("dve", epsl, epsl[:, :], LN_EPS)
    lg = P.sb("lg", [128, D], F32)
    lb = P.sb("lb", [128, D], F32)
    wr_s = P.sb("wr_s", [128, 8, 8], F32)
    P.dma("sp", wr_s[:, :, :], wr_d[:, :, :], writes=[wr_s])
    comb = P.sb("comb", [128, NS, 8], F32)

    acc = P.sb("acc", [128, NS, D], F32)
    x1T = P.sb("x1T", [128, 8, T], BF16)
    wbig = P.sb("wbig", [128, 24576], BF16)
    wbr = wbig.t[:, 0:16384].rearrange("p (n k c) -> p n k c", n=4, k=4)
    wo = wbig.t[:, 16384:24576].rearrange("p (k c) -> p k c", k=8)
    for n in range(4):
        for kc in range(4):
            P.dma("pool", wbr[:, n, kc, :], w_br[n, kc * 128:(kc + 1) * 128, :], writes=[wbig])
    for kc in range(8):
        P.dma("pool", wo[:, kc, :], w_o[kc * 128:(kc + 1) * 128, :], writes=[wbig])

    def load_ln(i):
        P.dma("sp", lg[:, :], lnp[i:i + 1, :].partition_broadcast(128), writes=[lg])
        P.dma("sp", lb[:, :], lnp[i + 1:i + 2, :].partition_broadcast(128), writes=[lb])

    stats = P.sb("stats", [128, 2, 6], F32)
    mv_ = P.sb("mv_", [128, 2], F32)
    rs_ = P.sb("rs_", [128, 1], F32)
    rs2 = P.sb("rs2", [128, 1], F32)

    def layer_norm(zb, z_ap, ob, o_ap):
        for hh in range(2):
            P.op("dve", lambda e, hh=hh: e.bn_stats(stats[:, hh, :], z_ap[:, hh * 512:(hh + 1) * 512]), reads=[zb], writes=[stats])
        P.op("dve", lambda e: e.bn_aggr(mv_[:, :], stats[:, :, :].rearrange("p a b -> p (a b)")), reads=[stats], writes=[mv_])
        P.act(rs_, rs_[:, :], mv_, mv_[:, 1:2], AF.Sqrt, bias=epsl[:, 0:1], extra_reads=[epsl])
        P.op("dve", lambda e: e.reciprocal(rs2[:, :], rs_[:, :]), reads=[rs_], writes=[rs2])
        P.ts("dve", ob, o_ap, zb, z_ap, mv_[:, 0:1], rs2[:, 0:1], ALU.subtract, ALU.mult, extra_reads=[mv_, rs2])
        P.tt("pool", ob, o_ap, ob, o_ap, lg, lg[:, :], ALU.mult)
        P.tt("pool", ob, o_ap, ob, o_ap, lb, lb[:, :], ALU.add)

    load_ln(0)
    gw = [P.sb("gw%d" % i, [128, 4, 8, 128], BF16) for i in range(1)] * 2
    W = 256
    NSW = W // 128
    xT_t = P.sb("xT_t", [128, 8, W], BF16)
    br_t = P.sb("br_t", [128, 4, 4, W], BF16)
    g_t = [P.sb("g_t%d" % i, [128, W], F32) for i in range(2)]
    tmp_t = [P.sb("tmp_t%d" % i, [128, W], F32) for i in range(2)]
    mrg = P.sb("mrg", [128, W], F32)
    mrgT = P.sb("mrgT", [128, 8, W], BF16)
    xs_ = [P.sb("xs_%d" % i, [128, D], F32) for i in range(1)] * 2
    z_t = P.sb("z_t", [128, D], F32)
    x1_t = P.sb("x1_t", [128, D], F32)
    x1T32 = [P.sb("x1T32_%d" % i, [128, 128], F32) for i in range(2)]
    lgt = P.sb("lgt", [128, 8], F32)
    sm = [P.sb("sm%d" % i, [128, 8], F32) for i in range(4)]
    sc1 = [P.sb("sc1_%d" % i, [128, 1], F32) for i in range(6)]
    gcount = 0
    for tt in range(T // W):
        t0 = tt * W
        P.dma("sp", xT_t[:, :, :], xT_d[:, t0:t0 + W].rearrange("(k p) t -> p k t", p=128), writes=[xT_t])
        for n in range(4):
            P.dma("act", br_t[:, n, :, :], brs[n, :, t0:t0 + W].rearrange("(k p) t -> p k t", p=128), writes=[br_t])
        for m in range(8):
            gwt = gw[gcount % 2]
            gcount += 1
            for n in range(4):
                c0 = C_GATE + n * 1024 + m * 128
                P.dma("pool", gwt[:, n, :, :], w_in[:, c0:c0 + 128].rearrange("(k p) c -> p k c", p=128), writes=[gwt])
            for n in range(4):
                bG = pb.next()
                for kc in range(8):
                    P.mm(bG, bG[:, 0:W], gwt, gwt[:, n, kc, :], xT_t, xT_t[:, kc, :], kc == 0, kc == 7)
                gt = g_t[n % 2]
                P.act(gt, gt[:, :], bG, bG[:, 0:W], AF.Sigmoid, bias=bgc[:, n * 8 + m:n * 8 + m + 1], extra_reads=[bgc])
                bP = pb.next()
                for kc in range(4):
                    P.mm(bP, bP[:, 0:W], wbig, wbr[:, n, kc, m * 128:(m + 1) * 128], br_t, br_t[:, n, kc, :], kc == 0, kc == 3)
                if n == 0:
                    P.tt("dve", mrg, mrg[:, :], bP, bP[:, 0:W], gt, gt[:, :], ALU.mult)
                else:
                    tp = tmp_t[n % 2]
                    P.tt("dve", tp, tp[:, :], bP, bP[:, 0:W], gt, gt[:, :], ALU.mult)
                    P.tt("pool", mrg, mrg[:, :], mrg, mrg[:, :], tp, tp[:, :], ALU.add)
            P.copy("act", mrgT, mrgT[:, m, :], mrg, mrg[:, :])
        for s in range(NSW):
            si = tt * NSW + s
            xs = xs_[si % 2]
            P.dma("sp", xs[:, :], x[t0 + s * 128:t0 + (s + 1) * 128, :], writes=[xs])
            for hh in range(2):
                b = pb.next()
                for kc in range(8):
                    P.mm(b, b[:, :], mrgT, mrgT[:, kc, s * 128:(s + 1) * 128], wbig, wo[:, kc, hh * 512:(hh + 1) * 512], kc == 0, kc == 7)
                P.stt(z_t, z_t[:, hh * 512:(hh + 1) * 512], xs, xs[:, hh * 512:(hh + 1) * 512], DN_ALPHA, b, b[:, :], ALU.mult, ALU.add)
            layer_norm(z_t, z_t[:, :], x1_t, x1_t[:, :])
            P.op("act", lambda e, si=si: e.mul(acc[:, si, :], x1_t[:, :], DN_ALPHA), reads=[x1_t], writes=[acc])
            bl = pbl
            for kc in range(8):
                b = pb.next()
                P.tr(b, b[:, 0:128], x1_t, x1_t[:, kc * 128:(kc + 1) * 128], identf, identf[:, :])
                P.copy("act", x1T, x1T[:, kc, si * 128:(si + 1) * 128], b, b[:, 0:128])
                if moe:
                    xt32 = x1T32[kc % 2]
                    P.copy("dve", xt32, xt32[:, :], b, b[:, 0:128])
                    P.mm(bl, bl[:, 0:8], xt32, xt32[:, :], wr_s, wr_s[:, kc, :], kc == 0, kc == 7)
            if moe:
                P.copy("dve", lgt, lgt[:, :], bl, bl[:, 0:8])
                m1, m2, dd, ee, w1, w2 = sc1
                eq1, l2, eq2, tq = sm
                P.op("dve", lambda e: e.reduce_max(m1[:, :], lgt[:, :], AX.X), reads=[lgt], writes=[m1])
                P.ts("dve", eq1, eq1[:, :], lgt, lgt[:, :], m1[:, 0:1], None, ALU.is_equal, extra_reads=[m1])
                P.stt(l2, l2[:, :], eq1, eq1[:, :], -1e30, lgt, lgt[:, :], ALU.mult, ALU.add)
                P.op("dve", lambda e: e.reduce_max(m2[:, :], l2[:, :], AX.X), reads=[l2], writes=[m2])
                P.ts("dve", eq2, eq2[:, :], l2, l2[:, :], m2[:, 0:1], None, ALU.is_equal, extra_reads=[m2])
                P.tt("dve", dd, dd[:, :], m2, m2[:, :], m1, m1[:, :], ALU.subtract)
                P.act(ee, ee[:, :], dd, dd[:, :], AF.Exp)
                P.ts("dve", dd, dd[:, :], ee, ee[:, :], 1.0, None, ALU.add)
                P.op("dve", lambda e: e.reciprocal(w1[:, :], dd[:, :]), reads=[dd], writes=[w1])
                P.tt("dve", w2, w2[:, :], ee, ee[:, :], w1, w1[:, :], ALU.mult)
                P.ts("dve", tq, tq[:, :], eq1, eq1[:, :], w1[:, 0:1], None, ALU.mult, extra_reads=[w1])
                P.stt(comb, comb[:, si, :], eq2, eq2[:, :], w2[:, 0:1], tq, tq[:, :], ALU.mult, ALU.add, extra_reads=[w2])

    NU = 7
    wg_t = [Buf(wbig.t, "wg_t%d" % i) for i in range(2)]
    wu_t = [Buf(wbig.t, "wu_t%d" % i) for i in range(2)]
    wd_t = [Buf(wbig.t, "wd_t%d" % i) for i in range(2)]

    def wv(i, which):
        base = i * 12288 + which * 4096
        if which < 2:
            return wbig.t[:, base:base + 4096].rearrange("p (k c) -> p k c", k=8)
        return wbig.t[:, base:base + 4096].rearrange("p (k c) -> p k c", k=4)

    sg_t = [P.sb("sg_t%d" % i, [128, 512], F32) for i in range(2)]
    hT = [P.sb("hT%d" % i, [128, 4, 512], BF16) for i in range(1)] * 2
    first = [True, True]
    u = 0
    for e_ in range(E):
        for fq in range(NU):
            i = u % 2
            u += 1
            extra = [wbig] if first[i] else []
            first[i] = False
            f0 = fq * 512
            P.dma("pool", wv(i, 0), f_g[e_, :, f0:f0 + 512].rearrange("(k p) c -> p k c", p=128), writes=[wg_t[i]] + extra)
            P.dma("pool", wv(i, 1), f_u[e_, :, f0:f0 + 512].rearrange("(k p) c -> p k c", p=128), writes=[wu_t[i]] + extra)
            P.dma("pool", wv(i, 2), f_d[e_, f0:f0 + 512, :].rearrange("(k p) c -> p k c", p=128), writes=[wd_t[i]] + extra)
            for tt in range(NT):
                t0 = tt * 512
                ht = hT[tt % 2]
                for fc in range(4):
                    bg = pb.next()
                    for kc in range(8):
                        P.mm(bg, bg[:, :], wg_t[i], wv(i, 0)[:, kc, fc * 128:(fc + 1) * 128], x1T, x1T[:, kc, t0:t0 + 512], kc == 0, kc == 7)
                    bu = pb.next()
                    for kc in range(8):
                        P.mm(bu, bu[:, :], wu_t[i], wv(i, 1)[:, kc, fc * 128:(fc + 1) * 128], x1T, x1T[:, kc, t0:t0 + 512], kc == 0, kc == 7)
                    sg = sg_t[fc % 2]
                    P.act(sg, sg[:, :], bg, bg[:, :], AF.Silu)
                    P.tt("dve", ht, ht[:, fc, :], bu, bu[:, :], sg, sg[:, :], ALU.mult)
                for s in range(4):
                    si = tt * 4 + s
                    for hh in range(2):
                        bd = pb.next()
                        for fc in range(4):
                            P.mm(bd, bd[:, :], ht, ht[:, fc, s * 128:(s + 1) * 128], wd_t[i], wv(i, 2)[:, fc, hh * 512:(hh + 1) * 512], fc == 0, fc == 3)
                        sc = comb[:, si, e_:e_ + 1] if moe else 1.0
                        P.stt(acc, acc[:, si, hh * 512:(hh + 1) * 512], bd, bd[:, :], sc, acc, acc[:, si, hh * 512:(hh + 1) * 512],
                              ALU.mult, ALU.add, extra_reads=[comb] if moe else [])
    load_ln(2)
    ot = [z_t, x1_t]
    for si in range(NS):
        o = ot[si % 2]
        layer_norm(acc, acc[:, si, :], o, o[:, :])
        P.dma("sp", xo[si * 128:(si + 1) * 128, :], o[:, :], reads=[o], is_output=True)
    P.pop_scope()
    if standalone:
        return P.build()
    return None


def build_phase_b2(S, NH, P, io):
    build_phase_b(S, NH, S, do_conv=True, do_mla=False, do_sb=False, P=P, io=io)
    P.push_scope()
    NB = S // 128
    NQC = S // 512
    mq = io["mq"]; mk = io["mk"]; mv = io["mv"]; sq = io["sq"]; sk = io["sk"]; sv = io["sv"]
    cst = io["cst"]; mo = io["mo"]; so = io["so"]
    pb = [P.ps("pb%d" % i, [128, 512], F32) for i in range(8)]
    cstf = P.sb("cstf", [128, 4, 128], F32)
    P.dma("sp", cstf[:, :, :], cst[:, :, :], writes=[cstf])
    cstb = P.sb("cstb", [128, 4, 128], BF16)
    P.copy("dve", cstb, cstb[:, :, :], cstf, cstf[:, :, :])
    onesb = P.sb("onesb", [128, 128], BF16)
    P.memset("dve", onesb, onesb[:, :], 1.0)
    onesf = P.sb("onesf", [128, 128], F32)
    P.memset("dve", onesf, onesf[:, :], 1.0)
    mask_sb16 = cstb[:, 1, :]
    negtri16 = cstb[:, 3, :]

    kT = P.sb("kT", [96, S], BF16)
    qT = P.sb("qT", [96, S], BF16)
    vA = [P.sb("vA%d" % i, [128, NB, 65], BF16) for i in range(2)]
    pT = [P.sb("pTm%d" % i, [128, 512], BF16) for i in range(3)]
    dsb = P.sb("dsb", [128, 512], F32)
    rsb = P.sb("rsb", [128, 512], F32)
    bcs = P.sb("bcs", [64, 512], F32)
    ost = [P.sb("ost%d" % i, [64, 512], BF16) for i in range(2)]
    m_items = []
    for h in range(NH):
        for qc in range(NQC):
            nk = 4 * (qc + 1)
            for kb in range(nk):
                m_items.append((h, qc, kb, nk))
    loaded = set()

    def load_head(h):
        if h in loaded:
            return
        loaded.add(h)
        P.dma("sp", kT[:, :], mk[h, :, :], writes=[kT])
        P.dma("act", qT[:, :], mq[h, :, :], writes=[qT])
        P.memset("pool", vA[h % 2], vA[h % 2][:, :, :], 1.0)
        P.dma("sp", vA[h % 2][:, :, 0:64], mv[:, h * 64:(h + 1) * 64].rearrange("(b p) c -> p b c", p=128), writes=[vA[h % 2]])

    def m_s1(k, it):
        h, qc, kb, nk = it
        load_head(h)
        c0 = max(0, kb - 4 * qc) * 128
        Sb = pb[k % 2]
        P.mm(Sb, Sb[:, c0:512], kT, kT[:, kb * 128:(kb + 1) * 128], qT, qT[:, qc * 512 + c0:qc * 512 + 512], True, True)
        pt = pT[k % 3]
        P.act(pt, pt[:, c0:512], Sb, Sb[:, c0:512], AF.Exp)
        if kb >= 4 * qc:
            P.memset("pool", pt, pt[64:128, c0:c0 + 64], 0.0)

    def m_s2(k, it):
        h, qc, kb, nk = it
        c0 = max(0, kb - 4 * qc) * 128
        O = pb[2]
        pt = pT[k % 3]
        P.mm(O, O[0:65, c0:512], vA[h % 2], vA[h % 2][:, kb, :], pt, pt[:, c0:512], kb == 0, True, sgc=(kb > 0))
        if kb == nk - 1:
            g = h * NQC + qc
            P.copy("act", dsb, dsb[64:65, :], O, O[64:65, :])
            P.op("dve", lambda e: e.reciprocal(rsb[64:65, :], dsb[64:65, :]), reads=[dsb], writes=[rsb])
            bc = pb[7]
            P.mm(bc, bc[0:64, :], onesf, onesf[64:65, 0:64], rsb, rsb[64:65, :], True, True)
            P.copy("act", bcs, bcs[:, :], bc, bc[0:64, :])
            os_ = ost[g % 2]
            P.tt("dve", os_, os_[:, :], O, O[0:64, :], bcs, bcs[:, :], ALU.mult)
            P.dma("sp", mo[h * 64:(h + 1) * 64, qc * 512:(qc + 1) * 512], os_[:, :], reads=[os_], is_output=True)

    GH = 4
    skT = [P.sb("skT%d" % i, [128, S], BF16) for i in range(2)]
    sqT = [P.sb("sqT%d" % i, [128, S], BF16) for i in range(2)]
    svt = [[P.sb("svt%d_%d" % (g, i), [128, NB, 64], BF16) for i in range(GH)] for g in range(2)]
    e_t = [P.sb("e_t%d" % i, [128, 512], F32) for i in range(3)]
    lp16 = [P.sb("lp16_%d" % i, [128, 512], BF16) for i in range(3)]
    t_t = [P.sb("t_t%d" % i, [128, 512], F32) for i in range(3)]
    a16 = [P.sb("a16_%d" % i, [128, 512], BF16) for i in range(3)]
    cacc = [P.sb("cacc%d" % i, [128, 512], F32) for i in range(2)]
    sst = [P.sb("sst%d" % i, [64, 512], BF16) for i in range(2)]
    s_items = []
    for h in range(NH):
        for qc in range(NQC):
            nk = 4 * (qc + 1)
            for kb in range(nk - 1, -1, -1):
                s_items.append((h, qc, kb, nk))
    gloaded = set()

    def load_group(hg):
        if hg in gloaded:
            return
        gloaded.add(hg)
        for pr in range(2):
            r0 = (hg * 2 + pr) * 128
            P.dma("sp", skT[pr][:, :], sk[r0:r0 + 128, :], writes=[skT[pr]])
            P.dma("act", sqT[pr][:, :], sq[r0:r0 + 128, :], writes=[sqT[pr]])
        for hl in range(GH):
            hh = hg * GH + hl
            t = svt[hg % 2][hl]
            P.dma("sp", t[:, :, :], sv[:, hh * 64:(hh + 1) * 64].rearrange("(b p) c -> p b c", p=128), writes=[t])

    def s_s1(k, it):
        h, qc, kb, nk = it
        load_group(h // GH)
        hl = h % GH
        c0 = max(0, kb - 4 * qc) * 128
        g = h * NQC + qc
        if kb == nk - 1:
            P.memset("pool", cacc[g % 2], cacc[g % 2][:, :], 0.0)
        Zb = pb[3 + k % 2]
        hp = slice((hl % 2) * 64, (hl % 2) * 64 + 64)
        P.mm(Zb, Zb[:, c0:512], skT[hl // 2], skT[hl // 2][hp, kb * 128:(kb + 1) * 128], sqT[hl // 2], sqT[hl // 2][hp, qc * 512 + c0:qc * 512 + 512], True, True)
        P.act(e_t[k % 3], e_t[k % 3][:, c0:512], Zb, Zb[:, c0:512], AF.Exp)
        P.act(lp16[k % 3], lp16[k % 3][:, c0:512], e_t[k % 3], e_t[k % 3][:, c0:512], AF.Ln, bias=1.0)
        if kb >= 4 * qc:
            P.tt("pool", lp16[k % 3], lp16[k % 3][:, c0:c0 + 128], lp16[k % 3], lp16[k % 3][:, c0:c0 + 128], cstb, mask_sb16, ALU.mult)

    def s_s2(k, it):
        h, qc, kb, nk = it
        c0 = max(0, kb - 4 * qc) * 128
        g = h * NQC + qc
        Zb = pb[3 + k % 2]
        Cb = pb[5]
        L = lp16[k % 3]
        P.mm(Zb, Zb[:, c0:512], cstb, negtri16, L, L[:, c0:512], False, True, sgc=True)
        P.mm(Cb, Cb[:, c0:512], onesb, onesb[:, :], L, L[:, c0:512], True, True)
        ca = cacc[g % 2]
        P.tt("dve", t_t[k % 3], t_t[k % 3][:, c0:512], Zb, Zb[:, c0:512], ca, ca[:, c0:512], ALU.subtract)
        P.act(a16[k % 3], a16[k % 3][:, c0:512], t_t[k % 3], t_t[k % 3][:, c0:512], AF.Exp)
        if kb >= 4 * qc:
            P.tt("pool", a16[k % 3], a16[k % 3][:, c0:c0 + 128], a16[k % 3], a16[k % 3][:, c0:c0 + 128], cstb, mask_sb16, ALU.mult)
        if kb > 0:
            P.tt("dve", ca, ca[:, c0:512], Cb, Cb[:, c0:512], ca, ca[:, c0:512], ALU.add)

    def s_s3(k, it):
        h, qc, kb, nk = it
        c0 = max(0, kb - 4 * qc) * 128
        g = h * NQC + qc
        O = pb[6]
        t = svt[(h // GH) % 2][h % GH]
        P.mm(O, O[0:64, c0:512], t, t[:, kb, :], a16[k % 3], a16[k % 3][:, c0:512], kb == nk - 1, True, sgc=(kb < nk - 1))
        if kb == 0:
            os_ = sst[g % 2]
            P.copy("act", os_, os_[:, :], O, O[0:64, :])
            P.dma("sp", so[h * 64:(h + 1) * 64, qc * 512:(qc + 1) * 512], os_[:, :], reads=[os_], is_output=True)

    nm, ns = len(m_items), len(s_items)
    for step in range(max(nm, ns) + 2):
        if step < nm:
            m_s1(step, m_items[step])
        if step < ns:
            s_s1(step, s_items[step])
        if 0 <= step - 1 < nm:
            m_s2(step - 1, m_items[step - 1])
        if 0 <= step - 1 < ns:
            s_s2(step - 1, s_items[step - 1])
        if 0 <= step - 2 < ns:
            s_s3(step - 2, s_items[step - 2])
    P.pop_scope()


def load_gate_weights(P, gwr, w_in):
    for n in range(4):
        for kc in range(8):
            c0 = C_GATE + n * 1024
            P.dma("pool", gwr[:, n, kc, :], w_in[kc * 128:(kc + 1) * 128, c0:c0 + 1024], writes=[gwr])


def build_phase_c1(T, moe, P, io):
    P.push_scope()
    x = io["x"]; xT_d = io["xT"]; brs = io["brs"]; w_in = io["w_in"]; bgc_d = io["bgc"]
    w_br = io["w_br"]; w_o = io["w_o"]; lnp = io["lnp"]; wr_d = io["wr"]; ident_d = io["ident"]
    x1_o = io["x1_o"]; x1T_o = io["x1T_o"]; comb_o = io["comb_o"]
    W = 512
    NSW = W // 128
    pb = Banks(P, 7)
    pbl = P.ps("pbl", [128, 512], F32)
    identf = P.sb("identf", [128, 128], F32)
    P.dma("sp", identf[:, :], ident_d[:, :], writes=[identf])
    bgc = P.sb("bgc_s", [128, 32], F32)
    P.dma("sp", bgc[:, :], bgc_d[:, :], writes=[bgc])
    epsl = P.sb("epsl", [128, 1], F32)
    P.memset("dve", epsl, epsl[:, :], LN_EPS)
    lg = P.sb("lg", [128, D], F32)
    lb = P.sb("lb", [128, D], F32)
    P.dma("sp", lg[:, :], lnp[0:1, :].partition_broadcast(128), writes=[lg])
    P.dma("sp", lb[:, :], lnp[1:2, :].partition_broadcast(128), writes=[lb])
    wr_s = P.sb("wr_s", [128, 8, 8], F32)
    P.dma("sp", wr_s[:, :, :], wr_d[:, :, :], writes=[wr_s])
    gwr = io.get("gwr_buf")
    pre = gwr is not None
    if not pre:
        gwr = P.sb("gwr", [128, 4, 8, 1024], BF16)
    wbr = P.sb("wbr", [128, 4, 4, 1024], BF16)
    wo = P.sb("wo", [128, 8, 1024], BF16)
    for n in range(4):
        for kc in range(4):
            P.dma("pool", wbr[:, n, kc, :], w_br[n, kc * 128:(kc + 1) * 128, :], writes=[wbr])
    if not pre:
        load_gate_weights(P, gwr, w_in)
    for kc in range(8):
        P.dma("pool", wo[:, kc, :], w_o[kc * 128:(kc + 1) * 128, :], writes=[wo])
    stats = P.sb("stats", [128, 2, 6], F32)
    mv_ = P.sb("mv_", [128, 2], F32)
    rs_ = P.sb("rs_", [128, 1], F32)
    rs2 = P.sb("rs2", [128, 1], F32)

    def layer_norm(zb, z_ap, ob, o_ap):
        for hh in range(2):
            P.op("dve", lambda e, hh=hh: e.bn_stats(stats[:, hh, :], z_ap[:, hh * 512:(hh + 1) * 512]), reads=[zb], writes=[stats])
        P.op("dve", lambda e: e.bn_aggr(mv_[:, :], stats[:, :, :].rearrange("p a b -> p (a b)")), reads=[stats], writes=[mv_])
        P.act(rs_, rs_[:, :], mv_, mv_[:, 1:2], AF.Sqrt, bias=epsl[:, 0:1], extra_reads=[epsl])
        P.op("dve", lambda e: e.reciprocal(rs2[:, :], rs_[:, :]), reads=[rs_], writes=[rs2])
        P.ts("dve", ob, o_ap, zb, z_ap, mv_[:, 0:1], rs2[:, 0:1], ALU.subtract, ALU.mult, extra_reads=[mv_, rs2])
        P.tt("pool", ob, o_ap, ob, o_ap, lg, lg[:, :], ALU.mult)
        P.tt("pool", ob, o_ap, ob, o_ap, lb, lb[:, :], ALU.add)

    xT_t = [P.sb("xT_t%d" % i, [128, 8, W], BF16) for i in range(1)] * 2
    br_t = [P.sb("br_t%d" % i, [128, 4, 4, W], BF16) for i in range(1)] * 2
    g_t = [P.sb("g_t%d" % i, [128, W], F32) for i in range(2)]
    tmp_t = [P.sb("tmp_t%d" % i, [128, W], F32) for i in range(1)] * 2
    mrg = P.sb("mrg", [128, W], F32)
    mrgT2 = [P.sb("mrgT%d" % i, [128, 8, W], BF16) for i in range(2)]
    xs_ = [P.sb("xs_%d" % i, [128, D], F32) for i in range(1)] * 2
    z_t = P.sb("z_t", [128, D], F32)
    x1_t = [P.sb("x1_t%d" % i, [128, D], F32) for i in range(4)]
    x1Ts = [P.sb("x1Ts%d" % i, [128, 8, 128], BF16) for i in range(2)]
    x1a = P.sb("x1a", [128, D], F32)
    x1T32 = [P.sb("x1T32_%d" % i, [128, 128], F32) for i in range(2)]
    lgt = P.sb("lgt", [128, 8], F32)
    lgt4 = P.sb("lgt4", [128, NSW, 8], F32)
    lgT = P.sb("lgT", [8, 128], F32)
    eq1_ = P.sb("eq1_", [128, NSW, 8], F32)
    eq2_ = P.sb("eq2_", [128, NSW, 8], F32)
    l2_ = P.sb("l2_", [128, NSW, 8], F32)
    m1_ = P.sb("m1_", [128, NSW], F32)
    m2_ = P.sb("m2_", [128, NSW], F32)
    dd_ = P.sb("dd_", [128, NSW], F32)
    w1_ = P.sb("w1_", [128, NSW], F32)
    w2_ = P.sb("w2_", [128, NSW], F32)
    cmb4 = [P.sb("cmb4_%d" % i, [128, NSW, 8], F32) for i in range(2)]
    sm = [P.sb("sm%d" % i, [128, 8], F32) for i in range(4)]
    cmb = [P.sb("cmb%d" % i, [128, 8], F32) for i in range(2)]
    sc1 = [P.sb("sc1_%d" % i, [128, 1], F32) for i in range(6)]
    NTt = T // W

    def loads(tt):
        t0 = tt * W
        xt = xT_t[tt % 2]
        brt = br_t[tt % 2]
        P.dma("sp", xt[:, :, :], xT_d[:, t0:t0 + W].rearrange("(k p) t -> p k t", p=128), writes=[xt])
        for n in range(4):
            P.dma("act", brt[:, n, :, :], brs[n, :, t0:t0 + W].rearrange("(k p) t -> p k t", p=128), writes=[brt])

    def merge_m(tt, m):
        xt = xT_t[tt % 2]
        brt = br_t[tt % 2]
        mrgT = mrgT2[tt % 2]
        for n in range(4):
            bG = pb.next()
            for kc in range(8):
                P.mm(bG, bG[:, 0:W], gwr, gwr[:, n, kc, m * 128:(m + 1) * 128], xt, xt[:, kc, :], kc == 0, kc == 7)
            gt = g_t[n % 2]
            P.act(gt, gt[:, :], bG, bG[:, 0:W], AF.Sigmoid, bias=bgc[:, n * 8 + m:n * 8 + m + 1], extra_reads=[bgc])
            bP = pb.next()
            for kc in range(4):
                P.mm(bP, bP[:, 0:W], wbr, wbr[:, n, kc, m * 128:(m + 1) * 128], brt, brt[:, n, kc, :], kc == 0, kc == 3)
            if n == 0:
                P.tt("dve", mrg, mrg[:, :], bP, bP[:, 0:W], gt, gt[:, :], ALU.mult)
            else:
                tp = tmp_t[n % 2]
                P.tt("dve", tp, tp[:, :], bP, bP[:, 0:W], gt, gt[:, :], ALU.mult)
                P.tt("pool", mrg, mrg[:, :], mrg, mrg[:, :], tp, tp[:, :], ALU.add)
        P.copy("act", mrgT, mrgT[:, m, :], mrg, mrg[:, :])

    def post_wout(tt, s):
        t0 = tt * W
        mrgT = mrgT2[tt % 2]
        si = tt * NSW + s
        xs = xs_[si % 2]
        x1t = x1_t[s % 4]
        P.dma("sp", xs[:, :], x[t0 + s * 128:t0 + (s + 1) * 128, :], writes=[xs])
        for hh in range(2):
            b = pb.next()
            for kc in range(8):
                P.mm(b, b[:, :], mrgT, mrgT[:, kc, s * 128:(s + 1) * 128], wo, wo[:, kc, hh * 512:(hh + 1) * 512], kc == 0, kc == 7)
            P.stt(z_t, z_t[:, hh * 512:(hh + 1) * 512], xs, xs[:, hh * 512:(hh + 1) * 512], DN_ALPHA, b, b[:, :], ALU.mult, ALU.add)
        layer_norm(z_t, z_t[:, :], x1t, x1t[:, :])
        P.op("act", lambda e: e.mul(x1a[:, :], x1t[:, :], DN_ALPHA), reads=[x1t], writes=[x1a])
        P.dma("sp", x1_o[si * 128:(si + 1) * 128, :], x1a[:, :], reads=[x1a], is_output=True)

    def post_tr(tt, s):
        si = tt * NSW + s
        x1t = x1_t[s % 4]
        xts = x1Ts[si % 2]
        bl = pbl
        for kc in range(8):
            b = pb.next()
            P.tr(b, b[:, 0:128], x1t, x1t[:, kc * 128:(kc + 1) * 128], identf, identf[:, :])
            P.copy("act", xts, xts[:, kc, :], b, b[:, 0:128])
            if moe:
                xt32 = x1T32[kc % 2]
                P.copy("dve", xt32, xt32[:, :], b, b[:, 0:128])
                P.mm(bl, bl[0:8, 0:128], wr_s, wr_s[:, kc, :], xt32, xt32[:, :], kc == 0, kc == 7)
        P.dma("sp", x1T_o[:, si * 128:(si + 1) * 128].rearrange("(k p) t -> p k t", p=128), xts[:, :, :], reads=[xts], is_output=True)
        if moe:
            P.copy("act", lgT, lgT[0:8, :], bl, bl[0:8, 0:128])
            b2 = pb.next()
            P.tr(b2, b2[:, 0:8], lgT, lgT[0:8, :], identf, identf[0:8, 0:8])
            P.copy("dve", lgt4, lgt4[:, s, :], b2, b2[:, 0:8])
            if s == NSW - 1:
                def bc_(t_):
                    return t_.t[:, :].unsqueeze(2).to_broadcast([128, NSW, 8])
                P.op("dve", lambda e: e.reduce_max(m1_[:, :], lgt4[:, :, :], AX.X), reads=[lgt4], writes=[m1_])
                P.tt("dve", eq1_, eq1_[:, :, :], lgt4, lgt4[:, :, :], m1_, bc_(m1_), ALU.is_equal)
                P.stt(l2_, l2_[:, :, :], eq1_, eq1_[:, :, :], -1e30, lgt4, lgt4[:, :, :], ALU.mult, ALU.add)
                P.op("dve", lambda e: e.reduce_max(m2_[:, :], l2_[:, :, :], AX.X), reads=[l2_], writes=[m2_])
                P.tt("dve", eq2_, eq2_[:, :, :], l2_, l2_[:, :, :], m2_, bc_(m2_), ALU.is_equal)
                P.tt("dve", dd_, dd_[:, :], m2_, m2_[:, :], m1_, m1_[:, :], ALU.subtract)
                P.act(w2_, w2_[:, :], dd_, dd_[:, :], AF.Sigmoid)
                P.act(w1_, w1_[:, :], dd_, dd_[:, :], AF.Sigmoid, scale=-1.0)
                P.tt("dve", eq1_, eq1_[:, :, :], eq1_, eq1_[:, :, :], w1_, bc_(w1_), ALU.mult)
                P.tt("dve", eq2_, eq2_[:, :, :], eq2_, eq2_[:, :, :], w2_, bc_(w2_), ALU.mult)
                cb = cmb4[tt % 2]
                P.tt("dve", cb, cb[:, :, :], eq1_, eq1_[:, :, :], eq2_, eq2_[:, :, :], ALU.add)
                r0 = tt * NSW * 128
                P.dma("sp", comb_o[r0:r0 + NSW * 128, :].rearrange("(s p) e -> p s e", p=128), cb[:, :, :], reads=[cb], is_output=True)

    for tt in range(NTt + 1):
        if tt < NTt:
            loads(tt)
        for m in range(8):
            if tt < NTt:
                merge_m(tt, m)
            if tt >= 1:
                if m < NSW:
                    post_wout(tt - 1, m)
                if 3 <= m < 3 + NSW:
                    post_tr(tt - 1, m - 3)
    P.pop_scope()


def build_phase_c2(T, E, P, io):
    moe = E > 1
    P.push_scope()
    x1_d = io["x1"]; x1T_d = io["x1T"]; comb_d = io["comb"]; lnp = io["lnp"]
    f_g = io["f_g"]; f_u = io["f_u"]; f_d = io["f_d"]; xo = io["xo"]
    NT = T // 512
    NS = T // 128
    pb = Banks(P, 8)
    epsl = P.sb("epsl", [128, 1], F32)
    P.memset("dve", epsl, epsl[:, :], LN_EPS)
    acc = P.sb("acc", [128, NS, D], F32)
    comb = P.sb("comb", [128, NS, 8], F32)
    if moe:
        P.dma("sp", comb[:, :, :], comb_d.rearrange("(s p) e -> p s e", p=128), writes=[comb])
    CH = min(8, NS)
    for c in range(NS // CH):
        P.dma("sp" if c % 2 else "act", acc[:, c * CH:(c + 1) * CH, :],
              x1_d[c * CH * 128:(c + 1) * CH * 128, :].rearrange("(s p) d -> p s d", p=128), writes=[acc])
    P.push_scope()
    wbig = P.sb("wbig", [128, 24576], BF16)
    wg_t = [Buf(wbig.t, "wg_t%d" % i) for i in range(2)]
    wu_t = [Buf(wbig.t, "wu_t%d" % i) for i in range(2)]
    wd_t = [Buf(wbig.t, "wd_t%d" % i) for i in range(2)]

    def wv(i, which):
        base = i * 12288 + which * 4096
        if which < 2:
            return wbig.t[:, base:base + 4096].rearrange("p (k c) -> p k c", k=8)
        return wbig.t[:, base:base + 4096].rearrange("p (k c) -> p k c", k=4)

    xT_t = [P.sb("xT_t%d" % i, [128, 8, 512], BF16) for i in range(2)]
    sg_t = [P.sb("sg_t%d" % i, [128, 512], F32) for i in range(2)]
    hT = [P.sb("hT%d" % i, [128, 4, 512], BF16) for i in range(2)]
    u = 0
    xi = 0
    for e_ in range(E):
        for fq in range(7):
            i = u % 2
            u += 1
            f0 = fq * 512
            P.dma("pool", wv(i, 0), f_g[e_, :, f0:f0 + 512].rearrange("(k p) c -> p k c", p=128), writes=[wg_t[i]])
            P.dma("pool", wv(i, 1), f_u[e_, :, f0:f0 + 512].rearrange("(k p) c -> p k c", p=128), writes=[wu_t[i]])
            P.dma("pool", wv(i, 2), f_d[e_, f0:f0 + 512, :].rearrange("(k p) c -> p k c", p=128), writes=[wd_t[i]])
            for tt in range(NT):
                t0 = tt * 512
                xt = xT_t[xi % 2]
                xi += 1
                P.dma("sp", xt[:, :, :], x1T_d[:, t0:t0 + 512].rearrange("(k p) t -> p k t", p=128), writes=[xt])
                ht = hT[tt % 2]
                for fc in range(4):
                    bg = pb.next()
                    for kc in range(8):
                        P.mm(bg, bg[:, :], wg_t[i], wv(i, 0)[:, kc, fc * 128:(fc + 1) * 128], xt, xt[:, kc, :], kc == 0, kc == 7)
                    bu = pb.next()
                    for kc in range(8):
                        P.mm(bu, bu[:, :], wu_t[i], wv(i, 1)[:, kc, fc * 128:(fc + 1) * 128], xt, xt[:, kc, :], kc == 0, kc == 7)
                    sg = sg_t[fc % 2]
                    P.act(sg, sg[:, :], bg, bg[:, :], AF.Silu)
                    P.tt("dve", ht, ht[:, fc, :], bu, bu[:, :], sg, sg[:, :], ALU.mult)
                for s in range(4):
                    si = tt * 4 + s
                    for hh in range(2):
                        bd = pb.next()
                        for fc in range(4):
                            P.mm(bd, bd[:, :], ht, ht[:, fc, s * 128:(s + 1) * 128], wd_t[i], wv(i, 2)[:, fc, hh * 512:(hh + 1) * 512], fc == 0, fc == 3)
                        sc = comb[:, si, e_:e_ + 1] if moe else 1.0
                        P.stt(acc, acc[:, si, hh * 512:(hh + 1) * 512], bd, bd[:, :], sc, acc, acc[:, si, hh * 512:(hh + 1) * 512],
                              ALU.mult, ALU.add, extra_reads=[comb] if moe else [])
    P.pop_scope()
    lg = P.sb("lg", [128, D], F32)
    lb = P.sb("lb", [128, D], F32)
    P.dma("sp", lg[:, :], lnp[2:3, :].partition_broadcast(128), writes=[lg])
    P.dma("sp", lb[:, :], lnp[3:4, :].partition_broadcast(128), writes=[lb])
    stats = [P.sb("stats%d" % i, [128, 2, 6], F32) for i in range(4)]
    mv_ = [P.sb("mv_%d" % i, [128, 2], F32) for i in range(4)]
    rs_ = [P.sb("rs_%d" % i, [128, 1], F32) for i in range(4)]
    rs2 = [P.sb("rs2%d" % i, [128, 1], F32) for i in range(4)]
    ot = [P.sb("ot%d" % i, [128, D], F32) for i in range(2)]
    for si in range(NS):
        o = ot[si % 2]
        z_ap = acc[:, si, :]
        st, mv1, r1, r2 = stats[si % 4], mv_[si % 4], rs_[si % 4], rs2[si % 4]
        for hh in range(2):
            P.op("dve", lambda e, hh=hh, z_ap=z_ap, st=st: e.bn_stats(st[:, hh, :], z_ap[:, hh * 512:(hh + 1) * 512]), reads=[acc], writes=[st])
        P.op("dve", lambda e, st=st, mv1=mv1: e.bn_aggr(mv1[:, :], st[:, :, :].rearrange("p a b -> p (a b)")), reads=[st], writes=[mv1])
        P.act(r1, r1[:, :], mv1, mv1[:, 1:2], AF.Sqrt, bias=epsl[:, 0:1], extra_reads=[epsl])
        P.op("dve", lambda e, r1=r1, r2=r2: e.reciprocal(r2[:, :], r1[:, :]), reads=[r1], writes=[r2])
        P.ts("dve", o, o[:, :], acc, z_ap, mv1[:, 0:1], r2[:, 0:1], ALU.subtract, ALU.mult, extra_reads=[mv1, r2])
        P.tt("dve", o, o[:, :], o, o[:, :], lg, lg[:, :], ALU.mult)
        P.tt("pool", o, o[:, :], o, o[:, :], lb, lb[:, :], ALU.add)
        P.dma("sp", xo[si * 128:(si + 1) * 128, :], o[:, :], reads=[o], is_output=True)
    P.pop_scope()


def build_fused(S=4096, TP=2048):
    P = Prog()
    nc = P.nc
    x = P.dram("x", [S, D], F32, "ExternalInput")
    posr = P.dram("posr", [32, S], I32, "ExternalInput")
    w_in = P.dram("w_in", [2, D, IN_COLS], F32, "ExternalInput")
    w_uq = P.dram("w_uq", [2, 256, 768], F32, "ExternalInput")
    w_ukv = P.dram("w_ukv", [2, 128, 1024], F32, "ExternalInput")
    qn = P.dram("qn", [2, 128, 2], F32, "ExternalInput")
    kvn = P.dram("kvn", [2, 128, 1], F32, "ExternalInput")
    ident = P.dram("ident", [128, 128], F32, "ExternalInput")
    invf = P.dram("invf", [128, 1], F32, "ExternalInput")
    sc_w = P.dram("sc_w", [2, 128, 4, 3], F32, "ExternalInput")
    cf_w = P.dram("cf_w", [2, 128, 4, 31], F32, "ExternalInput")
    cf_p = P.dram("cf_p", [2, 128, 3, 4], F32, "ExternalInput")
    cst = P.dram("cst", [128, 4, 128], F32, "ExternalInput")
    bgc = P.dram("bgc", [2, 128, 32], F32, "ExternalInput")
    w_br = P.dram("w_br", [2, 4, 512, D], F32, "ExternalInput")
    w_o = P.dram("w_o", [2, D, D], F32, "ExternalInput")
    lnp = P.dram("lnp", [2, 4, D], F32, "ExternalInput")
    f_g1 = P.dram("f_g1", [1, D, 3584], F32, "ExternalInput")
    f_u1 = P.dram("f_u1", [1, D, 3584], F32, "ExternalInput")
    f_d1 = P.dram("f_d1", [1, 3584, D], F32, "ExternalInput")
    f_g8 = P.dram("f_g8", [8, D, 3584], F32, "ExternalInput")
    f_u8 = P.dram("f_u8", [8, D, 3584], F32, "ExternalInput")
    f_d8 = P.dram("f_d8", [8, 3584, D], F32, "ExternalInput")
    wr = P.dram("wr", [128, 8, 8], F32, "ExternalInput")
    xo = P.dram("xo", [S, D], F32, "ExternalOutput")

    def I_(name, shape, dt):
        return nc.dram_tensor(name, list(shape), dt).ap()
    xT_s = I_("xT_s", [D, S], BF16)
    scb_s = I_("scb_s", [512, S], BF16)
    scch_s = I_("scch_s", [512, S + 2], BF16)
    cfu_s = I_("cfu_s", [512, S + 30], BF16)
    mq_s = I_("mq_s", [8, 96, S], BF16)
    mk_s = I_("mk_s", [8, 96, S], BF16)
    mv_s = I_("mv_s", [S, 512], BF16)
    sq_s = I_("sq_s", [512, S], BF16)
    sk_s = I_("sk_s", [512, S], BF16)
    sv_s = I_("sv_s", [S, 512], BF16)
    brs_s = I_("brs_s", [4, 512, S], BF16)
    xmid = I_("xmid", [S, D], F32)
    x1_s = I_("x1_s", [S, D], F32)
    x1T_s = I_("x1T_s", [D, S], BF16)
    comb_s = I_("comb_s", [S, 8], F32)

    P.push_scope()
    zt = P.sb("zt", [128, 4, 32], BF16)
    P.memset("dve", zt, zt[:, :, :], 0.0)
    P.dma("sp", scch_s[:, 0:2].rearrange("(j p) t -> p j t", p=128), zt[:, :, 0:2], reads=[zt])
    P.dma("sp", cfu_s[:, 0:30].rearrange("(j p) t -> p j t", p=128), zt[:, :, 0:30], reads=[zt])
    P.pop_scope()

    for l in range(2):
        xin = x if l == 0 else xmid
        xout = xmid if l == 0 else xo
        io_a = {"x": xin, "posr": posr, "w_in": w_in[l], "w_uq": w_uq[l], "w_ukv": w_ukv[l],
                "qn": qn[l], "kvn": kvn[l], "ident": ident, "invf": invf,
                "xT_o": xT_s, "scb_o": scb_s, "scch_o": scch_s[:, 2:2 + S], "cfu_o": cfu_s[:, 30:30 + S],
                "mq_o": mq_s, "mk_o": mk_s, "mv_o": mv_s, "sq_o": sq_s, "sk_o": sk_s, "sv_o": sv_s}
        build_phase_a(S, P=P, io=io_a)
        io_b = {"scb": scb_s, "scch": scch_s, "cfu": cfu_s, "sc_w": sc_w[l], "cf_w": cf_w[l], "cf_p": cf_p[l],
                "mq": mq_s, "mk": mk_s, "mv": mv_s, "sq": sq_s, "sk": sk_s, "sv": sv_s, "cst": cst,
                "br0": brs_s[0], "br1": brs_s[1], "mo": brs_s[2], "so": brs_s[3]}
        P.push_scope()
        gwr = P.sb("gwr", [128, 4, 8, 1024], BF16)
        load_gate_weights(P, gwr, w_in[l])
        build_phase_b2(S, 8, P, io_b)
        E = 1 if l == 0 else 8
        io_c1 = {"x": xin, "xT": xT_s, "brs": brs_s, "w_in": w_in[l], "bgc": bgc[l], "w_br": w_br[l], "w_o": w_o[l],
                 "lnp": lnp[l], "wr": wr, "ident": ident, "x1_o": x1_s, "x1T_o": x1T_s, "comb_o": comb_s}
        io_c1["gwr_buf"] = gwr
        build_phase_c1(S, E > 1, P, io_c1)
        P.pop_scope()
        io_c2 = {"x1": x1_s, "x1T": x1T_s, "comb": comb_s, "lnp": lnp[l],
                 "f_g": f_g1 if E == 1 else f_g8, "f_u": f_u1 if E == 1 else f_u8,
                 "f_d": f_d1 if E == 1 else f_d8, "xo": xout}
        build_phase_c2(S, E, P, io_c2)
    return P.build()


def fused_inputs(inp, seq, S=4096):
    f32 = np.float32
    x = np.asarray(inp["x"], f32)[seq][:S]
    pos = np.asarray(inp["positions"])[seq][:S].astype(np.int32)
    invf = np.zeros((128, 1), f32)
    inv = (1.0 / (10000.0 ** (np.arange(16, dtype=f32) * (2.0 / 32)))).astype(f32)
    invf[64:80, 0] = inv
    invf[80:96, 0] = inv
    cst = np.zeros((128, 4, 128), f32)
    pp = np.arange(128)[:, None]
    qq = np.arange(128)[None, :]
    cst[:, 0] = np.eye(128)
    cst[:, 1] = (pp < qq)
    cst[:, 2] = ~((pp >= 64) & (qq < 64))
    cst[:, 3] = -(pp >= qq).astype(f32)
    A = lambda k: np.asarray(inp[k], f32)
    d = {
        "x": np.ascontiguousarray(x),
        "posr": np.ascontiguousarray(np.broadcast_to(pos[None, :], (32, S))),
        "w_in": A("w_in"), "w_uq": A("mla_w_uq"), "w_ukv": A("mla_w_ukv"),
        "qn": np.ascontiguousarray(A("mla_q_norm").reshape(2, 2, 128).transpose(0, 2, 1)),
        "kvn": A("mla_kv_norm").reshape(2, 128, 1),
        "ident": np.eye(128, dtype=f32), "invf": invf,
        "sc_w": np.ascontiguousarray(A("sc_conv").transpose(0, 2, 1).reshape(2, 4, 128, 3).transpose(0, 2, 1, 3)),
        "cf_w": np.ascontiguousarray(A("cf_conv").transpose(0, 2, 1).reshape(2, 4, 128, 31).transpose(0, 2, 1, 3)),
        "cf_p": np.ascontiguousarray(np.stack([np.stack([_col4(A(k)[l]) for k in ("cf_conv_bias", "cf_ln_g", "cf_ln_b")], 1)
                                               for l in range(2)])),
        "cst": cst,
        "bgc": np.ascontiguousarray(A("b_gate").reshape(2, 32, 128).transpose(0, 2, 1)),
        "w_br": A("w_branch"), "w_o": A("w_out"),
        "lnp": np.ascontiguousarray(np.stack([np.stack([A("ln_mix_g")[l], A("ln_mix_b")[l], A("ln_ffn_g")[l], A("ln_ffn_b")[l]])
                                              for l in range(2)])),
        "f_g1": A("ffn_w_gate"), "f_u1": A("ffn_w_up"), "f_d1": A("ffn_w_down"),
        "f_g8": A("exp_w_gate")[0], "f_u8": A("exp_w_up")[0], "f_d8": A("exp_w_down")[0],
        "wr": np.ascontiguousarray(A("router_w")[0].reshape(8, 128, 8).transpose(1, 0, 2)),
    }
    return d


_PROGS = {}


def _prog(key, fn):
    if key not in _PROGS:
        _PROGS[key] = fn()
    return _PROGS[key]


def _run(nc, in_maps):
    res = run_bass_kernel_spmd(nc, in_maps, core_ids=list(range(8)))
    return res.results


def _col4(v):
    return np.ascontiguousarray(v.reshape(4, 128).T)


def kernel_unfused(x, positions, w_in, b_gate, sc_conv, cf_conv, cf_conv_bias, cf_ln_g, cf_ln_b,
           mla_q_norm, mla_w_uq, mla_kv_norm, mla_w_ukv, w_branch, w_out,
           ln_mix_g, ln_mix_b, ln_ffn_g, ln_ffn_b, ffn_w_gate, ffn_w_up, ffn_w_down,
           router_w, exp_w_gate, exp_w_up, exp_w_down):
    f32 = np.float32
    T = 2048
    S = 4096
    xf = np.ascontiguousarray(np.asarray(x, f32).reshape(-1, D))
    pos = np.asarray(positions).reshape(-1).astype(np.int32)
    ident = np.eye(128, dtype=f32)
    invf = np.zeros((128, 1), f32)
    inv = (1.0 / (10000.0 ** (np.arange(16, dtype=f32) * (2.0 / 32)))).astype(f32)
    invf[64:80, 0] = inv
    invf[80:96, 0] = inv
    cst = np.zeros((128, 4, 128), f32)
    pp = np.arange(128)[:, None]
    qq = np.arange(128)[None, :]
    cst[:, 0] = np.eye(128)
    cst[:, 1] = (pp < qq)
    cst[:, 2] = ~((pp >= 64) & (qq < 64))
    cst[:, 3] = -(pp >= qq).astype(f32)

    ncA = _prog("A", lambda: build_phase_a(T))
    ncB = _prog("B", lambda: build_phase_b(S, 4, T))
    for l in range(2):
        wl = np.ascontiguousarray(np.asarray(w_in[l], f32))
        insA = []
        for c in range(8):
            sl = slice(c * T, (c + 1) * T)
            insA.append({
                "x": xf[sl],
                "posr": np.ascontiguousarray(np.broadcast_to(pos[sl][None, :], (32, T))),
                "w_in": wl,
                "w_uq": np.asarray(mla_w_uq[l], f32), "w_ukv": np.asarray(mla_w_ukv[l], f32),
                "qn": np.ascontiguousarray(np.asarray(mla_q_norm[l], f32).reshape(2, 128).T),
                "kvn": np.asarray(mla_kv_norm[l], f32).reshape(128, 1),
                "ident": ident, "invf": invf})
        rA = _run(ncA, insA)
        insB = []
        for c in range(8):
            sq_, half = c // 2, c % 2
            r0, r1 = rA[2 * sq_], rA[2 * sq_ + 1]

            def cat(name, axis):
                return np.concatenate([np.asarray(r0[name]), np.asarray(r1[name])], axis=axis)
            scch_f = cat("scch_o", 1)
            cfu_f = cat("cfu_o", 1)
            zt = scch_f.dtype
            scch_p = np.concatenate([np.zeros((512, 2), zt), scch_f], axis=1)
            cfu_p = np.concatenate([np.zeros((512, 30), zt), cfu_f], axis=1)
            t0 = half * T
            hs = slice(half * 4, half * 4 + 4)
            hw = slice(half * 256, half * 256 + 256)
            insB.append({
                "scb": np.ascontiguousarray(np.asarray(rA[c]["scb_o"])),
                "scch": np.ascontiguousarray(scch_p[:, t0:t0 + T + 2]),
                "cfu": np.ascontiguousarray(cfu_p[:, t0:t0 + T + 30]),
                "sc_w": np.ascontiguousarray(np.asarray(sc_conv[l], f32).T.reshape(4, 128, 3).transpose(1, 0, 2)),
                "cf_w": np.ascontiguousarray(np.asarray(cf_conv[l], f32).T.reshape(4, 128, 31).transpose(1, 0, 2)),
                "cf_p": np.ascontiguousarray(np.stack([_col4(np.asarray(cf_conv_bias[l], f32)),
                                                       _col4(np.asarray(cf_ln_g[l], f32)),
                                                       _col4(np.asarray(cf_ln_b[l], f32))], 1)),
                "mq": np.ascontiguousarray(cat("mq_o", 2)[hs]),
                "mk": np.ascontiguousarray(cat("mk_o", 2)[hs]),
                "mv": np.ascontiguousarray(cat("mv_o", 0)[:, hw]),
                "sq": np.ascontiguousarray(cat("sq_o", 1)[hw]),
                "sk": np.ascontiguousarray(cat("sk_o", 1)[hw]),
                "sv": np.ascontiguousarray(cat("sv_o", 0)[:, hw]),
                "cst": cst})
        rB = _run(ncB, insB)
        moe = (l % 2 == 1)
        E = 8 if moe else 1
        ncC = _prog("C%d" % E, lambda: build_phase_c(T, E))
        i = l // 2
        if moe:
            fg, fu, fd = np.asarray(exp_w_gate[i], f32), np.asarray(exp_w_up[i], f32), np.asarray(exp_w_down[i], f32)
            wr = np.ascontiguousarray(np.asarray(router_w[i], f32).reshape(8, 128, 8).transpose(1, 0, 2))
        else:
            fg, fu, fd = (np.asarray(ffn_w_gate[i:i + 1], f32), np.asarray(ffn_w_up[i:i + 1], f32),
                          np.asarray(ffn_w_down[i:i + 1], f32))
            wr = np.zeros((128, 8, 8), f32)
        lnp = np.ascontiguousarray(np.stack([ln_mix_g[l], ln_mix_b[l], ln_ffn_g[l], ln_ffn_b[l]]).astype(f32))
        bgc = np.ascontiguousarray(np.asarray(b_gate[l], f32).reshape(32, 128).T)
        insC = []
        for c in range(8):
            sq_, half = c // 2, c % 2
            t0 = half * T
            br2 = np.concatenate([np.asarray(rB[2 * sq_]["mo"]), np.asarray(rB[2 * sq_ + 1]["mo"])], axis=0)[:, t0:t0 + T]
            br3 = np.concatenate([np.asarray(rB[2 * sq_]["so"]), np.asarray(rB[2 * sq_ + 1]["so"])], axis=0)[:, t0:t0 + T]
            brs_ = np.ascontiguousarray(np.stack([np.asarray(rB[c]["br0"]), np.asarray(rB[c]["br1"]), br2, br3]))
            insC.append({
                "x": xf[c * T:(c + 1) * T], "xT": np.ascontiguousarray(np.asarray(rA[c]["xT_o"])),
                "brs": brs_, "w_in": wl, "bgc": bgc,
                "w_br": np.asarray(w_branch[l], f32), "w_o": np.asarray(w_out[l], f32),
                "lnp": lnp, "f_g": fg, "f_u": fu, "f_d": fd, "wr": wr, "ident": ident})
        rC = _run(ncC, insC)
        xf = np.ascontiguousarray(np.concatenate([np.asarray(r["xo"], f32) for r in rC], axis=0))
    return xf.reshape(4, S, D).astype(f32)


def kernel(**inp):
    S = 4096
    nc = _prog("F", lambda: build_fused(S, 2048))
    maps = [fused_inputs(inp, s_, S) for s_ in range(4)]
    zero_map = {k: np.zeros_like(v) for k, v in maps[0].items()}
    active = [0, 1, 4, 5]
    in_maps = [zero_map] * 8
    in_maps = list(in_maps)
    for s_, c in enumerate(active):
        in_maps[c] = maps[s_]
    res = run_bass_kernel_spmd(nc, in_maps, core_ids=list(range(8)))
    out = np.stack([np.asarray(res.results[c]["xo"], np.float32) for c in active], axis=0)
    return out.astype(np.float32)
```

```python
from contextlib import ExitStack
import numpy as np
import concourse.bass as bass
import concourse.mybir as mybir
from concourse.bass_utils import run_bass_kernel_spmd

F32 = mybir.dt.float32
BF16 = mybir.dt.bfloat16
I32 = mybir.dt.int32
AF = mybir.ActivationFunctionType
ALU = mybir.AluOpType
AX = mybir.AxisListType

ENGS = ("pe", "act", "dve", "pool", "sp")
EPOCH = 16000
N_DMA_SEMS = 16
DMA_MAX_USES = 1900
SAME_ENG_SYNC = True


class Buf:
    def __init__(self, t, name):
        self.t = t
        self.name = name
        self.w = None
        self.r = {}
        self.psum = False

    def __getitem__(self, idx):
        return self.t[idx]

    def sub(self):
        return Buf(self.t, self.name + "_sub")


class Prog:
    def __init__(self):
        self.nc = bass.Bass("TRN2", target_bir_lowering=False)
        self.stack = ExitStack()
        self.root = self.stack
        self.scopes = []
        self.ops = {e: [] for e in ENGS}
        self.count = {e: 0 for e in ENGS}
        self.seen = {e: {} for e in ENGS}
        self.sems = {}
        self.dma_uses = [0] * (3 * N_DMA_SEMS)
        self.dma_rr = {"sp": 0, "act": 0, "pool": 0}
        self.dma_sem_h = []
        self.n_dram = 0
        self.out_events = []
        self.cc_inc = 16

    def dram(self, name, shape, dt, kind):
        return self.nc.dram_tensor(name, list(shape), dt, kind=kind).ap()

    def sb(self, name, shape, dt):
        self.uid = getattr(self, "uid", 0) + 1
        name = "%s_u%d" % (name, self.uid)
        t = self.stack.enter_context(self.nc.sbuf_tensor(name, list(shape), dt))
        return Buf(t, name)

    def ps(self, name, shape, dt):
        self.uid = getattr(self, "uid", 0) + 1
        name = "%s_u%d" % (name, self.uid)
        t = self.stack.enter_context(self.nc.psum_tensor(name, list(shape), dt))
        b = Buf(t, name)
        b.psum = True
        return b

    def _sem(self, key):
        if key not in self.sems:
            self.sems[key] = self.root.enter_context(self.nc.semaphore("s_%s_%s" % key))
        return self.sems[key]

    def push_scope(self):
        self.scopes.append(self.stack)
        self.stack = ExitStack()

    def pop_scope(self):
        self.barrier()
        self.flush()
        self.stack.close()
        self.stack = self.scopes.pop()

    def _need(self, eng, ev, waits):
        if ev is None:
            return
        kind, src, val = ev
        if kind == "e":
            if src == eng and (eng == "pe" or not SAME_ENG_SYNC):
                return
            if self.seen[eng].get(src, 0) >= val:
                return
            self.seen[eng][src] = val
            ep = (val - 1) // EPOCH
            waits.append((self._sem((src, ep)), val - ep * EPOCH))
        else:
            k = ("d", src)
            if self.seen[eng].get(k, 0) >= val:
                return
            self.seen[eng][k] = val
            waits.append((self.dma_sem_h[src], val))

    def _deps(self, eng, reads, writes):
        waits = []
        for b in reads:
            self._need(eng, b.w, waits)
            if b.psum:
                for k, ev in b.r.items():
                    if k != eng:
                        self._need(eng, ev, waits)
        for b in writes:
            self._need(eng, b.w, waits)
            for ev in b.r.values():
                self._need(eng, ev, waits)
        return waits

    def _mark(self, ev, key, reads, writes):
        for b in reads:
            b.r[key] = ev
        for b in writes:
            b.w = ev
            b.r = {}

    def op(self, eng, fn, reads=(), writes=()):
        reads = [b for b in reads if b is not None]
        writes = [b for b in writes if b is not None]
        waits = self._deps(eng, reads, writes)
        self.count[eng] += 1
        n = self.count[eng]
        ep = (n - 1) // EPOCH
        sem = self._sem((eng, ep))
        self.ops[eng].append((waits, fn, sem, 1))
        self._mark(("e", eng, n), eng, reads, writes)

    def dma(self, q, out, in_, reads=(), writes=(), is_output=False, **kw):
        reads = [b for b in reads if b is not None]
        writes = [b for b in writes if b is not None]
        if not self.dma_sem_h:
            for i in range(3 * N_DMA_SEMS):
                self.dma_sem_h.append(
                    self.root.enter_context(self.nc.semaphore("dsem%d" % i)))
        waits = self._deps(q, reads, writes)
        qbase = {"sp": 0, "act": 1, "pool": 2}[q] * N_DMA_SEMS
        i = qbase + self.dma_rr[q]
        self.dma_rr[q] = (self.dma_rr[q] + 1) % N_DMA_SEMS
        if self.dma_uses[i] > 0:
            self._need(q, ("d", i, 16 * self.dma_uses[i]), waits)
        self.dma_uses[i] += 1
        assert self.dma_uses[i] <= DMA_MAX_USES, "too many DMAs"
        val = 16 * self.dma_uses[i]
        sem = self.dma_sem_h[i]

        def fn(e, out=out, in_=in_, kw=kw):
            return e.dma_start(out=out, in_=in_, **kw)

        self.ops[q].append((waits, fn, sem, 16))
        ev = ("d", i, val)
        self._mark(ev, ("d", i), reads, writes)
        if is_output:
            self.out_events.append(ev)
        return ev

    def collective(self, fn, reads=(), writes=(), inc=16):
        if not self.dma_sem_h:
            for i in range(3 * N_DMA_SEMS):
                self.dma_sem_h.append(
                    self.root.enter_context(self.nc.semaphore("dsem%d" % i)))
        waits = self._deps("pool", list(reads), list(writes))
        sem = self.stack.enter_context(self.nc.semaphore("ccsem%d" % len(self.dma_sem_h)))
        self.dma_sem_h.append(sem)
        self.dma_uses.append(1)
        i = len(self.dma_sem_h) - 1
        self.ops["pool"].append((waits, fn, sem, inc))
        ev = ("d", i, inc)
        self._mark(ev, ("d", i), list(reads), list(writes))
        return ev

    def barrier(self):
        evs = [("e", e, self.count[e]) for e in ENGS if self.count[e] > 0]
        for i, u in enumerate(self.dma_uses):
            if u > 0:
                val = 16 * u if i < 3 * N_DMA_SEMS else self.cc_inc
                evs.append(("d", i, val))
        for eng in ENGS:
            waits = []
            for ev in evs:
                self._need(eng, ev, waits)
            if waits:
                self.ops[eng].append((waits, None, None, 0))

    def flush(self):
        nc = self.nc
        ops = self.ops
        if not any(ops[e] for e in ENGS):
            return

        def run(e, lst):
            for waits, fn, sem, inc in lst:
                for s, v in waits:
                    e.wait_ge(s, v)
                if fn is not None:
                    ins = fn(e)
                    ins.then_inc(sem, inc)

        with nc.Block() as block:
            @block.sync
            def _(e):
                run(e, ops["sp"])

            @block.scalar
            def _(e):
                run(e, ops["act"])

            @block.vector
            def _(e):
                run(e, ops["dve"])

            @block.gpsimd
            def _(e):
                run(e, ops["pool"])

            @block.tensor
            def _(e):
                run(e, ops["pe"])
        self.ops = {e: [] for e in ENGS}

    def build(self):
        for ev in self.out_events:
            w = []
            self._need("sp", ev, w)
            if w:
                self.ops["sp"].append((w, None, None, 0))
        self.flush()
        self.root.close()
        return self.nc

    def mm(self, out_b, out_ap, lhsT_b, lhsT_ap, rhs_b, rhs_ap, start, stop, sgc=False):
        self.op("pe", lambda e: e.matmul(out_ap, lhsT_ap, rhs_ap, start=start, stop=stop,
                                         skip_group_check=sgc),
                reads=[lhsT_b, rhs_b], writes=[out_b])

    def tr(self, out_b, out_ap, in_b, in_ap, id_b, id_ap):
        self.op("pe", lambda e: e.transpose(out_ap, in_ap, id_ap),
                reads=[in_b, id_b], writes=[out_b])

    def act(self, out_b, out_ap, in_b, in_ap, func, bias=None, scale=None, extra_reads=()):
        kw = {}
        if bias is not None:
            kw["bias"] = bias
        if scale is not None:
            kw["scale"] = scale
        self.op("act", lambda e: e.activation(out_ap, in_ap, func, **kw),
                reads=[in_b] + list(extra_reads), writes=[out_b])

    def tt(self, eng, out_b, out_ap, a_b, a_ap, b_b, b_ap, op):
        self.op(eng, lambda e: e.tensor_tensor(out_ap, a_ap, b_ap, op),
                reads=[a_b, b_b], writes=[out_b])

    def ts(self, eng, out_b, out_ap, a_b, a_ap, s1, s2, op0, op1=None, extra_reads=()):
        if op1 is None:
            self.op(eng, lambda e: e.tensor_scalar(out_ap, a_ap, s1, None, op0),
                    reads=[a_b] + list(extra_reads), writes=[out_b])
        else:
            self.op(eng, lambda e: e.tensor_scalar(out_ap, a_ap, s1, s2, op0, op1),
                    reads=[a_b] + list(extra_reads), writes=[out_b])

    def stt(self, out_b, out_ap, a_b, a_ap, scalar, b_b, b_ap, op0, op1, extra_reads=()):
        self.op("dve", lambda e: e.scalar_tensor_tensor(out_ap, a_ap, scalar, b_ap, op0, op1),
                reads=[a_b, b_b] + list(extra_reads), writes=[out_b])

    def copy(self, eng, out_b, out_ap, in_b, in_ap):
        if eng == "act":
            self.op("act", lambda e: e.copy(out_ap, in_ap), reads=[in_b], writes=[out_b])
        else:
            self.op(eng, lambda e: e.tensor_copy(out_ap, in_ap), reads=[in_b], writes=[out_b])

    def memset(self, eng, out_b, out_ap, val):
        self.op(eng, lambda e: e.memset(out_ap, val), reads=[], writes=[out_b])


def run_prog(nc, in_maps, n_cores=8, trace=False):
    res = run_bass_kernel_spmd(nc, in_maps, core_ids=list(range(n_cores)), trace=trace)
    return res


D = 1024
IN_COLS = 8608
C_SCB, C_SCC, C_SCH = 0, 512, 1024
C_CFV, C_CFG = 1536, 2048
C_CQ, C_CKV, C_KR = 2560, 2816, 2944
C_SBQ, C_SBK, C_SBV = 2976, 3488, 4000
C_GATE = 4512
NA = 4512
MLA_SCALE = 96 ** -0.5
SB_SCALE = 64 ** -0.5
TWO_PI = 2.0 * np.pi
CW1 = 6.28125
CW2 = TWO_PI - 6.28125
MAGIC = 12582912.0
PI_CL = 3.141592
DN_ALPHA = 4 ** 0.25
LN_EPS = 1e-5
RMS_EPS = 1e-6


class Banks:
    def __init__(self, P, n, prefix="pb"):
        self.b = [P.ps("%s%d" % (prefix, i), [128, 512], F32) for i in range(n)]
        self.i = 0

    def next(self):
        b = self.b[self.i]
        self.i = (self.i + 1) % len(self.b)
        return b


def build_phase_a(T=2048, P=None, io=None):
    standalone = P is None
    if standalone:
        P = Prog()
    P.push_scope()

    def D_(name, shape, dt, kind):
        return io[name] if io is not None else P.dram(name, shape, dt, kind)
    NT = T // 512
    x = D_("x", [T, D], F32, "ExternalInput")
    posr = D_("posr", [32, T], I32, "ExternalInput")
    w_in = D_("w_in", [D, IN_COLS], F32, "ExternalInput")
    w_uq = D_("w_uq", [256, 768], F32, "ExternalInput")
    w_ukv = D_("w_ukv", [128, 1024], F32, "ExternalInput")
    qn = D_("qn", [128, 2], F32, "ExternalInput")
    kvn = D_("kvn", [128, 1], F32, "ExternalInput")
    ident_d = D_("ident", [128, 128], F32, "ExternalInput")
    invf_d = D_("invf", [128, 1], F32, "ExternalInput")

    xT_o = D_("xT_o", [D, T], BF16, "ExternalOutput")
    scb_o = D_("scb_o", [512, T], BF16, "ExternalOutput")
    scch_o = D_("scch_o", [512, T], BF16, "ExternalOutput")
    cfu_o = D_("cfu_o", [512, T], BF16, "ExternalOutput")
    mq_o = D_("mq_o", [8, 96, T], BF16, "ExternalOutput")
    mk_o = D_("mk_o", [8, 96, T], BF16, "ExternalOutput")
    mv_o = D_("mv_o", [T, 512], BF16, "ExternalOutput")
    sq_o = D_("sq_o", [512, T], BF16, "ExternalOutput")
    sk_o = D_("sk_o", [512, T], BF16, "ExternalOutput")
    sv_o = D_("sv_o", [T, 512], BF16, "ExternalOutput")

    identb = P.sb("identb", [128, 128], BF16)
    P.dma("pool", identb[:, :], ident_d[:, :], writes=[identb])
    invf = P.sb("invf_s", [128, 1], F32)
    P.dma("sp", invf[:, :], invf_d[:, :], writes=[invf])
    qn_s = P.sb("qn_s", [128, 2], F32)
    P.dma("sp", qn_s[:, :], qn[:, :], writes=[qn_s])
    kvn_s = P.sb("kvn_s", [128, 1], F32)
    P.dma("sp", kvn_s[:, :], kvn[:, :], writes=[kvn_s])
    onesb = P.sb("onesb", [128, 128], BF16)
    P.memset("dve", onesb, onesb[:, :], 1.0)
    epsr = P.sb("epsr", [128, 1], F32)
    P.memset("dve", epsr, epsr[:, :], RMS_EPS)

    wb = P.sb("wb", [128, 8, NA], BF16)
    for kc in range(8):
        P.dma("pool", wb[:, kc, :], w_in[kc * 128:(kc + 1) * 128, 0:NA], writes=[wb])
    wq = P.sb("wq", [128, 2, 768], BF16)
    for j in range(2):
        P.dma("pool", wq[:, j, :], w_uq[j * 128:(j + 1) * 128, :], writes=[wq])
    wkv = P.sb("wkv", [128, 1024], BF16)
    P.dma("pool", wkv[:, :], w_ukv[:, :], writes=[wkv])
    wq_rh = P.sb("wq_rh", [128, 2, 8, 96], BF16)
    P.memset("pool", wq_rh, wq_rh[:, :, :, :], 0.0)
    wq4 = wq.t[:, :, :].rearrange("p j (h c) -> p j h c", c=96)
    for j in range(2):
        P.ts("dve", wq_rh, wq_rh[:, j, :, 64:80], wq, wq4[:, j, :, 80:96], -1.0, None, ALU.mult)
        P.copy("dve", wq_rh, wq_rh[:, j, :, 80:96], wq, wq4[:, j, :, 64:80])
    wkr = P.sb("wkr", [128, 8, 96], BF16)
    wkr_rh = P.sb("wkr_rh", [128, 8, 96], BF16)
    P.memset("pool", wkr, wkr[:, :, :], 0.0)
    P.memset("pool", wkr_rh, wkr_rh[:, :, :], 0.0)
    P.copy("dve", wkr, wkr[:, :, 64:96], wb, wb[:, :, C_KR:C_KR + 32])
    P.ts("dve", wkr_rh, wkr_rh[:, :, 64:80], wb, wb[:, :, C_KR + 16:C_KR + 32], -1.0, None, ALU.mult)
    P.copy("dve", wkr_rh, wkr_rh[:, :, 80:96], wb, wb[:, :, C_KR:C_KR + 16])

    posi = P.sb("posi", [128, 512], I32)
    ang = P.sb("ang", [128, 512], F32)
    kk = P.sb("kk", [128, 512], F32)
    rr = P.sb("rr", [128, 512], F32)
    sin2 = P.sb("sin2", [128, 512], F32)
    cos2 = P.sb("cos2", [128, 512], F32)
    R = slice(64, 96)

    def rope_tables(t0):
        P.dma("sp", posi[64:96, :], posr[:, t0:t0 + 512], writes=[posi])
        P.copy("dve", ang, ang[R, :], posi, posi[R, :])
        P.ts("dve", ang, ang[R, :], ang, ang[R, :], invf[R, 0:1], None, ALU.mult, extra_reads=[invf])
        P.ts("dve", kk, kk[R, :], ang, ang[R, :], 1.0 / TWO_PI, MAGIC, ALU.mult, ALU.add)
        P.ts("dve", kk, kk[R, :], kk, kk[R, :], -MAGIC, None, ALU.add)
        P.stt(rr, rr[R, :], kk, kk[R, :], -CW1, ang, ang[R, :], ALU.mult, ALU.add)
        P.stt(rr, rr[R, :], kk, kk[R, :], -CW2, rr, rr[R, :], ALU.mult, ALU.add)
        P.ts("dve", rr, rr[R, :], rr, rr[R, :], PI_CL, -PI_CL, ALU.min, ALU.max)
        P.act(sin2, sin2[R, :], rr, rr[R, :], AF.Sin)
        P.ts("dve", kk, kk[R, :], rr, rr[R, :], np.pi / 2, None, ALU.is_gt)
        P.stt(ang, ang[R, :], kk, kk[R, :], -TWO_PI, rr, rr[R, :], ALU.mult, ALU.add)
        P.ts("dve", ang, ang[R, :], ang, ang[R, :], np.pi / 2, PI_CL, ALU.add, ALU.min)
        P.ts("dve", ang, ang[R, :], ang, ang[R, :], -PI_CL, None, ALU.max)
        P.act(cos2, cos2[R, :], ang, ang[R, :], AF.Sin)

    pT = [P.ps("pT%d" % i, [128, 1024], BF16) for i in range(2)]
    banks = Banks(P, 6)

    xs = [P.sb("xs%d" % i, [128, 4, D], F32) for i in range(1)] * 2
    xbf = [P.sb("xbf%d" % i, [128, 4, D], BF16) for i in range(1)] * 2
    xT = [P.sb("xT%d" % i, [128, 8, 512], BF16) for i in range(1)] * 2
    st_b = [P.sb("st_b%d" % i, [128, 4, 512], BF16) for i in range(1)] * 2
    st_ch = [P.sb("st_ch%d" % i, [128, 4, 512], BF16) for i in range(1)] * 2
    st_u = [P.sb("st_u%d" % i, [128, 4, 512], BF16) for i in range(1)] * 2
    st_sq = [P.sb("st_sq%d" % i, [128, 4, 512], BF16) for i in range(1)] * 2
    st_sk = [P.sb("st_sk%d" % i, [128, 4, 512], BF16) for i in range(1)] * 2
    st_sv = [P.sb("st_sv%d" % i, [128, 4, 512], BF16) for i in range(1)] * 2
    st_mv = [P.sb("st_mv%d" % i, [128, 4, 512], BF16) for i in range(1)] * 2
    st_mq = [P.sb("st_mq%d" % i, [96, 8, 512], BF16) for i in range(1)] * 2
    st_mk = [P.sb("st_mk%d" % i, [96, 8, 512], BF16) for i in range(1)] * 2
    tmp_c = [P.sb("tmp_c%d" % i, [128, 512], F32) for i in range(3)]
    cq2 = P.sb("cq2", [128, 2, 512], BF16)
    cqg = P.sb("cqg", [128, 2, 512], BF16)
    ckv2 = P.sb("ckv2", [128, 512], BF16)
    ckvg = P.sb("ckvg", [128, 512], BF16)
    rq = P.sb("rq", [128, 512], F32)
    rkv = P.sb("rkv", [128, 512], F32)
    rkv_tm = P.sb("rkv_tm", [128, 4], F32)
    rkv_t0 = P.sb("rkv_t0", [128, 4], F32)
    rtmp = P.sb("rtmp", [128, 512], F32)
    rt1 = [P.sb("rt1_%d" % i, [128, 512], F32) for i in range(2)]
    rt2 = [P.sb("rt2_%d" % i, [128, 512], F32) for i in range(2)]
    tci = [0]

    def fm_chunk(tt, col0, M=128, w_b=None, w_ap_fn=None):
        b = banks.next()
        for kc in range(8):
            if w_ap_fn is None:
                lw = wb[:, kc, col0:col0 + M]
                lb = wb
            else:
                lw = w_ap_fn(kc)
                lb = w_b
            P.mm(b, b[0:M, :], lb, lw, xT[tt % 2], xT[tt % 2][:, kc, :], kc == 0, kc == 7)
        return b

    for tt in range(NT):
        par = tt % 2
        t0 = tt * 512
        xsb, xbb, xTb = xs[par], xbf[par], xT[par]
        rope_tables(t0)
        P.dma("sp", xsb[:, :, :], x[t0:t0 + 512, :].rearrange("(s p) d -> p s d", p=128), writes=[xsb])
        for s in range(4):
            P.copy("pool" if s % 2 else "act", xbb, xbb[:, s, :], xsb, xsb[:, s, :])
        for kc in range(8):
            pt = pT[kc % 2]
            for s in range(4):
                P.tr(pt, pt[:, s * 128:(s + 1) * 128], xbb, xbb[:, s, kc * 128:(kc + 1) * 128], identb, identb[:, :])
            P.copy("dve" if kc % 2 else "act", xTb, xTb[:, kc, :], pt, pt[:, 0:512])
        P.dma("sp", xT_o[:, t0:t0 + 512].rearrange("(k p) t -> p k t", p=128), xTb[:, :, :], reads=[xTb], is_output=True)

        for j in range(2):
            b = fm_chunk(tt, C_CQ + j * 128)
            P.act(cq2, cq2[:, j, :], b, b[:, :], AF.Square)
            P.ts("dve", cqg, cqg[:, j, :], b, b[:, :], qn_s[:, j:j + 1], None, ALU.mult, extra_reads=[qn_s])
        b = banks.next()
        for j in range(2):
            P.mm(b, b[:, :], onesb, onesb[:, :], cq2, cq2[:, j, :], j == 0, j == 1)
        P.act(rtmp, rtmp[:, :], b, b[:, :], AF.Sqrt, bias=epsr[:, 0:1], scale=1.0 / 256, extra_reads=[epsr])
        P.op("dve", lambda e: e.reciprocal(rq[:, :], rtmp[:, :]), reads=[rtmp], writes=[rq])
        b = fm_chunk(tt, C_CKV)
        P.act(ckv2, ckv2[:, :], b, b[:, :], AF.Square)
        P.ts("dve", ckvg, ckvg[:, :], b, b[:, :], kvn_s[:, 0:1], None, ALU.mult, extra_reads=[kvn_s])
        b = banks.next()
        P.mm(b, b[:, :], onesb, onesb[:, :], ckv2, ckv2[:, :], True, True)
        P.act(rtmp, rtmp[:, :], b, b[:, :], AF.Sqrt, bias=epsr[:, 0:1], scale=1.0 / 128, extra_reads=[epsr])
        P.op("dve", lambda e: e.reciprocal(rkv[:, :], rtmp[:, :]), reads=[rtmp], writes=[rkv])
        b = banks.next()
        for s in range(4):
            P.mm(b, b[:, s * 128:(s + 1) * 128], ckv2, ckv2[:, s * 128:(s + 1) * 128], onesb, onesb[:, :], True, True)
        P.act(rkv_t0, rkv_t0[:, :], b, b[:, :].rearrange("p (s c) -> p s c", c=128)[:, :, 0], AF.Sqrt, bias=epsr[:, 0:1], scale=1.0 / 128, extra_reads=[epsr])
        P.op("dve", lambda e: e.reciprocal(rkv_tm[:, :], rkv_t0[:, :]), reads=[rkv_t0], writes=[rkv_tm])
        for j in range(4):
            b = fm_chunk(tt, C_SCB + j * 128)
            P.copy("act", st_b[par], st_b[par][:, j, :], b, b[:, :])
            bc = fm_chunk(tt, C_SCC + j * 128)
            tc = tmp_c[tci[0] % 3]; tci[0] += 1
            P.copy("act", tc, tc[:, :], bc, bc[:, :])
            bh = fm_chunk(tt, C_SCH + j * 128)
            P.tt("dve", st_ch[par], st_ch[par][:, j, :], bh, bh[:, :], tc, tc[:, :], ALU.mult)
        P.dma("sp", scb_o[:, t0:t0 + 512].rearrange("(j p) t -> p j t", p=128), st_b[par][:, :, :], reads=[st_b[par]], is_output=True)
        P.dma("sp", scch_o[:, t0:t0 + 512].rearrange("(j p) t -> p j t", p=128), st_ch[par][:, :, :], reads=[st_ch[par]], is_output=True)
        for j in range(4):
            bg = fm_chunk(tt, C_CFG + j * 128)
            tc = tmp_c[tci[0] % 3]; tci[0] += 1
            P.act(tc, tc[:, :], bg, bg[:, :], AF.Sigmoid)
            bv = fm_chunk(tt, C_CFV + j * 128)
            P.tt("dve", st_u[par], st_u[par][:, j, :], bv, bv[:, :], tc, tc[:, :], ALU.mult)
        P.dma("sp", cfu_o[:, t0:t0 + 512].rearrange("(j p) t -> p j t", p=128), st_u[par][:, :, :], reads=[st_u[par]], is_output=True)
        for j in range(4):
            b = fm_chunk(tt, C_SBQ + j * 128)
            P.act(st_sq[par], st_sq[par][:, j, :], b, b[:, :], AF.Copy, scale=SB_SCALE)
            b = fm_chunk(tt, C_SBK + j * 128)
            P.copy("dve", st_sk[par], st_sk[par][:, j, :], b, b[:, :])
        P.dma("sp", sq_o[:, t0:t0 + 512].rearrange("(j p) t -> p j t", p=128), st_sq[par][:, :, :], reads=[st_sq[par]], is_output=True)
        P.dma("sp", sk_o[:, t0:t0 + 512].rearrange("(j p) t -> p j t", p=128), st_sk[par][:, :, :], reads=[st_sk[par]], is_output=True)
        for s in range(4):
            b = banks.next()
            for kc in range(8):
                P.mm(b, b[:, :], xTb, xTb[:, kc, s * 128:(s + 1) * 128], wb, wb[:, kc, C_SBV:C_SBV + 512], kc == 0, kc == 7)
            P.copy("act" if s % 2 else "dve", st_sv[par], st_sv[par][:, s, :], b, b[:, :])
        P.dma("sp", sv_o[t0:t0 + 512, :].rearrange("(s p) c -> p s c", p=128), st_sv[par][:, :, :], reads=[st_sv[par]], is_output=True)

        for h in range(8):
            bq = banks.next()
            for j in range(2):
                P.mm(bq, bq[0:96, :], wq, wq[:, j, h * 96:(h + 1) * 96], cqg, cqg[:, j, :], j == 0, j == 1)
            br = banks.next()
            for j in range(2):
                P.mm(br, br[0:96, :], wq_rh, wq_rh[:, j, h, :], cqg, cqg[:, j, :], j == 0, j == 1)
            P.stt(st_mq[par], st_mq[par][0:64, h, :], bq, bq[0:64, :], MLA_SCALE, rq, rq[0:64, :], ALU.mult, ALU.mult)
            a1, a2 = rt1[h % 2], rt2[h % 2]
            P.tt("dve", a1, a1[R, :], bq, bq[R, :], cos2, cos2[R, :], ALU.mult)
            P.tt("dve", a2, a2[R, :], br, br[R, :], sin2, sin2[R, :], ALU.mult)
            P.tt("pool", a1, a1[R, :], a1, a1[R, :], a2, a2[R, :], ALU.add)
            P.stt(st_mq[par], st_mq[par][R, h, :], a1, a1[R, :], MLA_SCALE, rq, rq[R, :], ALU.mult, ALU.mult)
        P.dma("sp", mq_o[:, :, t0:t0 + 512].rearrange("h p t -> p h t"), st_mq[par][:, :, :], reads=[st_mq[par]], is_output=True)
        bk = fm_chunk(tt, 0, M=96, w_b=wkr, w_ap_fn=lambda kc: wkr[:, kc, :])
        bkr = fm_chunk(tt, 0, M=96, w_b=wkr_rh, w_ap_fn=lambda kc: wkr_rh[:, kc, :])
        a1, a2 = rt1[0], rt2[0]
        P.tt("dve", a1, a1[R, :], bk, bk[R, :], cos2, cos2[R, :], ALU.mult)
        P.tt("dve", a2, a2[R, :], bkr, bkr[R, :], sin2, sin2[R, :], ALU.mult)
        for h in range(8):
            P.tt("pool" if h % 2 else "dve", st_mk[par], st_mk[par][R, h, :], a1, a1[R, :], a2, a2[R, :], ALU.add)
        for h in range(8):
            b = banks.next()
            P.mm(b, b[0:64, :], wkv, wkv[:, h * 128:h * 128 + 64], ckvg, ckvg[:, :], True, True)
            P.tt("dve", st_mk[par], st_mk[par][0:64, h, :], b, b[0:64, :], rkv, rkv[0:64, :], ALU.mult)
        P.dma("sp", mk_o[:, :, t0:t0 + 512].rearrange("h p t -> p h t"), st_mk[par][:, :, :], reads=[st_mk[par]], is_output=True)
        wkv_v = wkv.t[:, :].rearrange("p (h c) -> p h c", c=128)[:, :, 64:128]
        for s in range(4):
            b = banks.next()
            P.mm(b, b[:, :], ckvg, ckvg[:, s * 128:(s + 1) * 128], wkv, wkv_v, True, True)
            P.ts("dve", st_mv[par], st_mv[par][:, s, :], b, b[:, :], rkv_tm[:, s:s + 1], None, ALU.mult, extra_reads=[rkv_tm])
        P.dma("sp", mv_o[t0:t0 + 512, :].rearrange("(s p) c -> p s c", p=128), st_mv[par][:, :, :], reads=[st_mv[par]], is_output=True)
    P.pop_scope()
    if standalone:
        return P.build()
    return None


def run_pipeline(items, stages):
    n = len(items)
    d = len(stages)
    for step in range(n + d - 1):
        for si, st in enumerate(stages):
            k = step - si
            if 0 <= k < n:
                st(k, items[k])


def build_phase_b(S=4096, NH=4, CT=2048, do_conv=True, do_mla=True, do_sb=True, P=None, io=None):
    standalone = P is None
    if standalone:
        P = Prog()
    P.push_scope()

    def D_(name, shape, dt, kind):
        return io[name] if io is not None else P.dram(name, shape, dt, kind)
    HW = NH * 64
    NB = S // 128
    NQC = S // 512
    scb = D_("scb", [512, CT], BF16, "ExternalInput")
    scch = D_("scch", [512, CT + 2], BF16, "ExternalInput")
    cfu = D_("cfu", [512, CT + 30], BF16, "ExternalInput")
    sc_w = D_("sc_w", [128, 4, 3], F32, "ExternalInput")
    cf_w = D_("cf_w", [128, 4, 31], F32, "ExternalInput")
    cf_p = D_("cf_p", [128, 3, 4], F32, "ExternalInput")
    mq = D_("mq", [NH, 96, S], BF16, "ExternalInput")
    mk = D_("mk", [NH, 96, S], BF16, "ExternalInput")
    mv = D_("mv", [S, HW], BF16, "ExternalInput")
    sq = D_("sq", [HW, S], BF16, "ExternalInput")
    sk = D_("sk", [HW, S], BF16, "ExternalInput")
    sv = D_("sv", [S, HW], BF16, "ExternalInput")
    cst = D_("cst", [128, 4, 128], F32, "ExternalInput")
    br0 = D_("br0", [512, CT], BF16, "ExternalOutput")
    br1 = D_("br1", [512, CT], BF16, "ExternalOutput")
    mo = D_("mo", [HW, S], BF16, "ExternalOutput")
    so = D_("so", [HW, S], BF16, "ExternalOutput")

    pb = [P.ps("pb%d" % i, [128, 512], F32) for i in range(8)]
    cstf = P.sb("cstf", [128, 4, 128], F32)
    P.dma("sp", cstf[:, :, :], cst[:, :, :], writes=[cstf])
    cstb = P.sb("cstb", [128, 4, 128], BF16)
    P.copy("dve", cstb, cstb[:, :, :], cstf, cstf[:, :, :])
    onesb = P.sb("onesb", [128, 128], BF16)
    P.memset("dve", onesb, onesb[:, :], 1.0)
    onesf = P.sb("onesf", [128, 128], F32)
    P.memset("dve", onesf, onesf[:, :], 1.0)
    avgf = P.sb("avgf", [128, 128], F32)
    P.memset("dve", avgf, avgf[:, :], 1.0 / 512)
    epsl = P.sb("epsl", [128, 1], F32)
    P.memset("dve", epsl, epsl[:, :], LN_EPS)
    mask_sb16 = cstb[:, 1, :]
    negtri16 = cstb[:, 3, :]

    if do_conv:
        scw = P.sb("scw", [128, 4, 3], F32)
        P.dma("sp", scw[:, :, :], sc_w[:, :, :], writes=[scw])
        cfw = P.sb("cfw", [128, 4, 31], F32)
        P.dma("sp", cfw[:, :, :], cf_w[:, :, :], writes=[cfw])
        cfp = P.sb("cfp", [128, 3, 4], F32)
        P.dma("sp", cfp[:, :, :], cf_p[:, :, :], writes=[cfp])
        diag = P.sb("diag", [128, 4, 31, 128], BF16)
        dsubs = []
        for j in range(4):
            for l in range(31):
                dsub = diag.sub()
                dsubs.append(dsub)
                P.ts("dve" if (l % 4) else "pool", dsub, diag[:, j, l, :], cstf, cstf[:, 0, :], cfw[:, j, l:l + 1], None,
                     ALU.mult, extra_reads=[cfw])
        djoin = P.sb("djoin", [128, 1], F32)
        P.op("dve", lambda e: e.memset(djoin[:, :], 0.0), reads=dsubs, writes=[djoin, diag])
        chs = P.sb("chs", [128, 4, 514], BF16)
        bs = P.sb("bs", [128, 4, 512], BF16)
        us = P.sb("us", [128, 4, 542], BF16)
        acc = [P.sb("acc%d" % i, [128, 512], F32) for i in range(2)]
        o0 = P.sb("o0", [128, 4, 512], BF16)
        o1 = P.sb("o1", [128, 4, 512], BF16)
        vt = P.sb("vt", [128, 4, 512], F32)
        v2 = P.sb("v2", [128, 4, 512], F32)
        mean_s = P.sb("mean_s", [128, 512], F32)
        m2 = P.sb("m2", [128, 512], F32)
        var_s = P.sb("var_s", [128, 512], F32)
        rstd = P.sb("rstd", [128, 512], F32)
        xn = [P.sb("xn%d" % i, [128, 512], F32) for i in range(2)]
        for tt in range(CT // 512):
            t0 = tt * 512
            P.dma("sp", chs[:, :, :], scch[:, t0:t0 + 514].rearrange("(j p) t -> p j t", p=128), writes=[chs])
            P.dma("sp", bs[:, :, :], scb[:, t0:t0 + 512].rearrange("(j p) t -> p j t", p=128), writes=[bs])
            P.dma("act", us[:, :, :], cfu[:, t0:t0 + 542].rearrange("(j p) t -> p j t", p=128), writes=[us])
            for j in range(4):
                a = acc[j % 2]
                P.ts("dve", a, a[:, :], chs, chs[:, j, 0:512], scw[:, j, 0:1], None, ALU.mult, extra_reads=[scw])
                P.stt(a, a[:, :], chs, chs[:, j, 1:513], scw[:, j, 1:2], a, a[:, :], ALU.mult, ALU.add, extra_reads=[scw])
                P.stt(a, a[:, :], chs, chs[:, j, 2:514], scw[:, j, 2:3], a, a[:, :], ALU.mult, ALU.add, extra_reads=[scw])
                P.tt("pool", o0, o0[:, j, :], a, a[:, :], bs, bs[:, j, :], ALU.mult)
            P.dma("sp", br0[:, t0:t0 + 512].rearrange("(j p) t -> p j t", p=128), o0[:, :, :], reads=[o0], is_output=True)
            for j in range(4):
                b = pb[j % 3]
                for l in range(31):
                    P.mm(b, b[:, :], diag, diag[:, j, l, :], us, us[:, j, l:l + 512], l == 0, l == 30)
                P.act(vt, vt[:, j, :], b, b[:, :], AF.Identity, bias=cfp[:, 0, j:j + 1], extra_reads=[cfp])
                P.act(v2, v2[:, j, :], vt, vt[:, j, :], AF.Square)
            bm, bq = pb[3], pb[4]
            for j in range(4):
                P.mm(bm, bm[:, :], avgf, avgf[:, :], vt, vt[:, j, :], j == 0, j == 3)
            for j in range(4):
                P.mm(bq, bq[:, :], avgf, avgf[:, :], v2, v2[:, j, :], j == 0, j == 3)
            P.copy("act", mean_s, mean_s[:, :], bm, bm[:, :])
            P.tt("pool", m2, m2[:, :], mean_s, mean_s[:, :], mean_s, mean_s[:, :], ALU.mult)
            P.tt("dve", var_s, var_s[:, :], bq, bq[:, :], m2, m2[:, :], ALU.subtract)
            P.act(var_s, var_s[:, :], var_s, var_s[:, :], AF.Sqrt, bias=epsl[:, 0:1], extra_reads=[epsl])
            P.op("dve", lambda e: e.reciprocal(rstd[:, :], var_s[:, :]), reads=[var_s], writes=[rstd])
            for j in range(4):
                xx = xn[j % 2]
                P.tt("pool", xx, xx[:, :], vt, vt[:, j, :], mean_s, mean_s[:, :], ALU.subtract)
                P.tt("dve", xx, xx[:, :], xx, xx[:, :], rstd, rstd[:, :], ALU.mult)
                P.act(o1, o1[:, j, :], xx, xx[:, :], AF.Silu, bias=cfp[:, 2, j:j + 1], scale=cfp[:, 1, j:j + 1], extra_reads=[cfp])
            P.dma("sp", br1[:, t0:t0 + 512].rearrange("(j p) t -> p j t", p=128), o1[:, :, :], reads=[o1], is_output=True)

    if do_mla:
        kT = [P.sb("kT%d" % i, [96, S], BF16) for i in range(1)] * 2
        qT = [P.sb("qT%d" % i, [96, S], BF16) for i in range(1)] * 2
        vA = [P.sb("vA%d" % i, [128, NB, 65], BF16) for i in range(2)]
        pT = [P.sb("pTm%d" % i, [128, 512], BF16) for i in range(3)]
        dsb = P.sb("dsb", [128, 512], F32)
        rsb = P.sb("rsb", [128, 512], F32)
        bcs = P.sb("bcs", [64, 512], F32)
        ost = [P.sb("ost%d" % i, [64, 512], BF16) for i in range(2)]
        items = []
        for h in range(NH):
            for qc in range(NQC):
                nk = 4 * (qc + 1)
                for kb in range(nk):
                    items.append((h, qc, kb, nk))
        loaded = set()

        def load_head(h):
            if h in loaded or h >= NH:
                return
            loaded.add(h)
            P.dma("sp", kT[h % 2][:, :], mk[h, :, :], writes=[kT[h % 2]])
            P.dma("act", qT[h % 2][:, :], mq[h, :, :], writes=[qT[h % 2]])
            P.memset("pool", vA[h % 2], vA[h % 2][:, :, :], 1.0)
            P.dma("sp", vA[h % 2][:, :, 0:64], mv[:, h * 64:(h + 1) * 64].rearrange("(b p) c -> p b c", p=128), writes=[vA[h % 2]])

        def gidx(h, qc):
            return h * NQC + qc

        def m_s1(k, it):
            h, qc, kb, nk = it
            load_head(h)
            c0 = max(0, kb - 4 * qc) * 128
            Sb = pb[k % 3]
            P.mm(Sb, Sb[:, c0:512], kT[h % 2], kT[h % 2][:, kb * 128:(kb + 1) * 128], qT[h % 2], qT[h % 2][:, qc * 512 + c0:qc * 512 + 512], True, True)
            pt = pT[k % 3]
            P.act(pt, pt[:, c0:512], Sb, Sb[:, c0:512], AF.Exp)
            if kb >= 4 * qc:
                P.memset("pool", pt, pt[64:128, c0:c0 + 64], 0.0)

        def m_s2(k, it):
            h, qc, kb, nk = it
            c0 = max(0, kb - 4 * qc) * 128
            O = pb[3 + gidx(h, qc) % 2]
            pt = pT[k % 3]
            P.mm(O, O[0:65, c0:512], vA[h % 2], vA[h % 2][:, kb, :], pt, pt[:, c0:512], kb == 0, True, sgc=(kb > 0))
            if kb == nk - 1:
                g = gidx(h, qc)
                P.copy("act", dsb, dsb[64:65, :], O, O[64:65, :])
                P.op("dve", lambda e: e.reciprocal(rsb[64:65, :], dsb[64:65, :]), reads=[dsb], writes=[rsb])
                bc = pb[5]
                P.mm(bc, bc[0:64, :], onesf, onesf[64:65, 0:64], rsb, rsb[64:65, :], True, True)
                P.copy("act", bcs, bcs[:, :], bc, bc[0:64, :])
                os_ = ost[g % 2]
                P.tt("dve", os_, os_[:, :], O, O[0:64, :], bcs, bcs[:, :], ALU.mult)
                P.dma("sp", mo[h * 64:(h + 1) * 64, qc * 512:(qc + 1) * 512], os_[:, :], reads=[os_], is_output=True)

        run_pipeline(items, [m_s1, m_s2])

    if do_sb:
        GH = min(NH, 4)
        NP_ = GH // 2
        skT = [P.sb("skT%d" % i, [128, S], BF16) for i in range(NP_)]
        sqT = [P.sb("sqT%d" % i, [128, S], BF16) for i in range(NP_)]
        svt = [P.sb("svt%d" % i, [128, NB, 64], BF16) for i in range(GH)]
        e_t = [P.sb("e_t%d" % i, [128, 512], F32) for i in range(3)]
        lp16 = [P.sb("lp16_%d" % i, [128, 512], BF16) for i in range(3)]
        t_t = [P.sb("t_t%d" % i, [128, 512], F32) for i in range(3)]
        a16 = [P.sb("a16_%d" % i, [128, 512], BF16) for i in range(3)]
        cacc = [P.sb("cacc%d" % i, [128, 512], F32) for i in range(2)]
        sst = [P.sb("sst%d" % i, [64, 512], BF16) for i in range(2)]
        hb_ = [0]

        def gidx2(h, qc):
            return h * NQC + qc

        def s_s1(k, it):
            h, qc, kb, nk = it
            c0 = max(0, kb - 4 * qc) * 128
            g = gidx2(h, qc)
            if kb == nk - 1:
                P.memset("pool", cacc[g % 2], cacc[g % 2][:, :], 0.0)
            Zb = pb[k % 3]
            hp = slice((h % 2) * 64, (h % 2) * 64 + 64)
            P.mm(Zb, Zb[:, c0:512], skT[h // 2], skT[h // 2][hp, kb * 128:(kb + 1) * 128], sqT[h // 2], sqT[h // 2][hp, qc * 512 + c0:qc * 512 + 512], True, True)
            P.act(e_t[k % 3], e_t[k % 3][:, c0:512], Zb, Zb[:, c0:512], AF.Exp)
            P.act(lp16[k % 3], lp16[k % 3][:, c0:512], e_t[k % 3], e_t[k % 3][:, c0:512], AF.Ln, bias=1.0)
            if kb >= 4 * qc:
                P.tt("pool", lp16[k % 3], lp16[k % 3][:, c0:c0 + 128], lp16[k % 3], lp16[k % 3][:, c0:c0 + 128], cstb, mask_sb16, ALU.mult)

        def s_s2(k, it):
            h, qc, kb, nk = it
            c0 = max(0, kb - 4 * qc) * 128
            g = gidx2(h, qc)
            Zb = pb[k % 3]
            Cb = pb[5 + k % 2]
            L = lp16[k % 3]
            P.mm(Zb, Zb[:, c0:512], cstb, negtri16, L, L[:, c0:512], False, True, sgc=True)
            P.mm(Cb, Cb[:, c0:512], onesb, onesb[:, :], L, L[:, c0:512], True, True)
            ca = cacc[g % 2]
            P.tt("dve", t_t[k % 3], t_t[k % 3][:, c0:512], Zb, Zb[:, c0:512], ca, ca[:, c0:512], ALU.subtract)
            P.act(a16[k % 3], a16[k % 3][:, c0:512], t_t[k % 3], t_t[k % 3][:, c0:512], AF.Exp)
            if kb >= 4 * qc:
                P.tt("pool", a16[k % 3], a16[k % 3][:, c0:c0 + 128], a16[k % 3], a16[k % 3][:, c0:c0 + 128], cstb, mask_sb16, ALU.mult)
            if kb > 0:
                P.tt("dve", ca, ca[:, c0:512], Cb, Cb[:, c0:512], ca, ca[:, c0:512], ALU.add)

        def s_s3(k, it):
            h, qc, kb, nk = it
            c0 = max(0, kb - 4 * qc) * 128
            g = gidx2(h, qc)
            O = pb[3 + g % 2]
            P.mm(O, O[0:64, c0:512], svt[h], svt[h][:, kb, :], a16[k % 3], a16[k % 3][:, c0:512], kb == nk - 1, True, sgc=(kb < nk - 1))
            if kb == 0:
                os_ = sst[g % 2]
                P.copy("act", os_, os_[:, :], O, O[0:64, :])
                hg_ = hb_[0] + h
                P.dma("sp", so[hg_ * 64:(hg_ + 1) * 64, qc * 512:(qc + 1) * 512], os_[:, :], reads=[os_], is_output=True)

        for hg in range(NH // GH):
            hb_[0] = hg * GH
            for pr in range(NP_):
                r0 = (hg * NP_ + pr) * 128
                P.dma("sp", skT[pr][:, :], sk[r0:r0 + 128, :], writes=[skT[pr]])
                P.dma("act", sqT[pr][:, :], sq[r0:r0 + 128, :], writes=[sqT[pr]])
            for h in range(GH):
                hg_ = hg * GH + h
                P.dma("sp", svt[h][:, :, :], sv[:, hg_ * 64:(hg_ + 1) * 64].rearrange("(b p) c -> p b c", p=128), writes=[svt[h]])
            items = []
            for h in range(GH):
                for qc in range(NQC):
                    nk = 4 * (qc + 1)
                    for kb in range(nk - 1, -1, -1):
                        items.append((h, qc, kb, nk))
            run_pipeline(items, [s_s1, s_s2, s_s3])
    P.pop_scope()
    if standalone:
        return P.build()
    return None


def build_phase_c(T=2048, E=1, P=None, io=None):
    moe = E > 1
    standalone = P is None
    if standalone:
        P = Prog()
    P.push_scope()

    def D_(name, shape, dt, kind):
        return io[name] if io is not None else P.dram(name, shape, dt, kind)
    NT = T // 512
    NS = T // 128
    x = D_("x", [T, D], F32, "ExternalInput")
    xT_d = D_("xT", [D, T], BF16, "ExternalInput")
    brs = D_("brs", [4, 512, T], BF16, "ExternalInput")
    w_in = D_("w_in", [D, IN_COLS], F32, "ExternalInput")
    bgc_d = D_("bgc", [128, 32], F32, "ExternalInput")
    w_br = D_("w_br", [4, 512, D], F32, "ExternalInput")
    w_o = D_("w_o", [D, D], F32, "ExternalInput")
    lnp = D_("lnp", [4, D], F32, "ExternalInput")
    f_g = D_("f_g", [E, D, 3584], F32, "ExternalInput")
    f_u = D_("f_u", [E, D, 3584], F32, "ExternalInput")
    f_d = D_("f_d", [E, 3584, D], F32, "ExternalInput")
    wr_d = D_("wr", [128, 8, 8], F32, "ExternalInput")
    ident_d = D_("ident", [128, 128], F32, "ExternalInput")
    xo = D_("xo", [T, D], F32, "ExternalOutput")

    pb = Banks(P, 7)
    pbl = P.ps("pbl", [128, 512], F32)
    identf = P.sb("identf", [128, 128], F32)
    P.dma("sp", identf[:, :], ident_d[:, :], writes=[identf])
    bgc = P.sb("bgc_s", [128, 32], F32)
    P.dma("sp", bgc[:, :], bgc_d[:, :], writes=[bgc])
    epsl = P.sb("epsl", [128, 1], F32)
    P.memset("dve", epsl, epsl[:, :], LN_EPS)
    lg = P.sb("lg", [128, D], F32)
    lb = P.sb("lb", [128, D], F32)
    wr_s = P.sb("wr_s", [128, 8, 8], F32)
    P.dma("sp", wr_s[:, :, :], wr_d[:, :, :], writes=[wr_s])
    comb = P.sb("comb", [128, NS, 8], F32)

    acc = P.sb("acc", [128, NS, D], F32)
    x1T = P.sb("x1T", [128, 8, T], BF16)
    wbig = P.sb("wbig", [128, 24576], BF16)
    wbr = wbig.t[:, 0:16384].rearrange("p (n k c) -> p n k c", n=4, k=4)
    wo = wbig.t[:, 16384:24576].rearrange("p (k c) -> p k c", k=8)
    for n in range(4):
        for kc in range(4):
            P.dma("pool", wbr[:, n, kc, :], w_br[n, kc * 128:(kc + 1) * 128, :], writes=[wbig])
    for kc in range(8):
        P.dma("pool", wo[:, kc, :], w_o[kc * 128:(kc + 1) * 128, :], writes=[wbig])

    def load_ln(i):
        P.dma("sp", lg[:, :], lnp[i:i + 1, :].partition_broadcast(128), writes=[lg])
        P.dma("sp", lb[:, :], lnp[i + 1:i + 2, :].partition_broadcast(128), writes=[lb])

    stats = P.sb("stats", [128, 2, 6], F32)
    mv_ = P.sb("mv_", [128, 2], F32)
    rs_ = P.sb("rs_", [128, 1], F32)
    rs2 = P.sb("rs2", [128, 1], F32)

    def layer_norm(zb, z_ap, ob, o_ap):
        for hh in range(2):
            P.op("dve", lambda e, hh=hh: e.bn_stats(stats[:, hh, :], z_ap[:, hh * 512:(hh + 1) * 512]), reads=[zb], writes=[stats])
        P.op("dve", lambda e: e.bn_aggr(mv_[:, :], stats[:, :, :].rearrange("p a b -> p (a b)")), reads=[stats], writes=[mv_])
        P.act(rs_, rs_[:, :], mv_, mv_[:, 1:2], AF.Sqrt, bias=epsl[:, 0:1], extra_reads=[epsl])
        P.op("dve", lambda e: e.reciprocal(rs2[:, :], rs_[:, :]), reads=[rs_], writes=[rs2])
        P.ts("dve", ob, o_ap, zb, z_ap, mv_[:, 0:1], rs2[:, 0:1], ALU.subtract, ALU.mult, extra_reads=[mv_, rs2])
        P.tt("pool", ob, o_ap, ob, o_ap, lg, lg[:, :], ALU.mult)
        P.tt("pool", ob, o_ap, ob, o_ap, lb, lb[:, :], ALU.add)

    load_ln(0)
    gw = [P.sb("gw%d" % i, [128, 4, 8, 128], BF16) for i in range(1)] * 2
    W = 256
    NSW = W // 128
    xT_t = P.sb("xT_t", [128, 8, W], BF16)
    br_t = P.sb("br_t", [128, 4, 4, W], BF16)
    g_t = [P.sb("g_t%d" % i, [128, W], F32) for i in range(2)]
    tmp_t = [P.sb("tmp_t%d" % i, [128, W], F32) for i in range(2)]
    mrg = P.sb("mrg", [128, W], F32)
    mrgT = P.sb("mrgT", [128, 8, W], BF16)
    xs_ = [P.sb("xs_%d" % i, [128, D], F32) for i in range(1)] * 2
    z_t = P.sb("z_t", [128, D], F32)
    x1_t = P.sb("x1_t", [128, D], F32)
    x1T32 = [P.sb("x1T32_%d" % i, [128, 128], F32) for i in range(2)]
    lgt = P.sb("lgt", [128, 8], F32)
    sm = [P.sb("sm%d" % i, [128, 8], F32) for i in range(4)]
    sc1 = [P.sb("sc1_%d" % i, [128, 1], F32) for i in range(6)]
    gcount = 0
    for tt in range(T // W):
        t0 = tt * W
        P.dma("sp", xT_t[:, :, :], xT_d[:, t0:t0 + W].rearrange("(k p) t -> p k t", p=128), writes=[xT_t])
        for n in range(4):
            P.dma("act", br_t[:, n, :, :], brs[n, :, t0:t0 + W].rearrange("(k p) t -> p k t", p=128), writes=[br_t])
        for m in range(8):
            gwt = gw[gcount % 2]
            gcount += 1
            for n in range(4):
                c0 = C_GATE + n * 1024 + m * 128
                P.dma("pool", gwt[:, n, :, :], w_in[:, c0:c0 + 128].rearrange("(k p) c -> p k c", p=128), writes=[gwt])
            for n in range(4):
                bG = pb.next()
                for kc in range(8):
                    P.mm(bG, bG[:, 0:W], gwt, gwt[:, n, kc, :], xT_t, xT_t[:, kc, :], kc == 0, kc == 7)
                gt = g_t[n % 2]
                P.act(gt, gt[:, :], bG, bG[:, 0:W], AF.Sigmoid, bias=bgc[:, n * 8 + m:n * 8 + m + 1], extra_reads=[bgc])
                bP = pb.next()
                for kc in range(4):
                    P.mm(bP, bP[:, 0:W], wbig, wbr[:, n, kc, m * 128:(m + 1) * 128], br_t, br_t[:, n, kc, :], kc == 0, kc == 3)
                if n == 0:
                    P.tt("dve", mrg, mrg[:, :], bP, bP[:, 0:W], gt, gt[:, :], ALU.mult)
                else:
                    tp = tmp_t[n % 2]
                    P.tt("dve", tp, tp[:, :], bP, bP[:, 0:W], gt, gt[:, :], ALU.mult)
                    P.tt("pool", mrg, mrg[:, :], mrg, mrg[:, :], tp, tp[:, :], ALU.add)
            P.copy("act", mrgT, mrgT[:, m, :], mrg, mrg[:, :])
        for s in range(NSW):
            si = tt * NSW + s
            xs = xs_[si % 2]
            P.dma("sp", xs[:, :], x[t0 + s * 128:t0 + (s + 1) * 128, :], writes=[xs])
            for hh in range(2):
                b = pb.next()
                for kc in range(8):
                    P.mm(b, b[:, :], mrgT, mrgT[:, kc, s * 128:(s + 1) * 128], wbig, wo[:, kc, hh * 512:(hh + 1) * 512], kc == 0, kc == 7)
                P.stt(z_t, z_t[:, hh * 512:(hh + 1) * 512], xs, xs[:, hh * 512:(hh + 1) * 512], DN_ALPHA, b, b[:, :], ALU.mult, ALU.add)
            layer_norm(z_t, z_t[:, :], x1_t, x1_t[:, :])
            P.op("act", lambda e, si=si: e.mul(acc[:, si, :], x1_t[:, :], DN_ALPHA), reads=[x1_t], writes=[acc])
            bl = pbl
            for kc in range(8):
                b = pb.next()
                P.tr(b, b[:, 0:128], x1_t, x1_t[:, kc * 128:(kc + 1) * 128], identf, identf[:, :])
                P.copy("act", x1T, x1T[:, kc, si * 128:(si + 1) * 128], b, b[:, 0:128])
                if moe:
                    xt32 = x1T32[kc % 2]
                    P.copy("dve", xt32, xt32[:, :], b, b[:, 0:128])
                    P.mm(bl, bl[:, 0:8], xt32, xt32[:, :], wr_s, wr_s[:, kc, :], kc == 0, kc == 7)
            if moe:
                P.copy("dve", lgt, lgt[:, :], bl, bl[:, 0:8])
                m1, m2, dd, ee, w1, w2 = sc1
                eq1, l2, eq2, tq = sm
                P.op("dve", lambda e: e.reduce_max(m1[:, :], lgt[:, :], AX.X), reads=[lgt], writes=[m1])
                P.ts("dve", eq1, eq1[:, :], lgt, lgt[:, :], m1[:, 0:1], None, ALU.is_equal, extra_reads=[m1])
                P.stt(l2, l2[:, :], eq1, eq1[:, :], -1e30, lgt, lgt[:, :], ALU.mult, ALU.add)
                P.op("dve", lambda e: e.reduce_max(m2[:, :], l2[:, :], AX.X), reads=[l2], writes=[m2])
                P.ts("dve", eq2, eq2[:, :], l2, l2[:, :], m2[:, 0:1], None, ALU.is_equal, extra_reads=[m2])
                P.tt("dve", dd, dd[:, :], m2, m2[:, :], m1, m1[:, :], ALU.subtract)
                P.act(ee, ee[:, :], dd, dd[:, :], AF.Exp)
                P.ts("dve", dd, dd[:, :], ee, ee[:, :], 1.0, None, ALU.add)
                P.op("dve", lambda e: e.reciprocal(w1[:, :], dd[:, :]), reads=[dd], writes=[w1])
                P.tt("dve", w2, w2[:, :], ee, ee[:, :], w1, w1[:, :], ALU.mult)
                P.ts("dve", tq, tq[:, :], eq1, eq1[:, :], w1[:, 0:1], None, ALU.mult, extra_reads=[w1])
                P.stt(comb, comb[:, si, :], eq2, eq2[:, :], w2[:, 0:1], tq, tq[:, :], ALU.mult, ALU.add, extra_reads=[w2])

    NU = 7
    wg_t = [Buf(wbig.t, "wg_t%d" % i) for i in range(2)]
    wu_t = [Buf(wbig.t, "wu_t%d" % i) for i in range(2)]
    wd_t = [Buf(wbig.t, "wd_t%d" % i) for i in range(2)]

    def wv(i, which):
        base = i * 12288 + which * 4096
        if which < 2:
            return wbig.t[:, base:base + 4096].rearrange("p (k c) -> p k c", k=8)
        return wbig.t[:, base:base + 4096].rearrange("p (k c) -> p k c", k=4)

    sg_t = [P.sb("sg_t%d" % i, [128, 512], F32) for i in range(2)]
    hT = [P.sb("hT%d" % i, [128, 4, 512], BF16) for i in range(1)] * 2
    first = [True, True]
    u = 0
    for e_ in range(E):
        for fq in range(NU):
            i = u % 2
            u += 1
            extra = [wbig] if first[i] else []
            first[i] = False
            f0 = fq * 512
            P.dma("pool", wv(i, 0), f_g[e_, :, f0:f0 + 512].rearrange("(k p) c -> p k c", p=128), writes=[wg_t[i]] + extra)
            P.dma("pool", wv(i, 1), f_u[e_, :, f0:f0 + 512].rearrange("(k p) c -> p k c", p=128), writes=[wu_t[i]] + extra)
            P.dma("pool", wv(i, 2), f_d[e_, f0:f0 + 512, :].rearrange("(k p) c -> p k c", p=128), writes=[wd_t[i]] + extra)
            for tt in range(NT):
                t0 = tt * 512
                ht = hT[tt % 2]
                for fc in range(4):
                    bg = pb.next()
                    for kc in range(8):
                        P.mm(bg, bg[:, :], wg_t[i], wv(i, 0)[:, kc, fc * 128:(fc + 1) * 128], x1T, x1T[:, kc, t0:t0 + 512], kc == 0, kc == 7)
                    bu = pb.next()
                    for kc in range(8):
                        P.mm(bu, bu[:, :], wu_t[i], wv(i, 1)[:, kc, fc * 128:(fc + 1) * 128], x1T, x1T[:, kc, t0:t0 + 512], kc == 0, kc == 7)
                    sg = sg_t[fc % 2]
                    P.act(sg, sg[:, :], bg, bg[:, :], AF.Silu)
                    P.tt("dve", ht, ht[:, fc, :], bu, bu[:, :], sg, sg[:, :], ALU.mult)
                for s in range(4):
                    si = tt * 4 + s
                    for hh in range(2):
                        bd = pb.next()
                        for fc in range(4):
                            P.mm(bd, bd[:, :], ht, ht[:, fc, s * 128:(s + 1) * 128], wd_t[i], wv(i, 2)[:, fc, hh * 512:(hh + 1) * 512], fc == 0, fc == 3)
                        sc = comb[:, si, e_:e_ + 1] if moe else 1.0
                        P.stt(acc, acc[:, si, hh * 512:(hh + 1) * 512], bd, bd[:, :], sc, acc, acc[:, si, hh * 512:(hh + 1) * 512],
                              ALU.mult, ALU.add, extra_reads=[comb] if moe else [])
    load_ln(2)
    ot = [z_t, x1_t]
    for si in range(NS):
        o = ot[si % 2]
        layer_norm(acc, acc[:, si, :], o, o[:, :])
        P.dma("sp", xo[si * 128:(si + 1) * 128, :], o[:, :], reads=[o], is_output=True)
    P.pop_scope()
    if standalone:
        return P.build()
    return None


def build_phase_b2(S, NH, P, io):
    build_phase_b(S, NH, S, do_conv=True, do_mla=False, do_sb=False, P=P, io=io)
    P.push_scope()
    NB = S // 128
    NQC = S // 512
    mq = io["mq"]; mk = io["mk"]; mv = io["mv"]; sq = io["sq"]; sk = io["sk"]; sv = io["sv"]
    cst = io["cst"]; mo = io["mo"]; so = io["so"]
    pb = [P.ps("pb%d" % i, [128, 512], F32) for i in range(8)]
    cstf = P.sb("cstf", [128, 4, 128], F32)
    P.dma("sp", cstf[:, :, :], cst[:, :, :], writes=[cstf])
    cstb = P.sb("cstb", [128, 4, 128], BF16)
    P.copy("dve", cstb, cstb[:, :, :], cstf, cstf[:, :, :])
    onesb = P.sb("onesb", [128, 128], BF16)
    P.memset("dve", onesb, onesb[:, :], 1.0)
    onesf = P.sb("onesf", [128, 128], F32)
    P.memset("dve", onesf, onesf[:, :], 1.0)
    mask_sb16 = cstb[:, 1, :]
    negtri16 = cstb[:, 3, :]

    kT = P.sb("kT", [96, S], BF16)
    qT = P.sb("qT", [96, S], BF16)
    vA = [P.sb("vA%d" % i, [128, NB, 65], BF16) for i in range(2)]
    pT = [P.sb("pTm%d" % i, [128, 512], BF16) for i in range(3)]
    dsb = P.sb("dsb", [128, 512], F32)
    rsb = P.sb("rsb", [128, 512], F32)
    bcs = P.sb("bcs", [64, 512], F32)
    ost = [P.sb("ost%d" % i, [64, 512], BF16) for i in range(2)]
    m_items = []
    for h in range(NH):
        for qc in range(NQC):
            nk = 4 * (qc + 1)
            for kb in range(nk):
                m_items.append((h, qc, kb, nk))
    loaded = set()

    def load_head(h):
        if h in loaded:
            return
        loaded.add(h)
        P.dma("sp", kT[:, :], mk[h, :, :], writes=[kT])
        P.dma("act", qT[:, :], mq[h, :, :], writes=[qT])
        P.memset("pool", vA[h % 2], vA[h % 2][:, :, :], 1.0)
        P.dma("sp", vA[h % 2][:, :, 0:64], mv[:, h * 64:(h + 1) * 64].rearrange("(b p) c -> p b c", p=128), writes=[vA[h % 2]])

    def m_s1(k, it):
        h, qc, kb, nk = it
        load_head(h)
        c0 = max(0, kb - 4 * qc) * 128
        Sb = pb[k % 2]
        P.mm(Sb, Sb[:, c0:512], kT, kT[:, kb * 128:(kb + 1) * 128], qT, qT[:, qc * 512 + c0:qc * 512 + 512], True, True)
        pt = pT[k % 3]
        P.act(pt, pt[:, c0:512], Sb, Sb[:, c0:512], AF.Exp)
        if kb >= 4 * qc:
            P.memset("pool", pt, pt[64:128, c0:c0 + 64], 0.0)

    def m_s2(k, it):
        h, qc, kb, nk = it
        c0 = max(0, kb - 4 * qc) * 128
        O = pb[2]
        pt = pT[k % 3]
        P.mm(O, O[0:65, c0:512], vA[h % 2], vA[h % 2][:, kb, :], pt, pt[:, c0:512], kb == 0, True, sgc=(kb > 0))
        if kb == nk - 1:
            g = h * NQC + qc
            P.copy("act", dsb, dsb[64:65, :], O, O[64:65, :])
            P.op("dve", lambda e: e.reciprocal(rsb[64:65, :], dsb[64:65, :]), reads=[dsb], writes=[rsb])
            bc = pb[7]
            P.mm(bc, bc[0:64, :], onesf, onesf[64:65, 0:64], rsb, rsb[64:65, :], True, True)
            P.copy("act", bcs, bcs[:, :], bc, bc[0:64, :])
            os_ = ost[g % 2]
            P.tt("dve", os_, os_[:, :], O, O[0:64, :], bcs, bcs[:, :], ALU.mult)
            P.dma("sp", mo[h * 64:(h + 1) * 64, qc * 512:(qc + 1) * 512], os_[:, :], reads=[os_], is_output=True)

    GH = 4
    skT = [P.sb("skT%d" % i, [128, S], BF16) for i in range(2)]
    sqT = [P.sb("sqT%d" % i, [128, S], BF16) for i in range(2)]
    svt = [[P.sb("svt%d_%d" % (g, i), [128, NB, 64], BF16) for i in range(GH)] for g in range(2)]
    e_t = [P.sb("e_t%d" % i, [128, 512], F32) for i in range(3)]
    lp16 = [P.sb("lp16_%d" % i, [128, 512], BF16) for i in range(3)]
    t_t = [P.sb("t_t%d" % i, [128, 512], F32) for i in range(3)]
    a16 = [P.sb("a16_%d" % i, [128, 512], BF16) for i in range(3)]
    cacc = [P.sb("cacc%d" % i, [128, 512], F32) for i in range(2)]
    sst = [P.sb("sst%d" % i, [64, 512], BF16) for i in range(2)]
    s_items = []
    for h in range(NH):
        for qc in range(NQC):
            nk = 4 * (qc + 1)
            for kb in range(nk - 1, -1, -1):
                s_items.append((h, qc, kb, nk))
    gloaded = set()

    def load_group(hg):
        if hg in gloaded:
            return
        gloaded.add(hg)
        for pr in range(2):
            r0 = (hg * 2 + pr) * 128
            P.dma("sp", skT[pr][:, :], sk[r0:r0 + 128, :], writes=[skT[pr]])
            P.dma("act", sqT[pr][:, :], sq[r0:r0 + 128, :], writes=[sqT[pr]])
        for hl in range(GH):
            hh = hg * GH + hl
            t = svt[hg % 2][hl]
            P.dma("sp", t[:, :, :], sv[:, hh * 64:(hh + 1) * 64].rearrange("(b p) c -> p b c", p=128), writes=[t])

    def s_s1(k, it):
        h, qc, kb, nk = it
        load_group(h // GH)
        hl = h % GH
        c0 = max(0, kb - 4 * qc) * 128
        g = h * NQC + qc
        if kb == nk - 1:
            P.memset("pool", cacc[g % 2], cacc[g % 2][:, :], 0.0)
        Zb = pb[3 + k % 2]
        hp = slice((hl % 2) * 64, (hl % 2) * 64 + 64)
        P.mm(Zb, Zb[:, c0:512], skT[hl // 2], skT[hl // 2][hp, kb * 128:(kb + 1) * 128], sqT[hl // 2], sqT[hl // 2][hp, qc * 512 + c0:qc * 512 + 512], True, True)
        P.act(e_t[k % 3], e_t[k % 3][:, c0:512], Zb, Zb[:, c0:512], AF.Exp)
        P.act(lp16[k % 3], lp16[k % 3][:, c0:512], e_t[k % 3], e_t[k % 3][:, c0:512], AF.Ln, bias=1.0)
        if kb >= 4 * qc:
            P.tt("pool", lp16[k % 3], lp16[k % 3][:, c0:c0 + 128], lp16[k % 3], lp16[k % 3][:, c0:c0 + 128], cstb, mask_sb16, ALU.mult)

    def s_s2(k, it):
        h, qc, kb, nk = it
        c0 = max(0, kb - 4 * qc) * 128
        g = h * NQC + qc
        Zb = pb[3 + k % 2]
        Cb = pb[5]
        L = lp16[k % 3]
        P.mm(Zb, Zb[:, c0:512], cstb, negtri16, L, L[:, c0:512], False, True, sgc=True)
        P.mm(Cb, Cb[:, c0:512], onesb, onesb[:, :], L, L[:, c0:512], True, True)
        ca = cacc[g % 2]
        P.tt("dve", t_t[k % 3], t_t[k % 3][:, c0:512], Zb, Zb[:, c0:512], ca, ca[:, c0:512], ALU.subtract)
        P.act(a16[k % 3], a16[k % 3][:, c0:512], t_t[k % 3], t_t[k % 3][:, c0:512], AF.Exp)
        if kb >= 4 * qc:
            P.tt("pool", a16[k % 3], a16[k % 3][:, c0:c0 + 128], a16[k % 3], a16[k % 3][:, c0:c0 + 128], cstb, mask_sb16, ALU.mult)
        if kb > 0:
            P.tt("dve", ca, ca[:, c0:512], Cb, Cb[:, c0:512], ca, ca[:, c0:512], ALU.add)

    def s_s3(k, it):
        h, qc, kb, nk = it
        c0 = max(0, kb - 4 * qc) * 128
        g = h * NQC + qc
        O = pb[6]
        t = svt[(h // GH) % 2][h % GH]
        P.mm(O, O[0:64, c0:512], t, t[:, kb, :], a16[k % 3], a16[k % 3][:, c0:512], kb == nk - 1, True, sgc=(kb < nk - 1))
        if kb == 0:
            os_ = sst[g % 2]
            P.copy("act", os_, os_[:, :], O, O[0:64, :])
            P.dma("sp", so[h * 64:(h + 1) * 64, qc * 512:(qc + 1) * 512], os_[:, :], reads=[os_], is_output=True)

    nm, ns = len(m_items), len(s_items)
    for step in range(max(nm, ns) + 2):
        if step < nm:
            m_s1(step, m_items[step])
        if step < ns:
            s_s1(step, s_items[step])
        if 0 <= step - 1 < nm:
            m_s2(step - 1, m_items[step - 1])
        if 0 <= step - 1 < ns:
            s_s2(step - 1, s_items[step - 1])
        if 0 <= step - 2 < ns:
            s_s3(step - 2, s_items[step - 2])
    P.pop_scope()


def load_gate_weights(P, gwr, w_in):
    for n in range(4):
        for kc in range(8):
            c0 = C_GATE + n * 1024
            P.dma("pool", gwr[:, n, kc, :], w_in[kc * 128:(kc + 1) * 128, c0:c0 + 1024], writes=[gwr])


def build_phase_c1(T, moe, P, io):
    P.push_scope()
    x = io["x"]; xT_d = io["xT"]; brs = io["brs"]; w_in = io["w_in"]; bgc_d = io["bgc"]
    w_br = io["w_br"]; w_o = io["w_o"]; lnp = io["lnp"]; wr_d = io["wr"]; ident_d = io["ident"]
    x1_o = io["x1_o"]; x1T_o = io["x1T_o"]; comb_o = io["comb_o"]
    W = 512
    NSW = W // 128
    pb = Banks(P, 7)
    pbl = P.ps("pbl", [128, 512], F32)
    identf = P.sb("identf", [128, 128], F32)
    P.dma("sp", identf[:, :], ident_d[:, :], writes=[identf])
    bgc = P.sb("bgc_s", [128, 32], F32)
    P.dma("sp", bgc[:, :], bgc_d[:, :], writes=[bgc])
    epsl = P.sb("epsl", [128, 1], F32)
    P.memset("dve", epsl, epsl[:, :], LN_EPS)
    lg = P.sb("lg", [128, D], F32)
    lb = P.sb("lb", [128, D], F32)
    P.dma("sp", lg[:, :], lnp[0:1, :].partition_broadcast(128), writes=[lg])
    P.dma("sp", lb[:, :], lnp[1:2, :].partition_broadcast(128), writes=[lb])
    wr_s = P.sb("wr_s", [128, 8, 8], F32)
    P.dma("sp", wr_s[:, :, :], wr_d[:, :, :], writes=[wr_s])
    gwr = io.get("gwr_buf")
    pre = gwr is not None
    if not pre:
        gwr = P.sb("gwr", [128, 4, 8, 1024], BF16)
    wbr = P.sb("wbr", [128, 4, 4, 1024], BF16)
    wo = P.sb("wo", [128, 8, 1024], BF16)
    for n in range(4):
        for kc in range(4):
            P.dma("pool", wbr[:, n, kc, :], w_br[n, kc * 128:(kc + 1) * 128, :], writes=[wbr])
    if not pre:
        load_gate_weights(P, gwr, w_in)
    for kc in range(8):
        P.dma("pool", wo[:, kc, :], w_o[kc * 128:(kc + 1) * 128, :], writes=[wo])
    stats = P.sb("stats", [128, 2, 6], F32)
    mv_ = P.sb("mv_", [128, 2], F32)
    rs_ = P.sb("rs_", [128, 1], F32)
    rs2 = P.sb("rs2", [128, 1], F32)

    def layer_norm(zb, z_ap, ob, o_ap):
        for hh in range(2):
            P.op("dve", lambda e, hh=hh: e.bn_stats(stats[:, hh, :], z_ap[:, hh * 512:(hh + 1) * 512]), reads=[zb], writes=[stats])
        P.op("dve", lambda e: e.bn_aggr(mv_[:, :], stats[:, :, :].rearrange("p a b -> p (a b)")), reads=[stats], writes=[mv_])
        P.act(rs_, rs_[:, :], mv_, mv_[:, 1:2], AF.Sqrt, bias=epsl[:, 0:1], extra_reads=[epsl])
        P.op("dve", lambda e: e.reciprocal(rs2[:, :], rs_[:, :]), reads=[rs_], writes=[rs2])
        P.ts("dve", ob, o_ap, zb, z_ap, mv_[:, 0:1], rs2[:, 0:1], ALU.subtract, ALU.mult, extra_reads=[mv_, rs2])
        P.tt("pool", ob, o_ap, ob, o_ap, lg, lg[:, :], ALU.mult)
        P.tt("pool", ob, o_ap, ob, o_ap, lb, lb[:, :], ALU.add)

    xT_t = [P.sb("xT_t%d" % i, [128, 8, W], BF16) for i in range(1)] * 2
    br_t = [P.sb("br_t%d" % i, [128, 4, 4, W], BF16) for i in range(1)] * 2
    g_t = [P.sb("g_t%d" % i, [128, W], F32) for i in range(2)]
    tmp_t = [P.sb("tmp_t%d" % i, [128, W], F32) for i in range(1)] * 2
    mrg = P.sb("mrg", [128, W], F32)
    mrgT2 = [P.sb("mrgT%d" % i, [128, 8, W], BF16) for i in range(2)]
    xs_ = [P.sb("xs_%d" % i, [128, D], F32) for i in range(1)] * 2
    z_t = P.sb("z_t", [128, D], F32)
    x1_t = [P.sb("x1_t%d" % i, [128, D], F32) for i in range(4)]
    x1Ts = [P.sb("x1Ts%d" % i, [128, 8, 128], BF16) for i in range(2)]
    x1a = P.sb("x1a", [128, D], F32)
    x1T32 = [P.sb("x1T32_%d" % i, [128, 128], F32) for i in range(2)]
    lgt = P.sb("lgt", [128, 8], F32)
    lgt4 = P.sb("lgt4", [128, NSW, 8], F32)
    lgT = P.sb("lgT", [8, 128], F32)
    eq1_ = P.sb("eq1_", [128, NSW, 8], F32)
    eq2_ = P.sb("eq2_", [128, NSW, 8], F32)
    l2_ = P.sb("l2_", [128, NSW, 8], F32)
    m1_ = P.sb("m1_", [128, NSW], F32)
    m2_ = P.sb("m2_", [128, NSW], F32)
    dd_ = P.sb("dd_", [128, NSW], F32)
    w1_ = P.sb("w1_", [128, NSW], F32)
    w2_ = P.sb("w2_", [128, NSW], F32)
    cmb4 = [P.sb("cmb4_%d" % i, [128, NSW, 8], F32) for i in range(2)]
    sm = [P.sb("sm%d" % i, [128, 8], F32) for i in range(4)]
    cmb = [P.sb("cmb%d" % i, [128, 8], F32) for i in range(2)]
    sc1 = [P.sb("sc1_%d" % i, [128, 1], F32) for i in range(6)]
    NTt = T // W

    def loads(tt):
        t0 = tt * W
        xt = xT_t[tt % 2]
        brt = br_t[tt % 2]
        P.dma("sp", xt[:, :, :], xT_d[:, t0:t0 + W].rearrange("(k p) t -> p k t", p=128), writes=[xt])
        for n in range(4):
            P.dma("act", brt[:, n, :, :], brs[n, :, t0:t0 + W].rearrange("(k p) t -> p k t", p=128), writes=[brt])

    def merge_m(tt, m):
        xt = xT_t[tt % 2]
        brt = br_t[tt % 2]
        mrgT = mrgT2[tt % 2]
        for n in range(4):
            bG = pb.next()
            for kc in range(8):
                P.mm(bG, bG[:, 0:W], gwr, gwr[:, n, kc, m * 128:(m + 1) * 128], xt, xt[:, kc, :], kc == 0, kc == 7)
            gt = g_t[n % 2]
            P.act(gt, gt[:, :], bG, bG[:, 0:W], AF.Sigmoid, bias=bgc[:, n * 8 + m:n * 8 + m + 1], extra_reads=[bgc])
            bP = pb.next()
            for kc in range(4):
                P.mm(bP, bP[:, 0:W], wbr, wbr[:, n, kc, m * 128:(m + 1) * 128], brt, brt[:, n, kc, :], kc == 0, kc == 3)
            if n == 0:
                P.tt("dve", mrg, mrg[:, :], bP, bP[:, 0:W], gt, gt[:, :], ALU.mult)
            else:
                tp = tmp_t[n % 2]
                P.tt("dve", tp, tp[:, :], bP, bP[:, 0:W], gt, gt[:, :], ALU.mult)
                P.tt("pool", mrg, mrg[:, :], mrg, mrg[:, :], tp, tp[:, :], ALU.add)
        P.copy("act", mrgT, mrgT[:, m, :], mrg, mrg[:, :])

    def post_wout(tt, s):
        t0 = tt * W
        mrgT = mrgT2[tt % 2]
        si = tt * NSW + s
        xs = xs_[si % 2]
        x1t = x1_t[s % 4]
        P.dma("sp", xs[:, :], x[t0 + s * 128:t0 + (s + 1) * 128, :], writes=[xs])
        for hh in range(2):
            b = pb.next()
            for kc in range(8):
                P.mm(b, b[:, :], mrgT, mrgT[:, kc, s * 128:(s + 1) * 128], wo, wo[:, kc, hh * 512:(hh + 1) * 512], kc == 0, kc == 7)
            P.stt(z_t, z_t[:, hh * 512:(hh + 1) * 512], xs, xs[:, hh * 512:(hh + 1) * 512], DN_ALPHA, b, b[:, :], ALU.mult, ALU.add)
        layer_norm(z_t, z_t[:, :], x1t, x1t[:, :])
        P.op("act", lambda e: e.mul(x1a[:, :], x1t[:, :], DN_ALPHA), reads=[x1t], writes=[x1a])
        P.dma("sp", x1_o[si * 128:(si + 1) * 128, :], x1a[:, :], reads=[x1a], is_output=True)

    def post_tr(tt, s):
        si = tt * NSW + s
        x1t = x1_t[s % 4]
        xts = x1Ts[si % 2]
        bl = pbl
        for kc in range(8):
            b = pb.next()
            P.tr(b, b[:, 0:128], x1t, x1t[:, kc * 128:(kc + 1) * 128], identf, identf[:, :])
            P.copy("act", xts, xts[:, kc, :], b, b[:, 0:128])
            if moe:
                xt32 = x1T32[kc % 2]
                P.copy("dve", xt32, xt32[:, :], b, b[:, 0:128])
                P.mm(bl, bl[0:8, 0:128], wr_s, wr_s[:, kc, :], xt32, xt32[:, :], kc == 0, kc == 7)
        P.dma("sp", x1T_o[:, si * 128:(si + 1) * 128].rearrange("(k p) t -> p k t", p=128), xts[:, :, :], reads=[xts], is_output=True)
        if moe:
            P.copy("act", lgT, lgT[0:8, :], bl, bl[0:8, 0:128])
            b2 = pb.next()
            P.tr(b2, b2[:, 0:8], lgT, lgT[0:8, :], identf, identf[0:8, 0:8])
            P.copy("dve", lgt4, lgt4[:, s, :], b2, b2[:, 0:8])
            if s == NSW - 1:
                def bc_(t_):
                    return t_.t[:, :].unsqueeze(2).to_broadcast([128, NSW, 8])
                P.op("dve", lambda e: e.reduce_max(m1_[:, :], lgt4[:, :, :], AX.X), reads=[lgt4], writes=[m1_])
                P.tt("dve", eq1_, eq1_[:, :, :], lgt4, lgt4[:, :, :], m1_, bc_(m1_), ALU.is_equal)
                P.stt(l2_, l2_[:, :, :], eq1_, eq1_[:, :, :], -1e30, lgt4, lgt4[:, :, :], ALU.mult, ALU.add)
                P.op("dve", lambda e: e.reduce_max(m2_[:, :], l2_[:, :, :], AX.X), reads=[l2_], writes=[m2_])
                P.tt("dve", eq2_, eq2_[:, :, :], l2_, l2_[:, :, :], m2_, bc_(m2_), ALU.is_equal)
                P.tt("dve", dd_, dd_[:, :], m2_, m2_[:, :], m1_, m1_[:, :], ALU.subtract)
                P.act(w2_, w2_[:, :], dd_, dd_[:, :], AF.Sigmoid)
                P.act(w1_, w1_[:, :], dd_, dd_[:, :], AF.Sigmoid, scale=-1.0)
                P.tt("dve", eq1_, eq1_[:, :, :], eq1_, eq1_[:, :, :], w1_, bc_(w1_), ALU.mult)
                P.tt("dve", eq2_, eq2_[:, :, :], eq2_, eq2_[:, :, :], w2_, bc_(w2_), ALU.mult)
                cb = cmb4[tt % 2]
                P.tt("dve", cb, cb[:, :, :], eq1_, eq1_[:, :, :], eq2_, eq2_[:, :, :], ALU.add)
                r0 = tt * NSW * 128
                P.dma("sp", comb_o[r0:r0 + NSW * 128, :].rearrange("(s p) e -> p s e", p=128), cb[:, :, :], reads=[cb], is_output=True)

    for tt in range(NTt + 1):
        if tt < NTt:
            loads(tt)
        for m in range(8):
            if tt < NTt:
                merge_m(tt, m)
            if tt >= 1:
                if m < NSW:
                    post_wout(tt - 1, m)
                if 3 <= m < 3 + NSW:
                    post_tr(tt - 1, m - 3)
    P.pop_scope()


def build_phase_c2(T, E, P, io):
    moe = E > 1
    P.push_scope()
    x1_d = io["x1"]; x1T_d = io["x1T"]; comb_d = io["comb"]; lnp = io["lnp"]
    f_g = io["f_g"]; f_u = io["f_u"]; f_d = io["f_d"]; xo = io["xo"]
    NT = T // 512
    NS = T // 128
    pb = Banks(P, 8)
    epsl = P.sb("epsl", [128, 1], F32)
    P.memset("dve", epsl, epsl[:, :], LN_EPS)
    acc = P.sb("acc", [128, NS, D], F32)
    comb = P.sb("comb", [128, NS, 8], F32)
    if moe:
        P.dma("sp", comb[:, :, :], comb_d.rearrange("(s p) e -> p s e", p=128), writes=[comb])
    CH = min(8, NS)
    for c in range(NS // CH):
        P.dma("sp" if c % 2 else "act", acc[:, c * CH:(c + 1) * CH, :],
              x1_d[c * CH * 128:(c + 1) * CH * 128, :].rearrange("(s p) d -> p s d", p=128), writes=[acc])
    P.push_scope()
    wbig = P.sb("wbig", [128, 24576], BF16)
    wg_t = [Buf(wbig.t, "wg_t%d" % i) for i in range(2)]
    wu_t = [Buf(wbig.t, "wu_t%d" % i) for i in range(2)]
    wd_t = [Buf(wbig.t, "wd_t%d" % i) for i in range(2)]

    def wv(i, which):
        base = i * 12288 + which * 4096
        if which < 2:
            return wbig.t[:, base:base + 4096].rearrange("p (k c) -> p k c", k=8)
        return wbig.t[:, base:base + 4096].rearrange("p (k c) -> p k c", k=4)

    xT_t = [P.sb("xT_t%d" % i, [128, 8, 512], BF16) for i in range(2)]
    sg_t = [P.sb("sg_t%d" % i, [128, 512], F32) for i in range(2)]
    hT = [P.sb("hT%d" % i, [128, 4, 512], BF16) for i in range(2)]
    u = 0
    xi = 0
    for e_ in range(E):
        for fq in range(7):
            i = u % 2
            u += 1
            f0 = fq * 512
            P.dma("pool", wv(i, 0), f_g[e_, :, f0:f0 + 512].rearrange("(k p) c -> p k c", p=128), writes=[wg_t[i]])
            P.dma("pool", wv(i, 1), f_u[e_, :, f0:f0 + 512].rearrange("(k p) c -> p k c", p=128), writes=[wu_t[i]])
            P.dma("pool", wv(i, 2), f_d[e_, f0:f0 + 512, :].rearrange("(k p) c -> p k c", p=128), writes=[wd_t[i]])
            for tt in range(NT):
                t0 = tt * 512
                xt = xT_t[xi % 2]
                xi += 1
                P.dma("sp", xt[:, :, :], x1T_d[:, t0:t0 + 512].rearrange("(k p) t -> p k t", p=128), writes=[xt])
                ht = hT[tt % 2]
                for fc in range(4):
                    bg = pb.next()
                    for kc in range(8):
                        P.mm(bg, bg[:, :], wg_t[i], wv(i, 0)[:, kc, fc * 128:(fc + 1) * 128], xt, xt[:, kc, :], kc == 0, kc == 7)
                    bu = pb.next()
                    for kc in range(8):
                        P.mm(bu, bu[:, :], wu_t[i], wv(i, 1)[:, kc, fc * 128:(fc + 1) * 128], xt, xt[:, kc, :], kc == 0, kc == 7)
                    sg = sg_t[fc % 2]
                    P.act(sg, sg[:, :], bg, bg[:, :], AF.Silu)
                    P.tt("dve", ht, ht[:, fc, :], bu, bu[:, :], sg, sg[:, :], ALU.mult)
                for s in range(4):
                    si = tt * 4 + s
                    for hh in range(2):
                        bd = pb.next()
                        for fc in range(4):
                            P.mm(bd, bd[:, :], ht, ht[:, fc, s * 128:(s + 1) * 128], wd_t[i], wv(i, 2)[:, fc, hh * 512:(hh + 1) * 512], fc == 0, fc == 3)
                        sc = comb[:, si, e_:e_ + 1] if moe else 1.0
                        P.stt(acc, acc[:, si, hh * 512:(hh + 1) * 512], bd, bd[:, :], sc, acc, acc[:, si, hh * 512:(hh + 1) * 512],
                              ALU.mult, ALU.add, extra_reads=[comb] if moe else [])
    P.pop_scope()
    lg = P.sb("lg", [128, D], F32)
    lb = P.sb("lb", [128, D], F32)
    P.dma("sp", lg[:, :], lnp[2:3, :].partition_broadcast(128), writes=[lg])
    P.dma("sp", lb[:, :], lnp[3:4, :].partition_broadcast(128), writes=[lb])
    stats = [P.sb("stats%d" % i, [128, 2, 6], F32) for i in range(4)]
    mv_ = [P.sb("mv_%d" % i, [128, 2], F32) for i in range(4)]
    rs_ = [P.sb("rs_%d" % i, [128, 1], F32) for i in range(4)]
    rs2 = [P.sb("rs2%d" % i, [128, 1], F32) for i in range(4)]
    ot = [P.sb("ot%d" % i, [128, D], F32) for i in range(2)]
    for si in range(NS):
        o = ot[si % 2]
        z_ap = acc[:, si, :]
        st, mv1, r1, r2 = stats[si % 4], mv_[si % 4], rs_[si % 4], rs2[si % 4]
        for hh in range(2):
            P.op("dve", lambda e, hh=hh, z_ap=z_ap, st=st: e.bn_stats(st[:, hh, :], z_ap[:, hh * 512:(hh + 1) * 512]), reads=[acc], writes=[st])
        P.op("dve", lambda e, st=st, mv1=mv1: e.bn_aggr(mv1[:, :], st[:, :, :].rearrange("p a b -> p (a b)")), reads=[st], writes=[mv1])
        P.act(r1, r1[:, :], mv1, mv1[:, 1:2], AF.Sqrt, bias=epsl[:, 0:1], extra_reads=[epsl])
        P.op("dve", lambda e, r1=r1, r2=r2: e.reciprocal(r2[:, :], r1[:, :]), reads=[r1], writes=[r2])
        P.ts("dve", o, o[:, :], acc, z_ap, mv1[:, 0:1], r2[:, 0:1], ALU.subtract, ALU.mult, extra_reads=[mv1, r2])
        P.tt("dve", o, o[:, :], o, o[:, :], lg, lg[:, :], ALU.mult)
        P.tt("pool", o, o[:, :], o, o[:, :], lb, lb[:, :], ALU.add)
        P.dma("sp", xo[si * 128:(si + 1) * 128, :], o[:, :], reads=[o], is_output=True)
    P.pop_scope()


def build_fused(S=4096, TP=2048):
    P = Prog()
    nc = P.nc
    x = P.dram("x", [S, D], F32, "ExternalInput")
    posr = P.dram("posr", [32, S], I32, "ExternalInput")
    w_in = P.dram("w_in", [2, D, IN_COLS], F32, "ExternalInput")
    w_uq = P.dram("w_uq", [2, 256, 768], F32, "ExternalInput")
    w_ukv = P.dram("w_ukv", [2, 128, 1024], F32, "ExternalInput")
    qn = P.dram("qn", [2, 128, 2], F32, "ExternalInput")
    kvn = P.dram("kvn", [2, 128, 1], F32, "ExternalInput")
    ident = P.dram("ident", [128, 128], F32, "ExternalInput")
    invf = P.dram("invf", [128, 1], F32, "ExternalInput")
    sc_w = P.dram("sc_w", [2, 128, 4, 3], F32, "ExternalInput")
    cf_w = P.dram("cf_w", [2, 128, 4, 31], F32, "ExternalInput")
    cf_p = P.dram("cf_p", [2, 128, 3, 4], F32, "ExternalInput")
    cst = P.dram("cst", [128, 4, 128], F32, "ExternalInput")
    bgc = P.dram("bgc", [2, 128, 32], F32, "ExternalInput")
    w_br = P.dram("w_br", [2, 4, 512, D], F32, "ExternalInput")
    w_o = P.dram("w_o", [2, D, D], F32, "ExternalInput")
    lnp = P.dram("lnp", [2, 4, D], F32, "ExternalInput")
    f_g1 = P.dram("f_g1", [1, D, 3584], F32, "ExternalInput")
    f_u1 = P.dram("f_u1", [1, D, 3584], F32, "ExternalInput")
    f_d1 = P.dram("f_d1", [1, 3584, D], F32, "ExternalInput")
    f_g8 = P.dram("f_g8", [8, D, 3584], F32, "ExternalInput")
    f_u8 = P.dram("f_u8", [8, D, 3584], F32, "ExternalInput")
    f_d8 = P.dram("f_d8", [8, 3584, D], F32, "ExternalInput")
    wr = P.dram("wr", [128, 8, 8], F32, "ExternalInput")
    xo = P.dram("xo", [S, D], F32, "ExternalOutput")

    def I_(name, shape, dt):
        return nc.dram_tensor(name, list(shape), dt).ap()
    xT_s = I_("xT_s", [D, S], BF16)
    scb_s = I_("scb_s", [512, S], BF16)
    scch_s = I_("scch_s", [512, S + 2], BF16)
    cfu_s = I_("cfu_s", [512, S + 30], BF16)
    mq_s = I_("mq_s", [8, 96, S], BF16)
    mk_s = I_("mk_s", [8, 96, S], BF16)
    mv_s = I_("mv_s", [S, 512], BF16)
    sq_s = I_("sq_s", [512, S], BF16)
    sk_s = I_("sk_s", [512, S], BF16)
    sv_s = I_("sv_s", [S, 512], BF16)
    brs_s = I_("brs_s", [4, 512, S], BF16)
    xmid = I_("xmid", [S, D], F32)
    x1_s = I_("x1_s", [S, D], F32)
    x1T_s = I_("x1T_s", [D, S], BF16)
    comb_s = I_("comb_s", [S, 8], F32)

    P.push_scope()
    zt = P.sb("zt", [128, 4, 32], BF16)
    P.memset("dve", zt, zt[:, :, :], 0.0)
    P.dma("sp", scch_s[:, 0:2].rearrange("(j p) t -> p j t", p=128), zt[:, :, 0:2], reads=[zt])
    P.dma("sp", cfu_s[:, 0:30].rearrange("(j p) t -> p j t", p=128), zt[:, :, 0:30], reads=[zt])
    P.pop_scope()

    for l in range(2):
        xin = x if l == 0 else xmid
        xout = xmid if l == 0 else xo
        io_a = {"x": xin, "posr": posr, "w_in": w_in[l], "w_uq": w_uq[l], "w_ukv": w_ukv[l],
                "qn": qn[l], "kvn": kvn[l], "ident": ident, "invf": invf,
                "xT_o": xT_s, "scb_o": scb_s, "scch_o": scch_s[:, 2:2 + S], "cfu_o": cfu_s[:, 30:30 + S],
                "mq_o": mq_s, "mk_o": mk_s, "mv_o": mv_s, "sq_o": sq_s, "sk_o": sk_s, "sv_o": sv_s}
        build_phase_a(S, P=P, io=io_a)
        io_b = {"scb": scb_s, "scch": scch_s, "cfu": cfu_s, "sc_w": sc_w[l], "cf_w": cf_w[l], "cf_p": cf_p[l],
                "mq": mq_s, "mk": mk_s, "mv": mv_s, "sq": sq_s, "sk": sk_s, "sv": sv_s, "cst": cst,
                "br0": brs_s[0], "br1": brs_s[1], "mo": brs_s[2], "so": brs_s[3]}
        P.push_scope()
        gwr = P.sb("gwr", [128, 4, 8, 1024], BF16)
        load_gate_weights(P, gwr, w_in[l])
        build_phase_b2(S, 8, P, io_b)
        E = 1 if l == 0 else 8
        io_c1 = {"x": xin, "xT": xT_s, "brs": brs_s, "w_in": w_in[l], "bgc": bgc[l], "w_br": w_br[l], "w_o": w_o[l],
                 "lnp": lnp[l], "wr": wr, "ident": ident, "x1_o": x1_s, "x1T_o": x1T_s, "comb_o": comb_s}
        io_c1["gwr_buf"] = gwr
        build_phase_c1(S, E > 1, P, io_c1)
        P.pop_scope()
        io_c2 = {"x1": x1_s, "x1T": x1T_s, "comb": comb_s, "lnp": lnp[l],
                 "f_g": f_g1 if E == 1 else f_g8, "f_u": f_u1 if E == 1 else f_u8,
                 "f_d": f_d1 if E == 1 else f_d8, "xo": xout}
        build_phase_c2(S, E, P, io_c2)
    return P.build()


def fused_inputs(inp, seq, S=4096):
    f32 = np.float32
    x = np.asarray(inp["x"], f32)[seq][:S]
    pos = np.asarray(inp["positions"])[seq][:S].astype(np.int32)
    invf = np.zeros((128, 1), f32)
    inv = (1.0 / (10000.0 ** (np.arange(16, dtype=f32) * (2.0 / 32)))).astype(f32)
    invf[64:80, 0] = inv
    invf[80:96, 0] = inv
    cst = np.zeros((128, 4, 128), f32)
    pp = np.arange(128)[:, None]
    qq = np.arange(128)[None, :]
    cst[:, 0] = np.eye(128)
    cst[:, 1] = (pp < qq)
    cst[:, 2] = ~((pp >= 64) & (qq < 64))
    cst[:, 3] = -(pp >= qq).astype(f32)
    A = lambda k: np.asarray(inp[k], f32)
    d = {
        "x": np.ascontiguousarray(x),
        "posr": np.ascontiguousarray(np.broadcast_to(pos[None, :], (32, S))),
        "w_in": A("w_in"), "w_uq": A("mla_w_uq"), "w_ukv": A("mla_w_ukv"),
        "qn": np.ascontiguousarray(A("mla_q_norm").reshape(2, 2, 128).transpose(0, 2, 1)),
        "kvn": A("mla_kv_norm").reshape(2, 128, 1),
        "ident": np.eye(128, dtype=f32), "invf": invf,
        "sc_w": np.ascontiguousarray(A("sc_conv").transpose(0, 2, 1).reshape(2, 4, 128, 3).transpose(0, 2, 1, 3)),
        "cf_w": np.ascontiguousarray(A("cf_conv").transpose(0, 2, 1).reshape(2, 4, 128, 31).transpose(0, 2, 1, 3)),
        "cf_p": np.ascontiguousarray(np.stack([np.stack([_col4(A(k)[l]) for k in ("cf_conv_bias", "cf_ln_g", "cf_ln_b")], 1)
                                               for l in range(2)])),
        "cst": cst,
        "bgc": np.ascontiguousarray(A("b_gate").reshape(2, 32, 128).transpose(0, 2, 1)),
        "w_br": A("w_branch"), "w_o": A("w_out"),
        "lnp": np.ascontiguousarray(np.stack([np.stack([A("ln_mix_g")[l], A("ln_mix_b")[l], A("ln_ffn_g")[l], A("ln_ffn_b")[l]])
                                              for l in range(2)])),
        "f_g1": A("ffn_w_gate"), "f_u1": A("ffn_w_up"), "f_d1": A("ffn_w_down"),
        "f_g8": A("exp_w_gate")[0], "f_u8": A("exp_w_up")[0], "f_d8": A("exp_w_down")[0],
        "wr": np.ascontiguousarray(A("router_w")[0].reshape(8, 128, 8).transpose(1, 0, 2)),
    }
    return d


_PROGS = {}


def _prog(key, fn):
    if key not in _PROGS:
        _PROGS[key] = fn()
    return _PROGS[key]


def _run(nc, in_maps):
    res = run_bass_kernel_spmd(nc, in_maps, core_ids=list(range(8)))
    return res.results


def _col4(v):
    return np.ascontiguousarray(v.reshape(4, 128).T)


def kernel_unfused(x, positions, w_in, b_gate, sc_conv, cf_conv, cf_conv_bias, cf_ln_g, cf_ln_b,
           mla_q_norm, mla_w_uq, mla_kv_norm, mla_w_ukv, w_branch, w_out,
           ln_mix_g, ln_mix_b, ln_ffn_g, ln_ffn_b, ffn_w_gate, ffn_w_up, ffn_w_down,
           router_w, exp_w_gate, exp_w_up, exp_w_down):
    f32 = np.float32
    T = 2048
    S = 4096
    xf = np.ascontiguousarray(np.asarray(x, f32).reshape(-1, D))
    pos = np.asarray(positions).reshape(-1).astype(np.int32)
    ident = np.eye(128, dtype=f32)
    invf = np.zeros((128, 1), f32)
    inv = (1.0 / (10000.0 ** (np.arange(16, dtype=f32) * (2.0 / 32)))).astype(f32)
    invf[64:80, 0] = inv
    invf[80:96, 0] = inv
    cst = np.zeros((128, 4, 128), f32)
    pp = np.arange(128)[:, None]
    qq = np.arange(128)[None, :]
    cst[:, 0] = np.eye(128)
    cst[:, 1] = (pp < qq)
    cst[:, 2] = ~((pp >= 64) & (qq < 64))
    cst[:, 3] = -(pp >= qq).astype(f32)

    ncA = _prog("A", lambda: build_phase_a(T))
    ncB = _prog("B", lambda: build_phase_b(S, 4, T))
    for l in range(2):
        wl = np.ascontiguousarray(np.asarray(w_in[l], f32))
        insA = []
        for c in range(8):
            sl = slice(c * T, (c + 1) * T)
            insA.append({
                "x": xf[sl],
                "posr": np.ascontiguousarray(np.broadcast_to(pos[sl][None, :], (32, T))),
                "w_in": wl,
                "w_uq": np.asarray(mla_w_uq[l], f32), "w_ukv": np.asarray(mla_w_ukv[l], f32),
                "qn": np.ascontiguousarray(np.asarray(mla_q_norm[l], f32).reshape(2, 128).T),
                "kvn": np.asarray(mla_kv_norm[l], f32).reshape(128, 1),
                "ident": ident, "invf": invf})
        rA = _run(ncA, insA)
        insB = []
        for c in range(8):
            sq_, half = c // 2, c % 2
            r0, r1 = rA[2 * sq_], rA[2 * sq_ + 1]

            def cat(name, axis):
                return np.concatenate([np.asarray(r0[name]), np.asarray(r1[name])], axis=axis)
            scch_f = cat("scch_o", 1)
            cfu_f = cat("cfu_o", 1)
            zt = scch_f.dtype
            scch_p = np.concatenate([np.zeros((512, 2), zt), scch_f], axis=1)
            cfu_p = np.concatenate([np.zeros((512, 30), zt), cfu_f], axis=1)
            t0 = half * T
            hs = slice(half * 4, half * 4 + 4)
            hw = slice(half * 256, half * 256 + 256)
            insB.append({
                "scb": np.ascontiguousarray(np.asarray(rA[c]["scb_o"])),
                "scch": np.ascontiguousarray(scch_p[:, t0:t0 + T + 2]),
                "cfu": np.ascontiguousarray(cfu_p[:, t0:t0 + T + 30]),
                "sc_w": np.ascontiguousarray(np.asarray(sc_conv[l], f32).T.reshape(4, 128, 3).transpose(1, 0, 2)),
                "cf_w": np.ascontiguousarray(np.asarray(cf_conv[l], f32).T.reshape(4, 128, 31).transpose(1, 0, 2)),
                "cf_p": np.ascontiguousarray(np.stack([_col4(np.asarray(cf_conv_bias[l], f32)),
                                                       _col4(np.asarray(cf_ln_g[l], f32)),
                                                       _col4(np.asarray(cf_ln_b[l], f32))], 1)),
                "mq": np.ascontiguousarray(cat("mq_o", 2)[hs]),
                "mk": np.ascontiguousarray(cat("mk_o", 2)[hs]),
                "mv": np.ascontiguousarray(cat("mv_o", 0)[:, hw]),
                "sq": np.ascontiguousarray(cat("sq_o", 1)[hw]),
                "sk": np.ascontiguousarray(cat("sk_o", 1)[hw]),
                "sv": np.ascontiguousarray(cat("sv_o", 0)[:, hw]),
                "cst": cst})
        rB = _run(ncB, insB)
        moe = (l % 2 == 1)
        E = 8 if moe else 1
        ncC = _prog("C%d" % E, lambda: build_phase_c(T, E))
        i = l // 2
        if moe:
            fg, fu, fd = np.asarray(exp_w_gate[i], f32), np.asarray(exp_w_up[i], f32), np.asarray(exp_w_down[i], f32)
            wr = np.ascontiguousarray(np.asarray(router_w[i], f32).reshape(8, 128, 8).transpose(1, 0, 2))
        else:
            fg, fu, fd = (np.asarray(ffn_w_gate[i:i + 1], f32), np.asarray(ffn_w_up[i:i + 1], f32),
                          np.asarray(ffn_w_down[i:i + 1], f32))
            wr = np.zeros((128, 8, 8), f32)
        lnp = np.ascontiguousarray(np.stack([ln_mix_g[l], ln_mix_b[l], ln_ffn_g[l], ln_ffn_b[l]]).astype(f32))
        bgc = np.ascontiguousarray(np.asarray(b_gate[l], f32).reshape(32, 128).T)
        insC = []
        for c in range(8):
            sq_, half = c // 2, c % 2
            t0 = half * T
            br2 = np.concatenate([np.asarray(rB[2 * sq_]["mo"]), np.asarray(rB[2 * sq_ + 1]["mo"])], axis=0)[:, t0:t0 + T]
            br3 = np.concatenate([np.asarray(rB[2 * sq_]["so"]), np.asarray(rB[2 * sq_ + 1]["so"])], axis=0)[:, t0:t0 + T]
            brs_ = np.ascontiguousarray(np.stack([np.asarray(rB[c]["br0"]), np.asarray(rB[c]["br1"]), br2, br3]))
            insC.append({
                "x": xf[c * T:(c + 1) * T], "xT": np.ascontiguousarray(np.asarray(rA[c]["xT_o"])),
                "brs": brs_, "w_in": wl, "bgc": bgc,
                "w_br": np.asarray(w_branch[l], f32), "w_o": np.asarray(w_out[l], f32),
                "lnp": lnp, "f_g": fg, "f_u": fu, "f_d": fd, "wr": wr, "ident": ident})
        rC = _run(ncC, insC)
        xf = np.ascontiguousarray(np.concatenate([np.asarray(r["xo"], f32) for r in rC], axis=0))
    return xf.reshape(4, S, D).astype(f32)


def kernel(**inp):
    S = 4096
    nc = _prog("F", lambda: build_fused(S, 2048))
    maps = [fused_inputs(inp, s_, S) for s_ in range(4)]
    keep = ("ident", "cst", "invf")
    zero_map = {k: (v if k in keep else np.zeros_like(v)) for k, v in maps[0].items()}
    active = [0, 1, 4, 5]
    in_maps = [zero_map] * 8
    in_maps = list(in_maps)
    for s_, c in enumerate(active):
        in_maps[c] = maps[s_]
    res = run_bass_kernel_spmd(nc, in_maps, core_ids=list(range(8)))
    out = np.stack([np.asarray(res.results[c]["xo"], np.float32) for c in active], axis=0)
    return out.astype(np.float32)
```
